# Optimizing a Trainium2 kernel written in Bass

```python
import jax, jax.numpy as jnp
from jax import lax
import numpy as np

D_MODEL = 1024
BATCH = 8
SEQ = 2048
DEPTH = 1

GRID_W = 64
CTX_LEN = 256
HEAD_DIM = 64
A_HEADS = 8
A_KV_HEADS = 2
A_WINDOW = 128
A_BLOCK = 128
B_HEADS = 8
B_WIN_ROWS = 8
B_WIN_COLS = 16
N_EXPERTS = 32
TOP_K = 4
D_EXPERT = D_MODEL
SWIGLU_LIMIT = 7.0
SWIGLU_ALPHA = 1.702
ROPE_THETA = 10000.0
NORM_EPS = 1e-6
MOE_BLOCK = 128
A_Q = A_HEADS * HEAD_DIM
A_KV = A_KV_HEADS * HEAD_DIM
B_W = B_HEADS * HEAD_DIM
IN_COLS = A_Q + 2 * A_KV + 3 * B_W + 2 * D_MODEL

kernel_name = "hybrid_dit_window_natten_moe"


def rms_norm(x, g):
    xf = x.astype(jnp.float32)
    y = xf * lax.rsqrt(jnp.mean(xf * xf, axis=-1, keepdims=True) + NORM_EPS)
    return (y * g.astype(jnp.float32)).astype(x.dtype)


def modulate(h, shift, scale):
    return h * (1 + scale) + shift


def softmax_with_sink(s, sink):
    if sink is None:
        return jax.nn.softmax(s, axis=-1)
    sink = sink.astype(jnp.float32)
    m = jnp.maximum(jnp.max(s, axis=-1, keepdims=True), sink)
    e = jnp.exp(s - m)
    return e / (jnp.sum(e, axis=-1, keepdims=True) + jnp.exp(sink - m))


def axial_rope_tables(n_tokens):
    t = jnp.arange(n_tokens, dtype=jnp.int32)
    row = (t // GRID_W).astype(jnp.float32)
    col = (t % GRID_W).astype(jnp.float32)
    n_freq = HEAD_DIM // 4
    inv_freq = ROPE_THETA ** (-jnp.arange(n_freq, dtype=jnp.float32) / n_freq)
    ang = jnp.concatenate([row[:, None] * inv_freq, col[:, None] * inv_freq], axis=-1)
    return jnp.cos(ang), jnp.sin(ang)


def apply_axial_rope(x, cos, sin):
    b, s, h, d = x.shape
    nf = HEAD_DIM // 4
    xr = x.reshape(b, s, h, 2, 2, nf)
    x1, x2 = xr[..., 0, :], xr[..., 1, :]
    c = cos.reshape(s, 1, 2, nf).astype(x.dtype)
    sn = sin.reshape(s, 1, 2, nf).astype(x.dtype)
    out = jnp.stack([x1 * c - x2 * sn, x2 * c + x1 * sn], axis=-2)
    return out.reshape(b, s, h, d)


def split_projection(p):
    bounds = [A_Q, A_Q + A_KV, A_Q + 2 * A_KV, A_Q + 2 * A_KV + B_W,
              A_Q + 2 * A_KV + 2 * B_W, A_Q + 2 * A_KV + 3 * B_W,
              A_Q + 2 * A_KV + 3 * B_W + D_MODEL]
    qa, ka, va, qb, kb, vb, ga, gb = jnp.split(p, bounds, axis=-1)
    heads = lambda t, n: t.reshape(*t.shape[:-1], n, HEAD_DIM)
    return (heads(qa, A_HEADS), heads(ka, A_KV_HEADS), heads(va, A_KV_HEADS),
            heads(qb, B_HEADS), heads(kb, B_HEADS), heads(vb, B_HEADS), ga, gb)


def windowed_sink_attention(q, k, v, k_ctx, v_ctx, sink):
    b, s, _, hd = q.shape
    nb = s // A_BLOCK
    g = A_HEADS // A_KV_HEADS
    scale = HEAD_DIM ** -0.5
    qb = q.reshape(b, nb, A_BLOCK, A_KV_HEADS, g, hd)
    pad = ((0, 0), (A_BLOCK, A_BLOCK), (0, 0), (0, 0))
    kp = jnp.pad(k, pad).reshape(b, nb + 2, A_BLOCK, A_KV_HEADS, hd)
    vp = jnp.pad(v, pad).reshape(b, nb + 2, A_BLOCK, A_KV_HEADS, hd)
    kb = jnp.concatenate([kp[:, :-2], kp[:, 1:-1], kp[:, 2:]], axis=2)
    vb = jnp.concatenate([vp[:, :-2], vp[:, 1:-1], vp[:, 2:]], axis=2)
    s_loc = jnp.einsum('bnqkgd,bnjkd->bnkgqj', qb, kb).astype(jnp.float32) * scale
    qpos = jnp.arange(nb)[:, None] * A_BLOCK + jnp.arange(A_BLOCK)[None, :]
    kpos = jnp.arange(nb)[:, None] * A_BLOCK - A_BLOCK + jnp.arange(3 * A_BLOCK)[None, :]
    valid = ((jnp.abs(qpos[:, :, None] - kpos[:, None, :]) <= A_WINDOW)
             & (kpos >= 0)[:, None, :] & (kpos < s)[:, None, :])
    s_loc = jnp.where(valid[None, :, None, None], s_loc, -jnp.inf)
    s_ctx = jnp.einsum('bnqkgd,bckd->bnkgqc', qb, k_ctx).astype(jnp.float32) * scale
    n_loc = s_loc.shape[-1]
    p = softmax_with_sink(jnp.concatenate([s_loc, s_ctx], axis=-1),
                          sink.reshape(A_KV_HEADS, g)[None, None, :, :, None, None])
    p = p.astype(v.dtype)
    o = (jnp.einsum('bnkgqj,bnjkd->bnqkgd', p[..., :n_loc], vb)
         + jnp.einsum('bnkgqc,bckd->bnqkgd', p[..., n_loc:], v_ctx))
    return o.reshape(b, s, A_Q)


def neighbourhood_attention(q, k, v, k_ctx, v_ctx, rpb):
    b, s, h, hd = q.shape
    rows = s // GRID_W
    kr = min(B_WIN_ROWS, rows)
    kc = B_WIN_COLS
    scale = HEAD_DIM ** -0.5
    r = jnp.arange(rows)
    cidx = jnp.arange(GRID_W)
    row_start = jnp.clip(r - kr // 2, 0, rows - kr)
    row_idx = row_start[:, None] + jnp.arange(kr)[None, :]
    col_start = jnp.clip(cidx - kc // 2, 0, GRID_W - kc)
    col_in = (cidx[None, :] >= col_start[:, None]) & (cidx[None, :] < col_start[:, None] + kc)
    qg = q.reshape(b, rows, GRID_W, h, hd)
    kg = k.reshape(b, rows, GRID_W, h, hd)[:, row_idx]
    vg = v.reshape(b, rows, GRID_W, h, hd)[:, row_idx]
    s_loc = jnp.einsum('brqhd,brkwhd->bhrqkw', qg, kg).astype(jnp.float32) * scale
    roff = row_idx - r[:, None] + (B_WIN_ROWS - 1)
    coff = jnp.clip(cidx[None, :] - cidx[:, None], -(kc - 1), kc - 1) + (B_WIN_COLS - 1)
    bias = rpb[:, roff[:, None, :, None], coff[None, :, None, :]]
    s_loc = jnp.where(col_in[None, None, None, :, None, :], s_loc + bias[None].astype(jnp.float32), -jnp.inf)
    s_loc = s_loc.reshape(b, h, rows, GRID_W, kr * GRID_W)
    s_ctx = jnp.einsum('brqhd,bchd->bhrqc', qg, k_ctx).astype(jnp.float32) * scale
    n_loc = kr * GRID_W
    p = jax.nn.softmax(jnp.concatenate([s_loc, s_ctx], axis=-1), axis=-1).astype(v.dtype)
    p_loc = p[..., :n_loc].reshape(b, h, rows, GRID_W, kr, GRID_W)
    o = (jnp.einsum('bhrqkw,brkwhd->brqhd', p_loc, vg)
         + jnp.einsum('bhrqc,bchd->brqhd', p[..., n_loc:], v_ctx))
    return o.reshape(b, s, B_W)


def dense_context_attention(q, k, v, sink):
    b, c, h, hd = q.shape
    n_kv = k.shape[2]
    g = h // n_kv
    qg = q.reshape(b, c, n_kv, g, hd)
    s = jnp.einsum('bqkgd,bckd->bkgqc', qg, k).astype(jnp.float32) * (HEAD_DIM ** -0.5)
    snk = None if sink is None else sink.reshape(n_kv, g)[None, :, :, None, None]
    p = softmax_with_sink(s, snk).astype(v.dtype)
    o = jnp.einsum('bkgqc,bckd->bqkgd', p, v)
    return o.reshape(b, c, h * hd)


def merge_branches(ya, yb, ga, gb, w_branch_a, w_branch_b, w_out):
    merged = jax.nn.sigmoid(ga) * (ya @ w_branch_a) + jax.nn.sigmoid(gb) * (yb @ w_branch_b)
    return merged @ w_out


def moe_ffn(h, w_router, b_router, w_gate_up, b_gate_up, w_down, b_down):
    shp = h.shape
    xt = h.reshape(-1, D_MODEL)
    t = xt.shape[0]
    logits = (xt @ w_router + b_router).astype(jnp.float32)
    top_val, top_idx = lax.top_k(logits, TOP_K)
    weights = jax.nn.softmax(top_val, axis=-1)
    n_assign = t * TOP_K
    expert = top_idx.reshape(-1)
    token = jnp.arange(n_assign, dtype=jnp.int32) // TOP_K
    order = jnp.argsort(expert)
    sorted_expert = expert[order]
    sorted_token = token[order]
    counts = jnp.bincount(expert, length=N_EXPERTS)
    padded = ((counts + MOE_BLOCK - 1) // MOE_BLOCK) * MOE_BLOCK
    start = jnp.cumsum(counts) - counts
    pend = jnp.cumsum(padded)
    pstart = pend - padded
    dest = pstart[sorted_expert] + (jnp.arange(n_assign) - start[sorted_expert])
    n_blocks = -(-n_assign // MOE_BLOCK) + N_EXPERTS
    x_pad = jnp.zeros((n_blocks * MOE_BLOCK, D_MODEL), xt.dtype).at[dest].set(xt[sorted_token])
    block_expert = jnp.clip(jnp.searchsorted(pend, jnp.arange(n_blocks) * MOE_BLOCK, side='right'),
                            0, N_EXPERTS - 1)

    def expert_block(args):
        xb, e = args
        gu = xb @ w_gate_up[e] + b_gate_up[e]
        gate, up = jnp.split(gu, 2, axis=-1)
        gate = jnp.minimum(gate, SWIGLU_LIMIT)
        up = jnp.clip(up, -SWIGLU_LIMIT, SWIGLU_LIMIT)
        glu = gate * jax.nn.sigmoid(SWIGLU_ALPHA * gate)
        return ((up + 1) * glu) @ w_down[e] + b_down[e]

    y_pad = lax.map(expert_block, (x_pad.reshape(n_blocks, MOE_BLOCK, D_MODEL), block_expert))
    y_sorted = y_pad.reshape(-1, D_MODEL)[dest] * weights.reshape(-1)[order][:, None].astype(xt.dtype)
    y = jax.ops.segment_sum(y_sorted, sorted_token, num_segments=t)
    return y.reshape(shp)


def setup_inputs(seed: int = 0) -> dict:
    key = jax.random.key(seed)
    ks = jax.random.split(key, 24)
    f = jnp.float32
    L = DEPTH

    def nrm(k, shape, scale):
        return jax.random.normal(k, shape, f) * scale

    return {
        "x": nrm(ks[0], (BATCH, SEQ, D_MODEL), 1.0),
        "c": nrm(ks[1], (BATCH, D_MODEL), 1.0),
        "ctx": nrm(ks[2], (BATCH, CTX_LEN, D_MODEL), 1.0),
        "c_ctx": nrm(ks[3], (D_MODEL,), 1.0),
        "w_mod": nrm(ks[4], (L, D_MODEL, 6 * D_MODEL), 0.3 * D_MODEL ** -0.5),
        "b_mod": nrm(ks[5], (L, 6 * D_MODEL), 0.02),
        "g_pre_mix": 1.0 + nrm(ks[6], (L, D_MODEL), 0.05),
        "g_post_mix": 1.0 + nrm(ks[7], (L, D_MODEL), 0.05),
        "g_pre_ffn": 1.0 + nrm(ks[8], (L, D_MODEL), 0.05),
        "g_post_ffn": 1.0 + nrm(ks[9], (L, D_MODEL), 0.05),
        "w_in": nrm(ks[10], (L, D_MODEL, IN_COLS), D_MODEL ** -0.5),
        "a_sink": nrm(ks[11], (L, A_HEADS), 0.5),
        "b_rpb": nrm(ks[12], (L, B_HEADS, 2 * B_WIN_ROWS - 1, 2 * B_WIN_COLS - 1), 0.2),
        "w_branch_a": nrm(ks[13], (L, A_Q, D_MODEL), A_Q ** -0.5),
        "w_branch_b": nrm(ks[14], (L, B_W, D_MODEL), B_W ** -0.5),
        "w_out": nrm(ks[15], (L, D_MODEL, D_MODEL), D_MODEL ** -0.5),
        "w_router": nrm(ks[16], (L, D_MODEL, N_EXPERTS), D_MODEL ** -0.5),
        "b_router": nrm(ks[17], (L, N_EXPERTS), 0.01),
        "w_gate_up": nrm(ks[18], (L, N_EXPERTS, D_MODEL, 2 * D_EXPERT), D_MODEL ** -0.5),
        "b_gate_up": nrm(ks[19], (L, N_EXPERTS, 2 * D_EXPERT), 0.02),
        "w_down": nrm(ks[20], (L, N_EXPERTS, D_EXPERT, D_MODEL), D_EXPERT ** -0.5),
        "b_down": nrm(ks[21], (L, N_EXPERTS, D_MODEL), 0.02),
    }


def reference(x, c, ctx, c_ctx, w_mod, b_mod, g_pre_mix, g_post_mix, g_pre_ffn, g_post_ffn,
              w_in, a_sink, b_rpb, w_branch_a, w_branch_b, w_out, w_router, b_router,
              w_gate_up, b_gate_up, w_down, b_down):
    cos, sin = axial_rope_tables(x.shape[1])
    for l in range(DEPTH):
        mod_x = (jax.nn.silu(c) @ w_mod[l] + b_mod[l])[:, None, :]
        mod_c = (jax.nn.silu(c_ctx) @ w_mod[l] + b_mod[l])[None, None, :]
        sh1, sc1, g1, sh2, sc2, g2 = jnp.split(mod_x, 6, axis=-1)
        csh1, csc1, cg1, csh2, csc2, cg2 = jnp.split(mod_c, 6, axis=-1)

        hx = modulate(rms_norm(x, g_pre_mix[l]), sh1, sc1)
        hc = modulate(rms_norm(ctx, g_pre_mix[l]), csh1, csc1)
        qa, ka, va, qb, kb, vb, ga, gb = split_projection(hx @ w_in[l])
        cqa, cka, cva, cqb, ckb, cvb, cga, cgb = split_projection(hc @ w_in[l])
        qa = apply_axial_rope(qa, cos, sin)
        ka = apply_axial_rope(ka, cos, sin)
        ya = windowed_sink_attention(qa, ka, va, cka, cva, a_sink[l])
        yb = neighbourhood_attention(qb, kb, vb, ckb, cvb, b_rpb[l])
        mix = merge_branches(ya, yb, ga, gb, w_branch_a[l], w_branch_b[l], w_out[l])
        x = x + g1 * rms_norm(mix, g_post_mix[l])

        hx2 = modulate(rms_norm(x, g_pre_ffn[l]), sh2, sc2)
        ffn = moe_ffn(hx2, w_router[l], b_router[l], w_gate_up[l], b_gate_up[l], w_down[l], b_down[l])
        x = x + g2 * rms_norm(ffn, g_post_ffn[l])

        if l < DEPTH - 1:
            cya = dense_context_attention(cqa, cka, cva, a_sink[l])
            cyb = dense_context_attention(cqb, ckb, cvb, None)
            cmix = merge_branches(cya, cyb, cga, cgb, w_branch_a[l], w_branch_b[l], w_out[l])
            ctx = ctx + cg1 * rms_norm(cmix, g_post_mix[l])
            hc2 = modulate(rms_norm(ctx, g_pre_ffn[l]), csh2, csc2)
            cffn = moe_ffn(hc2, w_router[l], b_router[l], w_gate_up[l], b_gate_up[l], w_down[l], b_down[l])
            ctx = ctx + cg2 * rms_norm(cffn, g_post_ffn[l])
    return x
```

```python
import numpy as np
import ml_dtypes
from contextlib import ExitStack
import concourse.bass as bass
import concourse.mybir as mybir
from concourse.bass_utils import run_bass_kernel_spmd

F32 = mybir.dt.float32
BF16 = mybir.dt.bfloat16
I32 = mybir.dt.int32
ALU = mybir.AluOpType
AF = mybir.ActivationFunctionType

D = 1024
GRID_W = 64
HD = 64
TOPK = 4
NEG = -30000.0
EPS = 1e-6
FULL = dict(S=2048, CT=256, E=32, CAP=512)


class Q:
    def __init__(self, kb, name):
        self.name = name
        self.ops = []
        self.sem = kb.new_sem("q_" + name)
        self.count = 0
        self.seen = {}

    def wait(self, *tickets):
        for t in tickets:
            if t is None:
                continue
            sem, val = t
            key = id(sem)
            if self.seen.get(key, 0) >= val:
                continue
            self.seen[key] = val
            self.ops.append(lambda e, sem=sem, val=val: e.wait_ge(sem, val))

    def last(self):
        return (self.sem, self.count) if self.count else None


class Buf:
    def __init__(self, name, nowaw=False):
        self.name = name
        self.w = []
        self.r = []
        self.ds = None
        self.nowaw = nowaw


class KB:
    def __init__(self, nc, stack):
        self.nc = nc
        self.stack = stack
        self.dsems = []
        self.pe, self.act, self.dve, self.pool, self.sp = (Q(self, n) for n in ("pe", "act", "dve", "pool", "sp"))
        self.qs = [self.pe, self.act, self.dve, self.pool, self.sp]

    def new_sem(self, name):
        self.nsem = getattr(self, "nsem", 0) + 1
        return self.stack.enter_context(self.nc.semaphore("%s_%d" % (name, self.nsem)))

    def sb(self, name, shape, dt):
        return self.stack.enter_context(self.nc.sbuf_tensor("s_" + name, shape, dt))

    def ps(self, name, shape, dt):
        return self.stack.enter_context(self.nc.psum_tensor(name, shape, dt))

    def _pre(self, q, reads, writes):
        for b in reads:
            q.wait(*b.w)
        for b in writes:
            if not b.nowaw:
                q.wait(*b.w)
            q.wait(*b.r)

    def _post(self, t, reads, writes):
        for b in reads:
            b.r.append(t)
        for b in writes:
            if b.nowaw:
                b.w = [x for x in b.w if x[0] is not t[0]] + [t]
            else:
                b.w = [t]
            b.r = []

    def op(self, q, fn, reads=(), writes=()):
        self._pre(q, reads, writes)
        q.count += 1
        sem = q.sem
        q.ops.append(lambda e, fn=fn, sem=sem: fn(e).then_inc(sem, 1))
        t = (sem, q.count)
        self._post(t, reads, writes)
        return t

    def group(self, q, fns, reads=(), writes=()):
        self._pre(q, reads, writes)
        for fn in fns[:-1]:
            q.ops.append(lambda e, fn=fn: fn(e))
        q.count += 1
        sem = q.sem
        fn = fns[-1]
        q.ops.append(lambda e, fn=fn, sem=sem: fn(e).then_inc(sem, 1))
        t = (sem, q.count)
        self._post(t, reads, writes)
        return t

    def dma(self, q, fns, reads=(), writes=(), semof=None):
        if not isinstance(fns, (list, tuple)):
            fns = [fns]
        self._pre(q, reads, writes)
        b = semof if semof is not None else writes[0]
        if b.ds is None:
            b.ds = {}
        if q.name not in b.ds:
            b.ds[q.name] = [self.new_sem("d_" + b.name), 0]
            self.dsems.append(b.ds[q.name])
        d = b.ds[q.name]
        for fn in fns:
            d[1] += 16
            s = d[0]
            q.ops.append(lambda e, fn=fn, s=s: fn(e).then_inc(s, 16))
        t = (d[0], d[1])
        self._post(t, reads, writes)
        return t

    def barrier(self):
        ts = [q.last() for q in self.qs] + [(d[0], d[1]) for d in self.dsems if d[1]]
        for q in self.qs:
            q.wait(*ts)

    def emit(self):
        with self.nc.Block() as block:
            @block.tensor
            def _(e):
                for f in self.pe.ops:
                    f(e)

            @block.scalar
            def _(e):
                for f in self.act.ops:
                    f(e)

            @block.vector
            def _(e):
                for f in self.dve.ops:
                    f(e)

            @block.gpsimd
            def _(e):
                for f in self.pool.ops:
                    f(e)

            @block.sync
            def _(e):
                for f in self.sp.ops:
                    f(e)


class Arena:
    def __init__(self, t, n, dt):
        self.t, self.n, self.dt, self.off = t, n, dt, 0

    def reset(self, off=0):
        self.off = off

    def take(self, n, shape=None):
        assert self.off + n <= self.n, ("arena overflow", self.dt, self.off, n, self.n)
        v = self.t[:, self.off:self.off + n]
        self.off += (n + 31) // 32 * 32
        if shape is not None:
            names = " ".join("d%d" % i for i in range(len(shape)))
            v = v.rearrange("p (%s) -> p %s" % (names, names), **{"d%d" % i: s for i, s in enumerate(shape)})
        return v


def rope_tables(S):
    t = np.arange(S, dtype=np.int32)
    row = (t // GRID_W).astype(np.float32)
    col = (t % GRID_W).astype(np.float32)
    nf = HD // 4
    inv = (np.float32(10000.0) ** (-np.arange(nf, dtype=np.float32) / np.float32(nf))).astype(np.float32)
    ang = np.concatenate([row[:, None] * inv, col[:, None] * inv], axis=-1).astype(np.float32)
    cos, sin = np.cos(ang).astype(np.float32), np.sin(ang).astype(np.float32)
    cosT = np.zeros((HD, S), np.float32)
    sinT = np.zeros((HD, S), np.float32)
    perm = np.zeros((HD, HD), np.float32)
    for d in range(HD):
        axis, half, f = d // 32, (d % 32) // 16, d % 16
        cosT[d] = cos[:, axis * nf + f]
        if half == 0:
            sinT[d] = -sin[:, axis * nf + f]
            perm[d + 16, d] = 1.0
        else:
            sinT[d] = sin[:, axis * nf + f]
            perm[d - 16, d] = 1.0
    cos2 = np.concatenate([cosT, cosT], 0)
    sin2 = np.concatenate([sinT, sinT], 0)
    perm2 = np.zeros((128, 128), np.float32)
    perm2[:64, :64] = perm
    perm2[64:, 64:] = perm
    return cos2, sin2, perm2


def nbr_patterns(S):
    rows = S // GRID_W
    kr = min(8, rows)
    kc = 16
    NT = S // 128
    pats, pat_idx, chunks = [], {}, []
    qi = np.arange(128)
    kj = np.arange(128)
    qcol = (qi % 64)[None, :]
    kcol = (kj % 64)[:, None]
    col_start = np.clip(qcol - kc // 2, 0, GRID_W - kc)
    col_ok = (kcol >= col_start) & (kcol < col_start + kc)
    dc = np.clip(kcol - qcol, -(kc - 1), kc - 1) + 15
    for p in range(NT):
        lst = []
        for c in range(NT):
            qrow = (2 * p + qi // 64)[None, :]
            krow = (2 * c + kj // 64)[:, None]
            rs = np.clip(qrow - kr // 2, 0, rows - kr)
            ok = (krow >= rs) & (krow < rs + kr) & col_ok
            if not ok.any():
                continue
            dr = np.where(ok, krow - qrow + 7, 0)
            dcc = np.where(ok, dc, 0)
            key = (ok.tobytes(), dr.astype(np.int16).tobytes(), dcc.astype(np.int16).tobytes())
            if key not in pat_idx:
                pat_idx[key] = len(pats)
                pats.append((ok, dr, dcc))
            lst.append((c, pat_idx[key]))
        chunks.append(lst)
    return pats, chunks


def build(cfg, stop_after=99, debug=False):
    S, CT, E, CAP = cfg["S"], cfg["CT"], cfg["E"], cfg["CAP"]
    NT, NCT = S // 128, CT // 128
    NTT = NT + NCT
    BS = min(512, S)
    NB = S // BS
    NSLOT = E * CAP
    NJ = CAP // 128
    pats, bchunks = nbr_patterns(S)
    NPAT = len(pats)
    use_cnt = np.zeros(NPAT, int)
    for lst in bchunks:
        for _, pi in lst:
            use_cnt[pi] += 1
    res_p = list(np.argsort(-use_cnt)[:5])
    res_slot = {int(p): i for i, p in enumerate(res_p)}

    nc = bass.Bass("TRN2", target_bir_lowering=False)

    def din(name, shape, dt=F32):
        return nc.dram_tensor(name, list(shape), dt, kind="ExternalInput").ap()

    x_d = din("x", [S, D]); ctx_d = din("ctx", [CT, D]); cT_d = din("cT", [128, 16])
    wmod_d = din("w_mod", [D, 6 * D]); bmod_d = din("b_mod", [1, 6 * D])
    g_d = [din(n, [1, D]) for n in ("g_pre_mix", "g_post_mix", "g_pre_ffn", "g_post_ffn")]
    win_d = din("w_in", [D, 4352]); sink_d = din("a_sink", [1, 8])
    rpbx_d = din("rpbx", [NPAT, 128, 8, 128]); maskb_d = din("maskb", [NPAT, 128, 128])
    wba_d = din("w_branch_a", [512, D]); wbb_d = din("w_branch_b", [512, D]); wout_d = din("w_out", [D, D])
    wr_d = din("w_router", [D, E]); br_d = din("b_router", [1, E])
    wgu_d = din("w_gate_up", [E, D, 2 * D]); bgu_d = din("b_gate_up", [E, 2 * D])
    wd_d = din("w_down", [E, D, D]); bd_d = din("b_down", [E, D])
    identf_d = din("identf", [128, 128]); tri_d = din("tri", [128, 128], BF16); identb_d = din("identb", [128, 128], BF16)
    mA_d = din("maskA", [2, 128, 128], BF16); perm_d = din("perm2", [128, 128], BF16)
    cos_d = din("cos2", [128, S]); sin_d = din("sin2", [128, S]); base_d = din("baseE", [128, E])
    out_d = nc.dram_tensor("out", [S, D], F32, kind="ExternalOutput").ap()
    xs_d = nc.dram_tensor("xs", [NSLOT + 128, D], BF16, kind="Internal").ap()
    ys_d = nc.dram_tensor("ys", [NSLOT + 128, D], F32, kind="Internal").ap()
    win_v = win_d.rearrange("(k p) n -> p k n", p=128)
    wmod_v = wmod_d.rearrange("(k p) n -> p k n", p=128)

    st = ExitStack()
    with st:
        kb = KB(nc, st)
        pe, act, dve, pool, sp = kb.pe, kb.act, kb.dve, kb.pool, kb.sp

        def dump(name, ap, dt):
            if not debug:
                return
            kb.barrier()
            shp = list(ap.shape)
            flat = [shp[0], int(np.prod(shp[1:]))]
            dd = nc.dram_tensor("dbg_" + name, flat, dt, kind="ExternalOutput").ap()
            src = ap
            if len(shp) == 3:
                dd = dd.rearrange("p (a b) -> p a b", a=shp[1])
            elif len(shp) == 4:
                dd = dd.rearrange("p (a b c) -> p a b c", a=shp[1], b=shp[2])
            kb.dma(sp, lambda e: e.dma_start(out=dd, in_=src), writes=[Buf("dbg_" + name)])
            kb.barrier()
        NBF = 66 * 1024
        NF = 10 * 1024
        AB = Arena(kb.sb("arenaB", [128, NBF], BF16), NBF, BF16)
        AFr = Arena(kb.sb("arenaF", [128, NF], F32), NF, F32)
        P1g = kb.sb("P1g", [128, D], F32); G2 = kb.sb("G2", [128, D], F32)
        sh2 = kb.sb("sh2", [128, D], F32); P2g = kb.sb("P2g", [128, D], F32)
        identb = kb.sb("identb", [128, 128], BF16); identf = kb.sb("identf", [128, 128], F32)
        tri = kb.sb("tri", [128, 128], BF16); onesb = kb.sb("onesb", [128, 128], BF16)
        mA = kb.sb("mA", [128, 2, 128], BF16); perm2 = kb.sb("perm2", [128, 128], BF16)
        esink = kb.sb("esink", [128, 8], F32)
        small = kb.sb("small", [128, 64 + 4 * NTT], F32)
        wts = kb.sb("wts", [128, NT, 4], F32); desti = kb.sb("desti", [128, NT, 4], I32)
        bguT = kb.sb("bguT", [128, 16, E], F32)
        baseE = kb.sb("baseE", [128, E], F32); cb = kb.sb("cb", [128, E], F32); brep = kb.sb("brep", [128, E], F32)
        wr_sb = kb.sb("wr", [128, 8, E], F32)
        junkb = kb.sb("junkb", [128, D], BF16)
        dnt = kb.sb("dnt", [128, 2, 4], F32)
        zerob = kb.sb("zerob", [128, D], BF16); zerof = kb.sb("zerof", [128, D], F32)
        psA = kb.ps("psA", [128, 1024], F32); psB = kb.ps("psB", [128, 1024], F32)
        psC = kb.ps("psC", [128, 1024], F32); psD = kb.ps("psD", [128, 1024], F32)
        bA, bB, bC, bD = Buf("psA"), Buf("psB"), Buf("psC"), Buf("psD")
        banks = []
        bbufs = [Buf("bank%d" % i) for i in range(8)]
        for i, t in enumerate((psA, psB, psC, psD)):
            banks.append(t[:, 0:512]); banks.append(t[:, 512:1024])
        ssq_i = [0]

        def col():
            c = ssq_i[0] % 64
            ssq_i[0] += 1
            return small[:, c:c + 1]

        B_ = lambda n: Buf(n)
        consts = B_("consts")
        cl = [
            (identb, identb_d), (identf, identf_d), (tri, tri_d), (perm2, perm_d),
            (baseE, base_d),
        ]
        kb.dma(sp, [lambda e, o=o, i=i: e.dma_start(out=o[:], in_=i) for o, i in cl] +
               [lambda e: e.dma_start(out=mA[:], in_=mA_d.rearrange("a p q -> p a q")),
                lambda e: e.dma_start(out=esink[:], in_=sink_d.partition_broadcast(128)),
                lambda e: e.dma_start(out=brep[:], in_=br_d.partition_broadcast(128)),
                lambda e: e.dma_start(out=wr_sb[:], in_=wr_d.rearrange("(k p) n -> p k n", p=128))],
               writes=[consts])
        kb.op(dve, lambda e: e.memset(onesb[:], 1.0), writes=[consts])
        kb.op(dve, lambda e: e.memset(cb[:], 0.0), writes=[consts])
        kb.op(dve, lambda e: e.memset(zerob[:], 0.0), writes=[consts])
        kb.op(dve, lambda e: e.memset(zerof[:], 0.0), writes=[consts])
        kb.op(act, lambda e: e.activation(out=esink[:], in_=esink[:], func=AF.Exp), reads=[], writes=[consts])
        xsb, ysb_b = Buf("xs", nowaw=True), Buf("ys", nowaw=True)
        xs_zero, ys_zero = B_("xsz"), B_("ysz")
        AFr.reset()
        bgu_rows = AFr.take(2 * D)
        bgr = B_("bgr")
        kb.dma(sp, lambda e: e.dma_start(out=bgu_rows[0:E, :], in_=bgu_d), writes=[bgr])
        for c in range(16):
            bk = c % 2
            kb.op(pe, lambda e, c=c, bk=bk: e.transpose(banks[bk][:, 0:E], bgu_rows[0:E, c * 128:(c + 1) * 128], identf[0:E, 0:E]),
                  reads=[bgr, consts], writes=[bbufs[bk]])
            if c < 8:
                kb.op(dve, lambda e, c=c, bk=bk: e.tensor_copy(out=bguT[:, c, :], in_=banks[bk][:, 0:E]), reads=[bbufs[bk]], writes=[consts])
            else:
                kb.op(dve, lambda e, c=c, bk=bk: e.tensor_scalar(out=bguT[:, c, :], in0=banks[bk][:, 0:E], scalar1=1.0, scalar2=None, op0=ALU.add),
                      reads=[bbufs[bk]], writes=[consts])
        kb.barrier()
        if stop_after == 0:
            kb.emit()
            return nc

        AB.reset(); AFr.reset()
        sh1 = AFr.take(D); G1 = AFr.take(D); csh1 = AFr.take(D); G1c = AFr.take(D)
        greps = [AFr.take(D) for _ in range(4)]
        bm = [AFr.take(512) for _ in range(2)]
        tmpm = AFr.take(512)
        cTt = AFr.take(16); sil = AFr.take(16)
        silrep = AB.take(16 * 128, [16, 128])
        wm = [AB.take(8 * 512, [8, 512]) for _ in range(2)]
        b_g, b_c, b_sil, b_tmp = B_("greps"), B_("cT"), B_("silrep"), B_("tmpm")
        b_bm = [B_("bm0"), B_("bm1")]; b_wm = [B_("wm0"), B_("wm1")]
        b_mod = B_("modout")
        kb.dma(sp, [lambda e, j=j: e.dma_start(out=greps[j], in_=g_d[j].partition_broadcast(128)) for j in range(4)], writes=[b_g])
        kb.dma(sp, lambda e: e.dma_start(out=cTt, in_=cT_d), writes=[b_c])
        kb.op(act, lambda e: e.activation(out=sil, in_=cTt, func=AF.Silu), reads=[b_c], writes=[b_sil])
        for j in range(16):
            kb.op(dve, lambda e, j=j: e.tensor_copy(out=silrep[:, j, :], in_=sil[:, j:j + 1].to_broadcast([128, 128])),
                  reads=[b_sil], writes=[b_sil])
        gi = 0
        for j in range(6):
            for half in range(2):
                c0 = j * D + half * 512
                s = gi % 2
                gi += 1
                kb.dma(pool, lambda e, s=s, c0=c0: e.dma_start(out=wm[s], in_=wmod_v[:, :, c0:c0 + 512]), writes=[b_wm[s]])
                kb.dma(sp, lambda e, s=s, c0=c0: e.dma_start(out=bm[s], in_=bmod_d[0:1, c0:c0 + 512].partition_broadcast(128)), writes=[b_bm[s]])
                cs = slice(half * 512, half * 512 + 512)
                for which in range(2 if j < 2 else 1):
                    bk = 2 * s + which
                    kb.group(pe, [lambda e, k=k, bk=bk, s=s, which=which: e.matmul(banks[bk], lhsT=silrep[:, which * 8 + k, :], rhs=wm[s][:, k, :],
                                                                                  start=(k == 0), stop=(k == 7)) for k in range(8)],
                             reads=[b_sil, b_wm[s]], writes=[bbufs[bk]])
                    if j == 0 or j == 3:
                        dst = (sh1 if j == 0 else sh2[:]) if which == 0 else csh1
                        kb.op(dve, lambda e, bk=bk, s=s, dst=dst, cs=cs: e.tensor_tensor(out=dst[:, cs], in0=banks[bk], in1=bm[s], op=ALU.add),
                              reads=[bbufs[bk], b_bm[s]], writes=[b_mod])
                    else:
                        kb.op(dve, lambda e, bk=bk, s=s: e.tensor_tensor(out=tmpm, in0=banks[bk], in1=bm[s], op=ALU.add),
                              reads=[bbufs[bk], b_bm[s]], writes=[b_tmp])
                        if j == 1:
                            dst, gr = (G1, greps[0]) if which == 0 else (G1c, greps[0])
                        elif j == 2:
                            dst, gr = P1g[:], greps[1]
                        elif j == 4:
                            dst, gr = G2[:], greps[2]
                        else:
                            dst, gr = P2g[:], greps[3]
                        if j in (1, 4):
                            kb.op(dve, lambda e, dst=dst, gr=gr, cs=cs: e.scalar_tensor_tensor(out=dst[:, cs], in0=tmpm, scalar=1.0, in1=gr[:, cs], op0=ALU.add, op1=ALU.mult),
                                  reads=[b_tmp, b_g], writes=[b_mod])
                        else:
                            kb.op(dve, lambda e, dst=dst, gr=gr, cs=cs: e.tensor_tensor(out=dst[:, cs], in0=tmpm, in1=gr[:, cs], op=ALU.mult),
                                  reads=[b_tmp, b_g], writes=[b_mod])
        kb.barrier()
        if stop_after == 1:
            kb.emit()
            return nc

        AB.reset()
        hxT = AB.take(8 * (S + CT), [8, S + CT])
        yT = [AB.take(4 * S, [4, S]) for _ in range(2)]
        AB_base = AB.off
        AFr.reset(4 * D)
        xt = [AFr.take(D) for _ in range(2)]
        tmpf = AFr.take(D)
        hxb = [AB.take(D) for _ in range(2)]
        b_xt = [B_("xt0"), B_("xt1")]; b_tmpf = B_("tmpf"); b_hxb = [B_("hxb0"), B_("hxb1")]
        b_small = B_("small"); b_hxT = B_("hxT")

        def rms_rstd(src_ap, src_bufs, q_reads=()):
            c1, c2 = col(), col()
            kb.op(act, lambda e: e.activation(out=junkb[:], in_=src_ap, func=AF.Square, accum_out=c1), reads=list(src_bufs), writes=[b_small])
            kb.op(dve, lambda e: e.tensor_scalar(out=c2, in0=c1, scalar1=1.0 / D, scalar2=EPS, op0=ALU.mult, op1=ALU.add), reads=[b_small], writes=[b_small])
            kb.op(act, lambda e: e.activation(out=c2, in_=c2, func=AF.Sqrt), reads=[b_small], writes=[b_small])
            kb.op(dve, lambda e: e.reciprocal(out=c2, in_=c2), reads=[b_small], writes=[b_small])
            return c2

        for i in range(NTT):
            s = i % 2
            src = x_d[i * 128:(i + 1) * 128, :] if i < NT else ctx_d[(i - NT) * 128:(i - NT + 1) * 128, :]
            Gm, shm = (G1, sh1) if i < NT else (G1c, csh1)
            kb.dma(sp, lambda e, s=s, src=src: e.dma_start(out=xt[s], in_=src), writes=[b_xt[s]])
            r = rms_rstd(xt[s], [b_xt[s]])
            kb.op(dve, lambda e, s=s, r=r, Gm=Gm: e.scalar_tensor_tensor(out=tmpf, in0=xt[s], scalar=r, in1=Gm, op0=ALU.mult, op1=ALU.mult),
                  reads=[b_xt[s], b_small, b_mod], writes=[b_tmpf])
            kb.op(pool, lambda e, s=s, shm=shm: e.tensor_tensor(out=hxb[s], in0=tmpf, in1=shm, op=ALU.add), reads=[b_tmpf, b_mod], writes=[b_hxb[s]])
            pb = psA if s == 0 else psB
            pbuf = bA if s == 0 else bB
            pbv = pb[:, 0:512].bitcast(BF16)
            kb.group(pe, [lambda e, k=k, s=s, pbv=pbv: e.transpose(pbv[:, k * 128:(k + 1) * 128], hxb[s][:, k * 128:(k + 1) * 128], identb[:]) for k in range(8)],
                     reads=[b_hxb[s], consts], writes=[pbuf])
            kb.op(act, lambda e, i=i, pbv=pbv: e.activation(out=hxT[:, :, i * 128:(i + 1) * 128], in_=pbv.rearrange("p (k t) -> p k t", k=8), func=AF.Copy),
                  reads=[pbuf], writes=[b_hxT])
        kb.barrier()
        dump("hxT", hxT, BF16)
        if stop_after == 2:
            kb.emit()
            return nc

        def attn_branch(kind, hg):
            AB.reset(AB_base); AFr.reset(0)
            if kind == "A":
                heads = list(range(8)); npair = 4; nkt = 2; nv = 2
                qc0, vc0 = 0, 640
            else:
                heads = list(range(4 * hg, 4 * hg + 4)); npair = 2; nkt = 2; nv = 4
                qc0, kc0, vc0 = 768 + hg * 256, 1280 + hg * 256, 1792 + hg * 256
            nh = len(heads)
            Wq = AB.take(8 * npair * 128, [8, npair * 128])
            Wk = AB.take(8 * nkt * 128, [8, nkt * 128])
            Wv = AB.take(8 * nv * 64, [8, nv * 64])
            QT = AB.take(npair * S, [npair, S])
            KT = AB.take(nkt * (S + CT), [nkt, S + CT])
            V = AB.take(NTT * nv * 80, [NTT, nv, 80])
            PT = [AB.take(7 * 128) for _ in range(2)]
            ytile = [AB.take(4 * 64, [4, 64]) for _ in range(2)]
            qf = AB.take(BS)
            b_W, b_QT, b_KT, b_V = B_("W"), B_("QT"), B_("KT"), B_("V")
            b_PT = [B_("PT0"), B_("PT1")]; b_yt = [B_("yt0"), B_("yt1")]; b_qf = B_("qf")
            b_yT = B_("yT")
            fl = [lambda e: e.dma_start(out=Wq, in_=win_v[:, :, qc0:qc0 + npair * 128]),
                  lambda e: e.dma_start(out=Wv, in_=win_v[:, :, vc0:vc0 + nv * 64])]
            if kind == "A":
                Wk4 = Wk.rearrange("p k (a b c) -> p k a b c", a=2, b=2)
                src = win_v[:, :, 512:640].rearrange("p k (a c) -> p k a c", a=2)
                for a_ in range(2):
                    for d_ in range(2):
                        fl.append(lambda e, a_=a_, d_=d_: e.dma_start(out=Wk4[:, :, a_, d_, :], in_=src[:, :, a_, :]))
            else:
                fl.append(lambda e: e.dma_start(out=Wk, in_=win_v[:, :, kc0:kc0 + nkt * 128]))
            kb.dma(pool, fl, writes=[b_W])
            kb.op(dve, lambda e: e.memset(V[:, :, :, 64:65], 1.0), writes=[b_V])
            if kind == "A":
                cos2 = AFr.take(S); sin2 = AFr.take(S)
                t1 = AFr.take(BS); t2 = AFr.take(BS)
                b_cs, b_t1, b_t2 = B_("cs"), B_("t1"), B_("t2")
                kb.dma(sp, [lambda e: e.dma_start(out=cos2, in_=cos_d), lambda e: e.dma_start(out=sin2, in_=sin_d)], writes=[b_cs])
            else:
                bres = AB.take(5 * 4 * 128, [5, 4, 128])
                bdyn = AB.take(5 * 4 * 128, [5, 4, 128])
                bst = AFr.take(4 * 128, [4, 128]); mst = AFr.take(128)
                b_bres, b_bdyn, b_bst = B_("bres"), B_("bdyn"), B_("bst")

                def load_pat(pi, dst, dbuf):
                    kb.dma(sp, [lambda e: e.dma_start(out=bst, in_=rpbx_d[pi, :, 4 * hg:4 * hg + 4, :]),
                                lambda e: e.dma_start(out=mst, in_=maskb_d[pi])], writes=[b_bst])
                    kb.op(dve, lambda e: e.tensor_tensor(out=dst, in0=bst, in1=mst.unsqueeze(1).to_broadcast([128, 4, 128]), op=ALU.add),
                          reads=[b_bst], writes=[dbuf])
                for pi, sl in res_slot.items():
                    load_pat(pi, bres[:, sl, :, :], b_bres)

            if stop_after == 2.1:
                kb.barrier()
                return
            def proj_fm(Wt, ct, col0, N, bk):
                kb.group(pe, [lambda e, k=k: e.matmul(banks[bk][:, 0:N], lhsT=Wt[:, k, ct * 128:(ct + 1) * 128], rhs=hxT[:, k, col0:col0 + N],
                                                      start=(k == 0), stop=(k == 7)) for k in range(8)],
                         reads=[b_W, b_hxT], writes=[bbufs[bk]])

            def rope(bk, bk2, dst, col0, dbuf):
                kb.op(act, lambda e: e.activation(out=qf, in_=banks[bk][:, 0:BS], func=AF.Copy), reads=[bbufs[bk]], writes=[b_qf])
                if stop_after == 2.31:
                    return
                kb.op(pe, lambda e: e.matmul(banks[bk2][:, 0:BS], lhsT=perm2[:], rhs=qf, start=True, stop=True), reads=[b_qf, consts], writes=[bbufs[bk2]])
                if stop_after == 2.32:
                    return
                kb.op(dve, lambda e: e.tensor_tensor(out=t1, in0=banks[bk][:, 0:BS], in1=cos2[:, col0:col0 + BS], op=ALU.mult), reads=[bbufs[bk], b_cs, b_qf], writes=[b_t1])
                if stop_after == 2.33:
                    return
                kb.op(dve, lambda e: e.tensor_tensor(out=t2, in0=banks[bk2][:, 0:BS], in1=sin2[:, col0:col0 + BS], op=ALU.mult), reads=[bbufs[bk2], b_cs], writes=[b_t2])
                if stop_after == 2.34:
                    return
                kb.op(pool, lambda e: e.tensor_tensor(out=dst, in0=t1, in1=t2, op=ALU.add), reads=[b_t1, b_t2], writes=[dbuf])

            n = 0
            for tb in range(NB):
                col0 = tb * BS
                for ct in range(npair):
                    bk = 4 + (n % 2); n += 1
                    proj_fm(Wq, ct, col0, BS, bk)
                    if kind == "A":
                        rope(bk, 6 + (n % 2), QT[:, ct, col0:col0 + BS], col0, b_QT)
                    else:
                        kb.op(act, lambda e, bk=bk, ct=ct, col0=col0: e.activation(out=QT[:, ct, col0:col0 + BS], in_=banks[bk][:, 0:BS], func=AF.Copy, scale=0.125),
                              reads=[bbufs[bk]], writes=[b_QT])
                for ct in range(nkt):
                    bk = 4 + (n % 2); n += 1
                    proj_fm(Wk, ct, col0, BS, bk)
                    if kind == "A":
                        rope(bk, 6 + (n % 2), KT[:, ct, col0:col0 + BS], col0, b_KT)
                    else:
                        kb.op(act, lambda e, bk=bk, ct=ct, col0=col0: e.activation(out=KT[:, ct, col0:col0 + BS], in_=banks[bk][:, 0:BS], func=AF.Copy),
                              reads=[bbufs[bk]], writes=[b_KT])
            for ct in range(nkt):
                bk = 4 + (n % 2); n += 1
                proj_fm(Wk, ct, S, CT, bk)
                kb.op(act, lambda e, bk=bk, ct=ct: e.activation(out=KT[:, ct, S:S + CT], in_=banks[bk][:, 0:CT], func=AF.Copy), reads=[bbufs[bk]], writes=[b_KT])
            for i in range(NTT):
                bk = 4 + (n % 2); n += 1
                kb.group(pe, [lambda e, k=k, bk=bk, i=i: e.matmul(banks[bk][:, 0:nv * 64], lhsT=hxT[:, k, i * 128:(i + 1) * 128], rhs=Wv[:, k, :],
                                                                  start=(k == 0), stop=(k == 7)) for k in range(8)],
                         reads=[b_W, b_hxT], writes=[bbufs[bk]])
                kb.op(dve, lambda e, bk=bk, i=i: e.tensor_copy(out=V[:, i, :, 0:64], in_=banks[bk][:, 0:nv * 64].rearrange("p (a c) -> p a c", a=nv)),
                      reads=[bbufs[bk]], writes=[b_V])

            dump("QT_%s%d" % (kind, hg), QT, BF16); dump("KT_%s%d" % (kind, hg), KT, BF16); dump("V_%s%d" % (kind, hg), V[:, :, :, 0:65], BF16)
            if stop_after == 2.5:
                kb.barrier()
                return
            SP = [(psA, bA), (psB, bB)]
            units = []

            def mk_unit(u, qt, quad, hq, pre):
                qs = slice(qt * 128, (qt + 1) * 128)
                ob, obuf = banks[4 + (u // 4) % 2], bbufs[4 + (u // 4) % 2]
                hi = quad * 4 + hq
                h = heads[hi]
                half = hi % 2
                ps_ = slice(half * 64, half * 64 + 64)
                pair = hi // 2
                spt, sbuf_ = SP[u % 2]
                pt = PT[u % 2]
                st8 = {}

                def scores():
                    if kind == "B" and pre:
                        dyn_slot = {}
                        for c, pi in bchunks[qt]:
                            if pi not in res_slot and pi not in dyn_slot:
                                dyn_slot[pi] = len(dyn_slot)
                                load_pat(pi, bdyn[:, dyn_slot[pi], :, :], b_bdyn)
                    if kind == "A":
                        kti, vi = h // 4, h // 4
                        chunks = []
                        if qt > 0:
                            chunks.append((qt - 1, mA[:, 0, :], consts))
                        chunks.append((qt, None, None))
                        if qt < NT - 1:
                            chunks.append((qt + 1, mA[:, 1, :], consts))
                    else:
                        kti, vi = hi // 2, hi
                        dyn_slot = {}
                        for c, pi in bchunks[qt]:
                            if pi not in res_slot and pi not in dyn_slot:
                                dyn_slot[pi] = len(dyn_slot)
                        chunks = []
                        for c, pi in bchunks[qt]:
                            if pi in res_slot:
                                chunks.append((c, bres[:, res_slot[pi], hi, :], b_bres))
                            else:
                                chunks.append((c, bdyn[:, dyn_slot[pi], hi, :], b_bdyn))
                    for c in range(NCT):
                        chunks.append((NT + c, None, None))
                    ncu = len(chunks)
                    assert ncu <= 7
                    st8["chunks"], st8["vi"], st8["ncu"] = chunks, vi, ncu
                    fns = []
                    rd = [b_QT, b_KT, consts]
                    for ci, (kt_, bias, bb) in enumerate(chunks):
                        o_ = spt[:, ci * 128:(ci + 1) * 128]
                        fns.append(lambda e, o_=o_, kt_=kt_, bias=bias, kti=kti: e.matmul(
                            o_, lhsT=KT[ps_, kti, kt_ * 128:(kt_ + 1) * 128], rhs=QT[ps_, pair, qs], start=True, stop=(bias is None)))
                        if bias is not None:
                            fns.append(lambda e, o_=o_, bias=bias: e.matmul(o_, lhsT=identb[:], rhs=bias, start=False, stop=True))
                            if bb not in rd:
                                rd.append(bb)
                    kb.group(pe, fns, reads=rd, writes=[sbuf_])

                def rest():
                    chunks, vi, ncu = st8["chunks"], st8["vi"], st8["ncu"]
                    kb.op(act, lambda e: e.activation(out=pt[:, 0:ncu * 128], in_=spt[:, 0:ncu * 128], func=AF.Exp, scale=(0.125 if kind == "A" else 1.0)),
                          reads=[sbuf_], writes=[b_PT[u % 2]])
                    kb.group(pe, [lambda e, ci=ci, kt_=kt_: e.matmul(
                        ob[:, hq * 80:hq * 80 + 65], lhsT=pt[:, ci * 128:(ci + 1) * 128], rhs=V[:, kt_, vi, 0:65], start=(ci == 0), stop=(ci == ncu - 1))
                        for ci, (kt_, _, _) in enumerate(chunks)],
                             reads=[b_PT[u % 2], b_V], writes=[obuf] if hq == 0 else [], )
                    if hq > 0:
                        obuf.w = [pe.last()]
                    if hq < 3:
                        return
                    yi = ((u + 1) // 4) % 2
                    ob3 = ob[:, 0:320].rearrange("p (a c) -> p a c", a=4)
                    dn = dnt[:, yi, :]
                    if kind == "A":
                        kb.op(dve, lambda e: e.tensor_tensor(out=dn.unsqueeze(2), in0=ob3[:, :, 64:65], in1=esink[:, quad * 4:quad * 4 + 4].unsqueeze(2), op=ALU.add),
                              reads=[obuf, consts], writes=[b_small])
                        kb.op(dve, lambda e: e.reciprocal(out=dn, in_=dn), reads=[b_small], writes=[b_small])
                    else:
                        kb.op(dve, lambda e: e.reciprocal(out=dn.unsqueeze(2), in_=ob3[:, :, 64:65]), reads=[obuf], writes=[b_small])
                    kb.op(dve, lambda e: e.tensor_tensor(out=ytile[yi], in0=ob3[:, :, 0:64], in1=dn.unsqueeze(2).to_broadcast([128, 4, 64]), op=ALU.mult),
                          reads=[obuf, b_small], writes=[b_yt[yi]])
                    tb_, tbuf = banks[6 + yi], bbufs[6 + yi]
                    tbv = tb_.bitcast(BF16)
                    ytf = ytile[yi].rearrange("p a c -> p (a c)")
                    kb.group(pe, [lambda e, pr=pr: e.transpose(tbv[:, pr * 128:(pr + 1) * 128], ytf[:, pr * 128:(pr + 1) * 128], identb[:]) for pr in range(2)],
                             reads=[b_yt[yi], consts], writes=[tbuf])
                    yTd = yT[0] if kind == "A" else yT[1]
                    p0 = quad * 2 if kind == "A" else hg * 2
                    kb.op(act, lambda e: e.activation(out=yTd[:, p0:p0 + 2, qs], in_=tbv[:, 0:256].rearrange("p (a t) -> p a t", a=2), func=AF.Copy),
                          reads=[tbuf], writes=[b_yT])
                return scores, rest

            u = 0
            for qt in range(NT):
                for quad in range(nh // 4):
                    for hq in range(4):
                        units.append(mk_unit(u, qt, quad, hq, pre=(quad == 0 and hq == 0)))
                        u += 1
            units[0][0]()
            for i in range(len(units)):
                if i + 1 < len(units):
                    units[i + 1][0]()
                units[i][1]()
            kb.barrier()

        nrow = NSLOT + 128
        kb.dma(sp, [lambda e, r=r: e.dma_start(out=xs_d[r:r + 128, :], in_=zerob[:]) for r in range(0, nrow, 128)],
               reads=[consts], writes=[xs_zero])
        kb.dma(sp, lambda e: e.dma_start(out=ys_d[NSLOT:NSLOT + 128, :], in_=zerof[:]), reads=[consts], writes=[ys_zero])
        attn_branch("A", 0)
        if stop_after in (3, 2.5, 2.6, 2.7, 2.1, 2.2, 2.3, 2.31, 2.32, 2.33, 2.34):
            kb.emit()
            return nc
        attn_branch("B", 0)
        attn_branch("B", 1)
        dump("yaT", yT[0], BF16); dump("ybT", yT[1], BF16)
        if stop_after == 4:
            kb.emit()
            return nc

        AB.reset(AB_base); AFr.reset(0)
        mT = AB.take(8 * S, [8, S])
        AB_3b = AB.off
        wg = [[AB.take(8 * 128, [8, 128]) for _ in range(2)] for _ in range(2)]
        wbr = [[AB.take(4 * 128, [4, 128]) for _ in range(2)] for _ in range(2)]
        sg = [[AB.take(BS) for _ in range(2)] for _ in range(2)]
        m1 = [AFr.take(BS) for _ in range(2)]; m2 = [AFr.take(BS) for _ in range(2)]
        b_wg = [B_("wg0"), B_("wg1")]; b_sg = [[B_("sg00"), B_("sg01")], [B_("sg10"), B_("sg11")]]
        b_mT = B_("mT"); b_m1 = [B_("m10"), B_("m11")]; b_m2 = [B_("m20"), B_("m21")]
        wba_v = wba_d.rearrange("(k p) n -> p k n", p=128); wbb_v = wbb_d.rearrange("(k p) n -> p k n", p=128)

        def load_ct(ct):
            s = ct % 2
            kb.dma(pool, [lambda e: e.dma_start(out=wg[s][0], in_=win_v[:, :, 2304 + ct * 128:2304 + (ct + 1) * 128]),
                          lambda e: e.dma_start(out=wg[s][1], in_=win_v[:, :, 3328 + ct * 128:3328 + (ct + 1) * 128]),
                          lambda e: e.dma_start(out=wbr[s][0], in_=wba_v[:, :, ct * 128:(ct + 1) * 128]),
                          lambda e: e.dma_start(out=wbr[s][1], in_=wbb_v[:, :, ct * 128:(ct + 1) * 128])],
                   writes=[b_wg[s]])

        load_ct(0)
        n3 = 0
        for ct in range(8):
            s = ct % 2
            if ct + 1 < 8:
                load_ct(ct + 1)
            for tb in range(NB):
                cs_ = slice(tb * BS, (tb + 1) * BS)
                u3 = n3 % 2; n3 += 1
                for ab in range(2):
                    bk = 4 * u3 + ab
                    kb.group(pe, [lambda e, k=k, s=s, ab=ab, bk=bk, cs_=cs_: e.matmul(banks[bk][:, 0:BS], lhsT=wg[s][ab][:, k, :], rhs=hxT[:, k, cs_], start=(k == 0), stop=(k == 7))
                                  for k in range(8)], reads=[b_wg[s], b_hxT], writes=[bbufs[bk]])
                    kb.op(act, lambda e, u3=u3, ab=ab, bk=bk: e.activation(out=sg[u3][ab], in_=banks[bk][:, 0:BS], func=AF.Sigmoid), reads=[bbufs[bk]], writes=[b_sg[u3][ab]])
                    bk2 = 4 * u3 + 2 + ab
                    kb.group(pe, [lambda e, k=k, s=s, ab=ab, bk2=bk2, cs_=cs_: e.matmul(banks[bk2][:, 0:BS], lhsT=wbr[s][ab][:, k, :], rhs=yT[ab][:, k, cs_], start=(k == 0), stop=(k == 3))
                                  for k in range(4)], reads=[b_wg[s]], writes=[bbufs[bk2]])
                    mm, bmm = (m1[u3], b_m1[u3]) if ab == 0 else (m2[u3], b_m2[u3])
                    kb.op(dve, lambda e, u3=u3, ab=ab, bk2=bk2, mm=mm: e.tensor_tensor(out=mm, in0=banks[bk2][:, 0:BS], in1=sg[u3][ab], op=ALU.mult),
                          reads=[bbufs[bk2], b_sg[u3][ab]], writes=[bmm])
                kb.op(dve, lambda e, ct=ct, cs_=cs_, u3=u3: e.tensor_tensor(out=mT[:, ct, cs_], in0=m1[u3], in1=m2[u3], op=ALU.add), reads=[b_m1[u3], b_m2[u3]], writes=[b_mT])
        kb.barrier()

        AB.reset(AB_3b); AFr.reset(0)
        Wout = AB.take(8 * D, [8, D])
        hx2b = [AB.take(D) for _ in range(2)]
        Mb = AB.take(E)
        xt3 = [AFr.take(D) for _ in range(2)]
        x1t = [AFr.take(D) for _ in range(2)]
        t3 = AFr.take(D); hx2f = [AFr.take(D) for _ in range(2)]
        hx2T = AFr.take(8 * 128, [8, 128])
        lg = AFr.take(E); mx8 = AFr.take(8); ew = AFr.take(4); dfull = AFr.take(E); spos = AFr.take(E); pen = AFr.take(E)
        junkE = AFr.take(E); destf = AFr.take(4); negm = AFr.take(1); sumw = AFr.take(1)
        b_Wout = B_("Wout")
        b_xt3 = [B_("xt30"), B_("xt31")]; b_x1t = [B_("x1t0"), B_("x1t1")]; b_t3 = B_("t3"); b_hx2f = [B_("hx2f0"), B_("hx2f1")]
        b_hx2b = [B_("hx2b0"), B_("hx2b1")]; b_hx2T = B_("hx2T"); b_r = B_("route"); b_Mb = B_("Mb"); b_cb = B_("cb")
        b_route_out = B_("routeout"); b_outd = Buf("outd", nowaw=True)
        kb.dma(pool, lambda e: e.dma_start(out=Wout, in_=wout_d.rearrange("(k p) n -> p k n", p=128)), writes=[b_Wout])

        def stage1(ti):
            s = ti % 2
            js = slice(ti * 128, (ti + 1) * 128)
            rows = js
            kb.dma(sp, lambda e: e.dma_start(out=xt3[s], in_=x_d[rows, :]), writes=[b_xt3[s]])
            for half in range(2):
                kb.group(pe, [lambda e, k=k, half=half: e.matmul(psC[:, half * 512:(half + 1) * 512], lhsT=mT[:, k, js], rhs=Wout[:, k, half * 512:(half + 1) * 512],
                                                                 start=(k == 0), stop=(k == 7)) for k in range(8)],
                         reads=[b_mT, b_Wout], writes=[bC] if half == 0 else [])
            bC.w = [pe.last()]
            r = rms_rstd(psC[:, :], [bC])
            kb.op(dve, lambda e: e.scalar_tensor_tensor(out=t3, in0=psC[:, :], scalar=r, in1=P1g[:], op0=ALU.mult, op1=ALU.mult), reads=[bC, b_small], writes=[b_t3])
            kb.op(pool, lambda e: e.tensor_tensor(out=x1t[s], in0=t3, in1=xt3[s], op=ALU.add), reads=[b_t3, b_xt3[s]], writes=[b_x1t[s]])
            kb.dma(sp, lambda e: e.dma_start(out=out_d[rows, :], in_=x1t[s]), reads=[b_x1t[s]], writes=[b_outd], semof=b_x1t[s])
            r2 = rms_rstd(x1t[s], [b_x1t[s]])
            kb.op(dve, lambda e: e.scalar_tensor_tensor(out=t3, in0=x1t[s], scalar=r2, in1=G2[:], op0=ALU.mult, op1=ALU.mult), reads=[b_x1t[s], b_small], writes=[b_t3])
            kb.op(pool, lambda e: e.tensor_tensor(out=hx2f[s], in0=t3, in1=sh2[:], op=ALU.add), reads=[b_t3], writes=[b_hx2f[s]])
            kb.op(act, lambda e: e.activation(out=hx2b[s], in_=hx2f[s], func=AF.Copy), reads=[b_hx2f[s]], writes=[b_hx2b[s]])

        def stage2(ti):
            s = ti % 2
            kb.group(pe, [lambda e, k=k: e.transpose(psD[:, k * 128:(k + 1) * 128], hx2f[s][:, k * 128:(k + 1) * 128], identf[:]) for k in range(8)],
                     reads=[b_hx2f[s], consts], writes=[bD])
            kb.op(dve, lambda e: e.tensor_copy(out=hx2T, in_=psD[:, :].rearrange("p (k t) -> p k t", k=8)), reads=[bD], writes=[b_hx2T])
            kb.group(pe, [lambda e, k=k: e.matmul(banks[0][:, 0:E], lhsT=hx2T[:, k, :], rhs=wr_sb[:, k, :], start=(k == 0), stop=(k == 7)) for k in range(8)],
                     reads=[b_hx2T, consts], writes=[bbufs[0]])
            R = [b_r]
            kb.op(dve, lambda e: e.tensor_tensor(out=lg, in0=banks[0][:, 0:E], in1=brep[:], op=ALU.add), reads=[bbufs[0], consts], writes=R)
            kb.op(dve, lambda e: e.max(out=mx8, in_=lg), reads=R, writes=R)
            kb.op(dve, lambda e: e.tensor_scalar(out=negm, in0=mx8[:, 0:1], scalar1=-1.0, scalar2=None, op0=ALU.mult), reads=R, writes=R)
            kb.op(act, lambda e: e.activation(out=ew, in_=mx8[:, 0:4], func=AF.Exp, bias=negm, scale=1.0, accum_out=sumw), reads=R, writes=R)
            kb.op(dve, lambda e: e.reciprocal(out=sumw, in_=sumw), reads=R, writes=R)
            kb.op(dve, lambda e: e.tensor_scalar(out=wts[:, ti, :], in0=ew, scalar1=sumw, scalar2=None, op0=ALU.mult), reads=R, writes=[b_route_out])
            kb.op(dve, lambda e: e.tensor_scalar(out=Mb, in0=lg, scalar1=mx8[:, 3:4], scalar2=None, op0=ALU.is_ge), reads=R, writes=[b_Mb])
            kb.op(pe, lambda e: e.matmul(banks[1][:, 0:E], lhsT=tri[:], rhs=Mb, start=True, stop=True), reads=[b_Mb, consts], writes=[bbufs[1]])
            kb.op(pe, lambda e: e.matmul(banks[2][:, 0:E], lhsT=onesb[:], rhs=Mb, start=True, stop=True), reads=[b_Mb, consts], writes=[bbufs[2]])
            kb.op(dve, lambda e: e.tensor_tensor(out=spos, in0=banks[1][:, 0:E], in1=cb[:], op=ALU.add), reads=[bbufs[1], b_cb], writes=R)
            kb.op(dve, lambda e: e.tensor_tensor(out=cb[:], in0=banks[2][:, 0:E], in1=cb[:], op=ALU.add), reads=[bbufs[2], b_r], writes=[b_cb])
            kb.op(dve, lambda e: e.tensor_scalar(out=pen, in0=spos, scalar1=float(CAP), scalar2=1.0e6, op0=ALU.is_ge, op1=ALU.mult), reads=R, writes=R)
            kb.op(dve, lambda e: e.tensor_tensor(out=dfull, in0=spos, in1=baseE[:], op=ALU.add), reads=R + [consts], writes=R)
            kb.op(dve, lambda e: e.tensor_tensor(out=dfull, in0=dfull, in1=pen, op=ALU.add), reads=R, writes=R)
            for k in range(4):
                kb.op(dve, lambda e, k=k: e.scalar_tensor_tensor(out=junkE, in0=lg, scalar=mx8[:, k:k + 1], in1=dfull, op0=ALU.is_equal, op1=ALU.mult,
                                                                 accum_out=destf[:, k:k + 1]), reads=R, writes=R)
            kb.op(dve, lambda e: e.tensor_scalar(out=desti[:, ti, :], in0=destf, scalar1=float(NSLOT), scalar2=None, op0=ALU.min), reads=R, writes=[b_route_out])
            kb.dma(pool, [lambda e, k=k: e.indirect_dma_start(out=xs_d[:, :], out_offset=bass.IndirectOffsetOnAxis(ap=desti[:, ti, k:k + 1], axis=0),
                                                             in_=hx2b[s], in_offset=None)
                          for k in range(4)], reads=[b_hx2b[s], b_route_out, xs_zero], writes=[xsb], semof=b_hx2b[s])

        stage1(0)
        for ti in range(NT):
            if ti + 1 < NT:
                stage1(ti + 1)
            stage2(ti)
        kb.barrier()
        dump("wts", wts[:], F32); dump("desti", desti[:], I32)
        if stop_after == 5:
            kb.emit()
            return nc

        AB.reset(0); AFr.reset(0)
        Wgu = [AB.take(8 * 2 * D, [8, 2 * D]) for _ in range(2)]
        Wd = [AB.take(8 * D, [8, D]) for _ in range(2)]
        XT = [AB.take(8 * CAP, [8, CAP]) for _ in range(2)]
        xrows = AB.take(NJ * D, [NJ, D])
        hT = AB.take(8 * CAP, [8, CAP])
        gt = [AFr.take(CAP) for _ in range(2)]; sgm = [AFr.take(CAP) for _ in range(2)]
        ua = [AFr.take(CAP) for _ in range(2)]; glu = [AFr.take(CAP) for _ in range(2)]
        ysb = [AFr.take(512) for _ in range(4)]
        bdr = [AFr.take(D) for _ in range(2)]
        b_Wgu = [B_("Wgu0"), B_("Wgu1")]; b_Wd = [B_("Wd0"), B_("Wd1")]; b_XT = [B_("XT0"), B_("XT1")]; b_xr, b_hT = B_("xrows"), B_("hT")
        b_gt = [B_("gt0"), B_("gt1")]; b_sgm = [B_("sgm0"), B_("sgm1")]; b_ua = [B_("ua0"), B_("ua1")]; b_glu = [B_("glu0"), B_("glu1")]
        b_ysb = [B_("ysb%d" % i) for i in range(4)]; b_bdr = [B_("bdr0"), B_("bdr1")]

        def load_w(ex):
            s = ex % 2
            wgv = wgu_d[ex].rearrange("(k p) n -> p k n", p=128)
            wdv = wd_d[ex].rearrange("(k p) n -> p k n", p=128)
            kb.dma(pool, [lambda e, q4=q4: e.dma_start(out=Wgu[s][:, 2 * q4:2 * q4 + 2, :], in_=wgv[:, 2 * q4:2 * q4 + 2, :]) for q4 in range(4)],
                   writes=[b_Wgu[s]])
            kb.dma(pool, [lambda e, q2=q2: e.dma_start(out=Wd[s][:, 4 * q2:4 * q2 + 4, :], in_=wdv[:, 4 * q2:4 * q2 + 4, :]) for q2 in range(2)],
                   writes=[b_Wd[s]])
            kb.dma(sp, lambda e: e.dma_start(out=bdr[s], in_=bd_d[ex:ex + 1, :].partition_broadcast(128)), writes=[b_bdr[s]])

        def prep(ex):
            s = ex % 2
            kb.dma(sp, lambda e: e.dma_start(out=xrows, in_=xs_d[ex * CAP:(ex + 1) * CAP, :].rearrange("(j p) d -> p j d", p=128)), reads=[xsb], writes=[b_xr])
            for j in range(NJ):
                bk = j % 2
                pbv = banks[bk].bitcast(BF16)
                kb.group(pe, [lambda e, k=k, j=j, pbv=pbv: e.transpose(pbv[:, k * 128:(k + 1) * 128], xrows[:, j, k * 128:(k + 1) * 128], identb[:]) for k in range(8)],
                         reads=[b_xr, consts], writes=[bbufs[bk]])
                kb.op(act, lambda e, j=j, pbv=pbv: e.activation(out=XT[s][:, :, j * 128:(j + 1) * 128], in_=pbv.rearrange("p (k t) -> p k t", k=8), func=AF.Copy),
                      reads=[bbufs[bk]], writes=[b_XT[s]])

        load_w(0)
        prep(0)
        ny = 0
        for ex in range(E):
            s = ex % 2
            if ex + 1 < E:
                load_w(ex + 1)
            for f in range(8):
                gb_, ub_ = (0, 1) if f % 2 == 0 else (2, 3)
                fs = f % 2
                kb.group(pe, [lambda e, k=k, f=f, s=s, gb_=gb_: e.matmul(banks[gb_][:, 0:CAP], lhsT=Wgu[s][:, k, f * 128:(f + 1) * 128], rhs=XT[s][:, k, :], start=(k == 0), stop=(k == 7))
                              for k in range(8)], reads=[b_Wgu[s], b_XT[s]], writes=[bbufs[gb_]])
                kb.group(pe, [lambda e, k=k, f=f, s=s, ub_=ub_: e.matmul(banks[ub_][:, 0:CAP], lhsT=Wgu[s][:, k, D + f * 128:D + (f + 1) * 128], rhs=XT[s][:, k, :], start=(k == 0), stop=(k == 7))
                              for k in range(8)], reads=[b_Wgu[s], b_XT[s]], writes=[bbufs[ub_]])
                kb.op(dve, lambda e, f=f, ex=ex, gb_=gb_, fs=fs: e.tensor_scalar(out=gt[fs], in0=banks[gb_][:, 0:CAP], scalar1=bguT[:, f, ex:ex + 1], scalar2=7.0, op0=ALU.add, op1=ALU.min),
                      reads=[bbufs[gb_], consts], writes=[b_gt[fs]])
                kb.op(act, lambda e, fs=fs: e.activation(out=sgm[fs], in_=gt[fs], func=AF.Sigmoid, scale=1.702), reads=[b_gt[fs]], writes=[b_sgm[fs]])
                kb.op(dve, lambda e, f=f, ex=ex, ub_=ub_, fs=fs: e.tensor_scalar(out=ua[fs], in0=banks[ub_][:, 0:CAP], scalar1=bguT[:, 8 + f, ex:ex + 1], scalar2=8.0, op0=ALU.add, op1=ALU.min),
                      reads=[bbufs[ub_], consts], writes=[b_ua[fs]])
                kb.op(pool, lambda e, fs=fs: e.tensor_tensor(out=glu[fs], in0=gt[fs], in1=sgm[fs], op=ALU.mult), reads=[b_gt[fs], b_sgm[fs]], writes=[b_glu[fs]])
                kb.op(dve, lambda e, f=f, fs=fs: e.scalar_tensor_tensor(out=hT[:, f, :], in0=ua[fs], scalar=-6.0, in1=glu[fs], op0=ALU.max, op1=ALU.mult), reads=[b_ua[fs], b_glu[fs]], writes=[b_hT])
            if ex + 1 < E:
                prep(ex + 1)
            for j in range(NJ):
                js = slice(j * 128, (j + 1) * 128)
                r0 = ex * CAP + j * 128
                for half in range(2):
                    yb_ = ny % 4; ny += 1
                    bk = 4 + yb_
                    kb.group(pe, [lambda e, k=k, half=half, s=s, js=js, bk=bk: e.matmul(banks[bk], lhsT=hT[:, k, js], rhs=Wd[s][:, k, half * 512:(half + 1) * 512],
                                                                                      start=(k == 0), stop=(k == 7)) for k in range(8)],
                             reads=[b_hT, b_Wd[s]], writes=[bbufs[bk]])
                    kb.op(dve, lambda e, yb_=yb_, s=s, bk=bk, half=half: e.tensor_tensor(out=ysb[yb_], in0=banks[bk], in1=bdr[s][:, half * 512:(half + 1) * 512], op=ALU.add),
                          reads=[bbufs[bk], b_bdr[s]], writes=[b_ysb[yb_]])
                    kb.dma(sp, lambda e, yb_=yb_, r0=r0, half=half: e.dma_start(out=ys_d[r0:r0 + 128, half * 512:(half + 1) * 512], in_=ysb[yb_]), reads=[b_ysb[yb_]], writes=[ysb_b], semof=b_ysb[yb_])
        kb.barrier()
        if stop_after == 6:
            kb.emit()
            return nc

        AFr.reset(0); AB.reset(0)
        Yk = [[AB.take(2 * D).bitcast(F32) for _ in range(4)] for _ in range(2)]
        x1r = [AFr.take(D) for _ in range(2)]; acc = AFr.take(D); ot = [AFr.take(D) for _ in range(2)]
        b_Yk = [[B_("Yk%d%d" % (st_, k)) for k in range(4)] for st_ in range(2)]
        b_x1r = [B_("x1r0"), B_("x1r1")]; b_acc = B_("acc"); b_ot = [B_("ot0"), B_("ot1")]
        b_fin = Buf("fin", nowaw=True)

        def gath(ti):
            st_ = ti % 2
            rows = slice(ti * 128, (ti + 1) * 128)
            for k in range(4):
                kb.dma(pool, lambda e, k=k: e.indirect_dma_start(out=Yk[st_][k], out_offset=None, in_=ys_d[:, :],
                                                                 in_offset=bass.IndirectOffsetOnAxis(ap=desti[:, ti, k:k + 1], axis=0)),
                       reads=[ysb_b, ys_zero, b_route_out], writes=[b_Yk[st_][k]])
            kb.dma(sp, lambda e: e.dma_start(out=x1r[st_], in_=out_d[rows, :]), reads=[b_outd], writes=[b_x1r[st_]])

        gath(0)
        for ti in range(NT):
            st_ = ti % 2
            rows = slice(ti * 128, (ti + 1) * 128)
            if ti + 1 < NT:
                gath(ti + 1)
            kb.op(dve, lambda e, ti=ti, st_=st_: e.tensor_scalar(out=acc, in0=Yk[st_][0], scalar1=wts[:, ti, 0:1], scalar2=None, op0=ALU.mult), reads=[b_Yk[st_][0], b_route_out], writes=[b_acc])
            for k in range(1, 4):
                kb.op(dve, lambda e, ti=ti, k=k, st_=st_: e.scalar_tensor_tensor(out=acc, in0=Yk[st_][k], scalar=wts[:, ti, k:k + 1], in1=acc, op0=ALU.mult, op1=ALU.add),
                      reads=[b_Yk[st_][k], b_route_out, b_acc], writes=[b_acc])
            r = rms_rstd(acc, [b_acc])
            kb.op(dve, lambda e, r=r: e.scalar_tensor_tensor(out=acc, in0=acc, scalar=r, in1=P2g[:], op0=ALU.mult, op1=ALU.mult), reads=[b_acc, b_small], writes=[b_acc])
            kb.op(dve, lambda e, st_=st_: e.tensor_tensor(out=ot[st_], in0=acc, in1=x1r[st_], op=ALU.add), reads=[b_acc, b_x1r[st_]], writes=[b_ot[st_]])
            kb.dma(sp, lambda e, rows=rows, st_=st_: e.dma_start(out=out_d[rows, :], in_=ot[st_]), reads=[b_ot[st_], b_x1r[st_]], writes=[b_fin], semof=b_ot[st_])
        kb.barrier()
        kb.emit()
    return nc


def host_inputs(cfg, inp, b):
    S, CT, E, CAP = cfg["S"], cfg["CT"], cfg["E"], cfg["CAP"]
    f = np.float32
    c = np.asarray(inp["c"][b], f); cc = np.asarray(inp["c_ctx"], f)
    cT = np.concatenate([c.reshape(8, 128).T, cc.reshape(8, 128).T], axis=1)
    pats, _ = nbr_patterns(S)
    rpb = np.asarray(inp["b_rpb"][0], f)
    rpbx = np.stack([rpb[:, dr, dc].transpose(1, 0, 2) for ok, dr, dc in pats]).astype(f)
    maskb = np.stack([np.where(ok, 0.0, NEG) for ok, dr, dc in pats]).astype(f)
    cos2, sin2, perm2 = rope_tables(S)
    kj = np.arange(128)[:, None]; qi = np.arange(128)[None, :]
    maskA = np.stack([np.where(qi <= kj, 0.0, NEG), np.where(kj <= qi, 0.0, NEG)]).astype(ml_dtypes.bfloat16)
    tri = (kj < qi).astype(ml_dtypes.bfloat16)
    return {
        "x": np.ascontiguousarray(inp["x"][b], f), "ctx": np.ascontiguousarray(inp["ctx"][b], f), "cT": np.ascontiguousarray(cT, f),
        "w_mod": np.asarray(inp["w_mod"][0], f), "b_mod": np.asarray(inp["b_mod"], f).reshape(1, -1),
        "g_pre_mix": np.asarray(inp["g_pre_mix"], f).reshape(1, -1), "g_post_mix": np.asarray(inp["g_post_mix"], f).reshape(1, -1),
        "g_pre_ffn": np.asarray(inp["g_pre_ffn"], f).reshape(1, -1), "g_post_ffn": np.asarray(inp["g_post_ffn"], f).reshape(1, -1),
        "w_in": np.asarray(inp["w_in"][0], f), "a_sink": np.asarray(inp["a_sink"], f).reshape(1, 8),
        "rpbx": rpbx, "maskb": maskb,
        "w_branch_a": np.asarray(inp["w_branch_a"][0], f), "w_branch_b": np.asarray(inp["w_branch_b"][0], f), "w_out": np.asarray(inp["w_out"][0], f),
        "w_router": np.asarray(inp["w_router"][0], f), "b_router": np.asarray(inp["b_router"], f).reshape(1, -1),
        "w_gate_up": np.asarray(inp["w_gate_up"][0], f), "b_gate_up": np.asarray(inp["b_gate_up"][0], f),
        "w_down": np.asarray(inp["w_down"][0], f), "b_down": np.asarray(inp["b_down"][0], f),
        "identf": np.eye(128, dtype=f), "tri": tri, "identb": np.eye(128).astype(ml_dtypes.bfloat16),
        "maskA": maskA, "perm2": perm2.astype(ml_dtypes.bfloat16), "cos2": cos2, "sin2": sin2,
        "baseE": np.tile((np.arange(E, dtype=f) * CAP)[None, :], (128, 1)).astype(f),
    }


def kernel(**inputs):
    cfg = FULL
    nb = inputs["x"].shape[0]
    nc = build(cfg)
    in_maps = [host_inputs(cfg, inputs, b) for b in range(nb)]
    res = run_bass_kernel_spmd(nc, in_maps, core_ids=list(range(nb)))
    return np.stack([np.asarray(r["out"], np.float32) for r in res.results], axis=0)
```

```python
import numpy as np
import ml_dtypes
from contextlib import ExitStack
import concourse.bass as bass
import concourse.mybir as mybir
from concourse.bass_utils import run_bass_kernel_spmd

F32 = mybir.dt.float32
BF16 = mybir.dt.bfloat16
I32 = mybir.dt.int32
ALU = mybir.AluOpType
AF = mybir.ActivationFunctionType

D = 1024
GRID_W = 64
HD = 64
TOPK = 4
NEG = -30000.0
EPS = 1e-6
FULL = dict(S=2048, CT=256, E=32, CAP=512)


class Q:
    def __init__(self, kb, name):
        self.name = name
        self.ops = []
        self.sem = kb.new_sem("q_" + name)
        self.count = 0
        self.seen = {}

    def wait(self, *tickets):
        for t in tickets:
            if t is None:
                continue
            sem, val = t
            key = id(sem)
            if self.seen.get(key, 0) >= val:
                continue
            self.seen[key] = val
            self.ops.append(lambda e, sem=sem, val=val: e.wait_ge(sem, val))

    def last(self):
        return (self.sem, self.count) if self.count else None


class Buf:
    def __init__(self, name, nowaw=False):
        self.name = name
        self.w = []
        self.r = []
        self.ds = None
        self.nowaw = nowaw


class KB:
    def __init__(self, nc, stack):
        self.nc = nc
        self.stack = stack
        self.dsems = []
        self.pe, self.act, self.dve, self.pool, self.sp = (Q(self, n) for n in ("pe", "act", "dve", "pool", "sp"))
        self.qs = [self.pe, self.act, self.dve, self.pool, self.sp]

    def new_sem(self, name):
        self.nsem = getattr(self, "nsem", 0) + 1
        return self.stack.enter_context(self.nc.semaphore("%s_%d" % (name, self.nsem)))

    def sb(self, name, shape, dt):
        return self.stack.enter_context(self.nc.sbuf_tensor("s_" + name, shape, dt))

    def ps(self, name, shape, dt):
        return self.stack.enter_context(self.nc.psum_tensor(name, shape, dt))

    def _pre(self, q, reads, writes):
        for b in reads:
            q.wait(*b.w)
        for b in writes:
            if not b.nowaw:
                q.wait(*b.w)
            q.wait(*b.r)

    def _post(self, t, reads, writes):
        for b in reads:
            b.r.append(t)
        for b in writes:
            if b.nowaw:
                b.w = [x for x in b.w if x[0] is not t[0]] + [t]
            else:
                b.w = [t]
            b.r = []

    def op(self, q, fn, reads=(), writes=()):
        self._pre(q, reads, writes)
        q.count += 1
        sem = q.sem
        q.ops.append(lambda e, fn=fn, sem=sem: fn(e).then_inc(sem, 1))
        t = (sem, q.count)
        self._post(t, reads, writes)
        return t

    def group(self, q, fns, reads=(), writes=()):
        self._pre(q, reads, writes)
        for fn in fns[:-1]:
            q.ops.append(lambda e, fn=fn: fn(e))
        q.count += 1
        sem = q.sem
        fn = fns[-1]
        q.ops.append(lambda e, fn=fn, sem=sem: fn(e).then_inc(sem, 1))
        t = (sem, q.count)
        self._post(t, reads, writes)
        return t

    def dma(self, q, fns, reads=(), writes=(), semof=None):
        if not isinstance(fns, (list, tuple)):
            fns = [fns]
        self._pre(q, reads, writes)
        b = semof if semof is not None else writes[0]
        if b.ds is None:
            b.ds = {}
        if q.name not in b.ds:
            b.ds[q.name] = [self.new_sem("d_" + b.name), 0]
            self.dsems.append(b.ds[q.name])
        d = b.ds[q.name]
        for fn in fns:
            d[1] += 16
            s = d[0]
            q.ops.append(lambda e, fn=fn, s=s: fn(e).then_inc(s, 16))
        t = (d[0], d[1])
        self._post(t, reads, writes)
        return t

    def barrier(self):
        ts = [q.last() for q in self.qs] + [(d[0], d[1]) for d in self.dsems if d[1]]
        for q in self.qs:
            q.wait(*ts)

    def emit(self):
        with self.nc.Block() as block:
            @block.tensor
            def _(e):
                for f in self.pe.ops:
                    f(e)

            @block.scalar
            def _(e):
                for f in self.act.ops:
                    f(e)

            @block.vector
            def _(e):
                for f in self.dve.ops:
                    f(e)

            @block.gpsimd
            def _(e):
                for f in self.pool.ops:
                    f(e)

            @block.sync
            def _(e):
                for f in self.sp.ops:
                    f(e)


class Arena:
    def __init__(self, t, n, dt):
        self.t, self.n, self.dt, self.off = t, n, dt, 0

    def reset(self, off=0):
        self.off = off

    def take(self, n, shape=None):
        assert self.off + n <= self.n, ("arena overflow", self.dt, self.off, n, self.n)
        v = self.t[:, self.off:self.off + n]
        self.off += (n + 31) // 32 * 32
        if shape is not None:
            names = " ".join("d%d" % i for i in range(len(shape)))
            v = v.rearrange("p (%s) -> p %s" % (names, names), **{"d%d" % i: s for i, s in enumerate(shape)})
        return v


def rope_tables(S):
    t = np.arange(S, dtype=np.int32)
    row = (t // GRID_W).astype(np.float32)
    col = (t % GRID_W).astype(np.float32)
    nf = HD // 4
    inv = (np.float32(10000.0) ** (-np.arange(nf, dtype=np.float32) / np.float32(nf))).astype(np.float32)
    ang = np.concatenate([row[:, None] * inv, col[:, None] * inv], axis=-1).astype(np.float32)
    cos, sin = np.cos(ang).astype(np.float32), np.sin(ang).astype(np.float32)
    cosT = np.zeros((HD, S), np.float32)
    sinT = np.zeros((HD, S), np.float32)
    perm = np.zeros((HD, HD), np.float32)
    for d in range(HD):
        axis, half, f = d // 32, (d % 32) // 16, d % 16
        cosT[d] = cos[:, axis * nf + f]
        if half == 0:
            sinT[d] = -sin[:, axis * nf + f]
            perm[d + 16, d] = 1.0
        else:
            sinT[d] = sin[:, axis * nf + f]
            perm[d - 16, d] = 1.0
    cos2 = np.concatenate([cosT, cosT], 0)
    sin2 = np.concatenate([sinT, sinT], 0)
    perm2 = np.zeros((128, 128), np.float32)
    perm2[:64, :64] = perm
    perm2[64:, 64:] = perm
    return cos2, sin2, perm2


def nbr_patterns(S):
    rows = S // GRID_W
    kr = min(8, rows)
    kc = 16
    NT = S // 128
    pats, pat_idx, chunks = [], {}, []
    qi = np.arange(128)
    kj = np.arange(128)
    qcol = (qi % 64)[None, :]
    kcol = (kj % 64)[:, None]
    col_start = np.clip(qcol - kc // 2, 0, GRID_W - kc)
    col_ok = (kcol >= col_start) & (kcol < col_start + kc)
    dc = np.clip(kcol - qcol, -(kc - 1), kc - 1) + 15
    for p in range(NT):
        lst = []
        for c in range(NT):
            qrow = (2 * p + qi // 64)[None, :]
            krow = (2 * c + kj // 64)[:, None]
            rs = np.clip(qrow - kr // 2, 0, rows - kr)
            ok = (krow >= rs) & (krow < rs + kr) & col_ok
            if not ok.any():
                continue
            dr = np.where(ok, krow - qrow + 7, 0)
            dcc = np.where(ok, dc, 0)
            key = (ok.tobytes(), dr.astype(np.int16).tobytes(), dcc.astype(np.int16).tobytes())
            if key not in pat_idx:
                pat_idx[key] = len(pats)
                pats.append((ok, dr, dcc))
            lst.append((c, pat_idx[key]))
        chunks.append(lst)
    return pats, chunks


def build(cfg, stop_after=99, debug=False):
    S, CT, E, CAP = cfg["S"], cfg["CT"], cfg["E"], cfg["CAP"]
    NT, NCT = S // 128, CT // 128
    NTT = NT + NCT
    BS = min(512, S)
    NB = S // BS
    NSLOT = E * CAP
    NJ = CAP // 128
    pats, bchunks = nbr_patterns(S)
    NPAT = len(pats)
    use_cnt = np.zeros(NPAT, int)
    for lst in bchunks:
        for _, pi in lst:
            use_cnt[pi] += 1
    res_p = list(np.argsort(-use_cnt)[:5])
    res_slot = {int(p): i for i, p in enumerate(res_p)}

    nc = bass.Bass("TRN2", target_bir_lowering=False)

    def din(name, shape, dt=F32):
        return nc.dram_tensor(name, list(shape), dt, kind="ExternalInput").ap()

    x_d = din("x", [S, D]); ctx_d = din("ctx", [CT, D]); cT_d = din("cT", [128, 16])
    wmod_d = din("w_mod", [D, 6 * D]); bmod_d = din("b_mod", [1, 6 * D])
    g_d = [din(n, [1, D]) for n in ("g_pre_mix", "g_post_mix", "g_pre_ffn", "g_post_ffn")]
    win_d = din("w_in", [D, 4352]); sink_d = din("a_sink", [1, 8])
    rpbx_d = din("rpbx", [NPAT, 128, 8, 128]); maskb_d = din("maskb", [NPAT, 128, 128])
    wba_d = din("w_branch_a", [512, D]); wbb_d = din("w_branch_b", [512, D]); wout_d = din("w_out", [D, D])
    wr_d = din("w_router", [D, E]); br_d = din("b_router", [1, E])
    wgu_d = din("w_gate_up", [E, D, 2 * D]); bgu_d = din("b_gate_up", [E, 2 * D])
    wd_d = din("w_down", [E, D, D]); bd_d = din("b_down", [E, D])
    identf_d = din("identf", [128, 128]); tri_d = din("tri", [128, 128], BF16); identb_d = din("identb", [128, 128], BF16)
    mA_d = din("maskA", [2, 128, 128], BF16); perm_d = din("perm2", [128, 128], BF16)
    cos_d = din("cos2", [128, S]); sin_d = din("sin2", [128, S]); base_d = din("baseE", [128, E])
    out_d = nc.dram_tensor("out", [S, D], F32, kind="ExternalOutput").ap()
    xs_d = nc.dram_tensor("xs", [NSLOT + 128, D], BF16, kind="Internal").ap()
    ys_d = nc.dram_tensor("ys", [NSLOT + 128, D], F32, kind="Internal").ap()
    win_v = win_d.rearrange("(k p) n -> p k n", p=128)
    wmod_v = wmod_d.rearrange("(k p) n -> p k n", p=128)

    st = ExitStack()
    with st:
        kb = KB(nc, st)
        pe, act, dve, pool, sp = kb.pe, kb.act, kb.dve, kb.pool, kb.sp

        def dump(name, ap, dt):
            if not debug:
                return
            kb.barrier()
            shp = list(ap.shape)
            flat = [shp[0], int(np.prod(shp[1:]))]
            dd = nc.dram_tensor("dbg_" + name, flat, dt, kind="ExternalOutput").ap()
            src = ap
            if len(shp) == 3:
                dd = dd.rearrange("p (a b) -> p a b", a=shp[1])
            elif len(shp) == 4:
                dd = dd.rearrange("p (a b c) -> p a b c", a=shp[1], b=shp[2])
            kb.dma(sp, lambda e: e.dma_start(out=dd, in_=src), writes=[Buf("dbg_" + name)])
            kb.barrier()
        NBF = 66 * 1024
        NF = 10 * 1024
        AB = Arena(kb.sb("arenaB", [128, NBF], BF16), NBF, BF16)
        AFr = Arena(kb.sb("arenaF", [128, NF], F32), NF, F32)
        P1g = kb.sb("P1g", [128, D], F32); G2 = kb.sb("G2", [128, D], F32)
        sh2 = kb.sb("sh2", [128, D], F32); P2g = kb.sb("P2g", [128, D], F32)
        identb = kb.sb("identb", [128, 128], BF16); identf = kb.sb("identf", [128, 128], F32)
        tri = kb.sb("tri", [128, 128], BF16); onesb = kb.sb("onesb", [128, 128], BF16)
        mA = kb.sb("mA", [128, 2, 128], BF16); perm2 = kb.sb("perm2", [128, 128], BF16)
        esink = kb.sb("esink", [128, 8], F32)
        small = kb.sb("small", [128, 64 + 4 * NTT], F32)
        wts = kb.sb("wts", [128, NT, 4], F32); desti = kb.sb("desti", [128, NT, 4], I32)
        bguT = kb.sb("bguT", [128, 16, E], F32)
        baseE = kb.sb("baseE", [128, E], F32); cb = kb.sb("cb", [128, E], F32); brep = kb.sb("brep", [128, E], F32)
        wr_sb = kb.sb("wr", [128, 8, E], F32)
        junkb = kb.sb("junkb", [128, D], BF16)
        dnt = kb.sb("dnt", [128, 2, 4], F32)
        zerob = kb.sb("zerob", [128, D], BF16); zerof = kb.sb("zerof", [128, D], F32)
        psA = kb.ps("psA", [128, 1024], F32); psB = kb.ps("psB", [128, 1024], F32)
        psC = kb.ps("psC", [128, 1024], F32); psD = kb.ps("psD", [128, 1024], F32)
        bA, bB, bC, bD = Buf("psA"), Buf("psB"), Buf("psC"), Buf("psD")
        banks = []
        bbufs = [Buf("bank%d" % i) for i in range(8)]
        for i, t in enumerate((psA, psB, psC, psD)):
            banks.append(t[:, 0:512]); banks.append(t[:, 512:1024])
        ssq_i = [0]

        def col():
            c = ssq_i[0] % 64
            ssq_i[0] += 1
            return small[:, c:c + 1]

        B_ = lambda n: Buf(n)
        consts = B_("consts")
        cl = [
            (identb, identb_d), (identf, identf_d), (tri, tri_d), (perm2, perm_d),
            (baseE, base_d),
        ]
        kb.dma(sp, [lambda e, o=o, i=i: e.dma_start(out=o[:], in_=i) for o, i in cl] +
               [lambda e: e.dma_start(out=mA[:], in_=mA_d.rearrange("a p q -> p a q")),
                lambda e: e.dma_start(out=esink[:], in_=sink_d.partition_broadcast(128)),
                lambda e: e.dma_start(out=brep[:], in_=br_d.partition_broadcast(128)),
                lambda e: e.dma_start(out=wr_sb[:], in_=wr_d.rearrange("(k p) n -> p k n", p=128))],
               writes=[consts])
        kb.op(dve, lambda e: e.memset(onesb[:], 1.0), writes=[consts])
        kb.op(dve, lambda e: e.memset(cb[:], 0.0), writes=[consts])
        kb.op(dve, lambda e: e.memset(zerob[:], 0.0), writes=[consts])
        kb.op(dve, lambda e: e.memset(zerof[:], 0.0), writes=[consts])
        kb.op(act, lambda e: e.activation(out=esink[:], in_=esink[:], func=AF.Exp), reads=[], writes=[consts])
        xsb, ysb_b = Buf("xs", nowaw=True), Buf("ys", nowaw=True)
        xs_zero, ys_zero = B_("xsz"), B_("ysz")
        AFr.reset()
        bgu_rows = AFr.take(2 * D)
        bgr = B_("bgr")
        kb.dma(sp, lambda e: e.dma_start(out=bgu_rows[0:E, :], in_=bgu_d), writes=[bgr])
        for c in range(16):
            bk = c % 2
            kb.op(pe, lambda e, c=c, bk=bk: e.transpose(banks[bk][:, 0:E], bgu_rows[0:E, c * 128:(c + 1) * 128], identf[0:E, 0:E]),
                  reads=[bgr, consts], writes=[bbufs[bk]])
            if c < 8:
                kb.op(dve, lambda e, c=c, bk=bk: e.tensor_copy(out=bguT[:, c, :], in_=banks[bk][:, 0:E]), reads=[bbufs[bk]], writes=[consts])
            else:
                kb.op(dve, lambda e, c=c, bk=bk: e.tensor_scalar(out=bguT[:, c, :], in0=banks[bk][:, 0:E], scalar1=1.0, scalar2=None, op0=ALU.add),
                      reads=[bbufs[bk]], writes=[consts])
        kb.barrier()
        if stop_after == 0:
            kb.emit()
            return nc

        AB.reset(); AFr.reset()
        sh1 = AFr.take(D); G1 = AFr.take(D); csh1 = AFr.take(D); G1c = AFr.take(D)
        greps = [AFr.take(D) for _ in range(4)]
        bm = [AFr.take(512) for _ in range(2)]
        tmpm = AFr.take(512)
        cTt = AFr.take(16); sil = AFr.take(16)
        silrep = AB.take(16 * 128, [16, 128])
        wm = [AB.take(8 * 512, [8, 512]) for _ in range(2)]
        b_g, b_c, b_sil, b_tmp = B_("greps"), B_("cT"), B_("silrep"), B_("tmpm")
        b_bm = [B_("bm0"), B_("bm1")]; b_wm = [B_("wm0"), B_("wm1")]
        b_mod = B_("modout")
        kb.dma(sp, [lambda e, j=j: e.dma_start(out=greps[j], in_=g_d[j].partition_broadcast(128)) for j in range(4)], writes=[b_g])
        kb.dma(sp, lambda e: e.dma_start(out=cTt, in_=cT_d), writes=[b_c])
        kb.op(act, lambda e: e.activation(out=sil, in_=cTt, func=AF.Silu), reads=[b_c], writes=[b_sil])
        for j in range(16):
            kb.op(dve, lambda e, j=j: e.tensor_copy(out=silrep[:, j, :], in_=sil[:, j:j + 1].to_broadcast([128, 128])),
                  reads=[b_sil], writes=[b_sil])
        gi = 0
        for j in range(6):
            for half in range(2):
                c0 = j * D + half * 512
                s = gi % 2
                gi += 1
                kb.dma(pool, lambda e, s=s, c0=c0: e.dma_start(out=wm[s], in_=wmod_v[:, :, c0:c0 + 512]), writes=[b_wm[s]])
                kb.dma(sp, lambda e, s=s, c0=c0: e.dma_start(out=bm[s], in_=bmod_d[0:1, c0:c0 + 512].partition_broadcast(128)), writes=[b_bm[s]])
                cs = slice(half * 512, half * 512 + 512)
                for which in range(2 if j < 2 else 1):
                    bk = 2 * s + which
                    kb.group(pe, [lambda e, k=k, bk=bk, s=s, which=which: e.matmul(banks[bk], lhsT=silrep[:, which * 8 + k, :], rhs=wm[s][:, k, :],
                                                                                  start=(k == 0), stop=(k == 7)) for k in range(8)],
                             reads=[b_sil, b_wm[s]], writes=[bbufs[bk]])
                    if j == 0 or j == 3:
                        dst = (sh1 if j == 0 else sh2[:]) if which == 0 else csh1
                        kb.op(dve, lambda e, bk=bk, s=s, dst=dst, cs=cs: e.tensor_tensor(out=dst[:, cs], in0=banks[bk], in1=bm[s], op=ALU.add),
                              reads=[bbufs[bk], b_bm[s]], writes=[b_mod])
                    else:
                        kb.op(dve, lambda e, bk=bk, s=s: e.tensor_tensor(out=tmpm, in0=banks[bk], in1=bm[s], op=ALU.add),
                              reads=[bbufs[bk], b_bm[s]], writes=[b_tmp])
                        if j == 1:
                            dst, gr = (G1, greps[0]) if which == 0 else (G1c, greps[0])
                        elif j == 2:
                            dst, gr = P1g[:], greps[1]
                        elif j == 4:
                            dst, gr = G2[:], greps[2]
                        else:
                            dst, gr = P2g[:], greps[3]
                        if j in (1, 4):
                            kb.op(dve, lambda e, dst=dst, gr=gr, cs=cs: e.scalar_tensor_tensor(out=dst[:, cs], in0=tmpm, scalar=1.0, in1=gr[:, cs], op0=ALU.add, op1=ALU.mult),
                                  reads=[b_tmp, b_g], writes=[b_mod])
                        else:
                            kb.op(dve, lambda e, dst=dst, gr=gr, cs=cs: e.tensor_tensor(out=dst[:, cs], in0=tmpm, in1=gr[:, cs], op=ALU.mult),
                                  reads=[b_tmp, b_g], writes=[b_mod])
        kb.barrier()
        if stop_after == 1:
            kb.emit()
            return nc

        AB.reset()
        hxT = AB.take(8 * (S + CT), [8, S + CT])
        yT = [AB.take(4 * S, [4, S]) for _ in range(2)]
        AB_base = AB.off
        AFr.reset(4 * D)
        xt = [AFr.take(D) for _ in range(2)]
        tmpf = AFr.take(D)
        hxb = [AB.take(D) for _ in range(2)]
        b_xt = [B_("xt0"), B_("xt1")]; b_tmpf = B_("tmpf"); b_hxb = [B_("hxb0"), B_("hxb1")]
        b_small = B_("small"); b_hxT = B_("hxT")

        def rms_rstd(src_ap, src_bufs, psum=False):
            c1, c2 = col(), col()
            if psum:
                kb.op(act, lambda e: e.activation(out=junkb[:], in_=src_ap, func=AF.Square, accum_out=c1), reads=list(src_bufs), writes=[b_small])
            else:
                kb.op(dve, lambda e: e.scalar_tensor_tensor(out=junkb[:], in0=src_ap, scalar=1.0, in1=src_ap, op0=ALU.mult, op1=ALU.mult, accum_out=c1),
                      reads=list(src_bufs), writes=[b_small])
            kb.op(dve, lambda e: e.tensor_scalar(out=c2, in0=c1, scalar1=1.0 / D, scalar2=EPS, op0=ALU.mult, op1=ALU.add), reads=[b_small], writes=[b_small])
            kb.op(act, lambda e: e.activation(out=c2, in_=c2, func=AF.Ln), reads=[b_small], writes=[b_small])
            kb.op(act, lambda e: e.activation(out=c2, in_=c2, func=AF.Exp, scale=-0.5), reads=[b_small], writes=[b_small])
            return c2

        tmpf2 = [tmpf, AFr.take(D)]
        b_tmpf2 = [b_tmpf, B_("tmpf1")]

        def p1_a(i):
            s = i % 2
            src = x_d[i * 128:(i + 1) * 128, :] if i < NT else ctx_d[(i - NT) * 128:(i - NT + 1) * 128, :]
            Gm, shm = (G1, sh1) if i < NT else (G1c, csh1)
            kb.dma(sp, lambda e: e.dma_start(out=xt[s], in_=src), writes=[b_xt[s]])
            r = rms_rstd(xt[s], [b_xt[s]])
            kb.op(dve, lambda e: e.scalar_tensor_tensor(out=tmpf2[s], in0=xt[s], scalar=r, in1=Gm, op0=ALU.mult, op1=ALU.mult),
                  reads=[b_xt[s], b_small, b_mod], writes=[b_tmpf2[s]])
            kb.op(pool, lambda e: e.tensor_tensor(out=hxb[s], in0=tmpf2[s], in1=shm, op=ALU.add), reads=[b_tmpf2[s], b_mod], writes=[b_hxb[s]])

        def p1_b(i):
            s = i % 2
            pb = psA if s == 0 else psB
            pbuf = bA if s == 0 else bB
            pbv = pb[:, 0:512].bitcast(BF16)
            kb.group(pe, [lambda e, k=k: e.transpose(pbv[:, k * 128:(k + 1) * 128], hxb[s][:, k * 128:(k + 1) * 128], identb[:]) for k in range(8)],
                     reads=[b_hxb[s], consts], writes=[pbuf])
            kb.op(act, lambda e: e.activation(out=hxT[:, :, i * 128:(i + 1) * 128], in_=pbv.rearrange("p (k t) -> p k t", k=8), func=AF.Copy),
                  reads=[pbuf], writes=[b_hxT])

        p1_a(0)
        for i in range(NTT):
            if i + 1 < NTT:
                p1_a(i + 1)
            p1_b(i)
        kb.barrier()
        dump("hxT", hxT, BF16)
        if stop_after == 2:
            kb.emit()
            return nc

        def attn_branch(kind, hg):
            AB.reset(AB_base); AFr.reset(0)
            if kind == "A":
                heads = list(range(8)); npair = 4; nkt = 2; nv = 2
                qc0, vc0 = 0, 640
            else:
                heads = list(range(4 * hg, 4 * hg + 4)); npair = 2; nkt = 2; nv = 4
                qc0, kc0, vc0 = 768 + hg * 256, 1280 + hg * 256, 1792 + hg * 256
            nh = len(heads)
            Wq = AB.take(8 * npair * 128, [8, npair * 128])
            Wk = AB.take(8 * nkt * 128, [8, nkt * 128])
            Wv = AB.take(8 * nv * 64, [8, nv * 64])
            QT = AB.take(npair * S, [npair, S])
            KT = AB.take(nkt * (S + CT), [nkt, S + CT])
            V = AB.take(NTT * nv * 80, [NTT, nv, 80])
            PT = [AB.take(7 * 128) for _ in range(2)]
            ytile = [AB.take(4 * 64, [4, 64]) for _ in range(2)]
            qf = AB.take(BS)
            b_W, b_QT, b_KT, b_V = B_("W"), B_("QT"), B_("KT"), B_("V")
            b_PT = [B_("PT0"), B_("PT1")]; b_yt = [B_("yt0"), B_("yt1")]; b_qf = B_("qf")
            b_yT = B_("yT")
            fl = [lambda e: e.dma_start(out=Wq, in_=win_v[:, :, qc0:qc0 + npair * 128]),
                  lambda e: e.dma_start(out=Wv, in_=win_v[:, :, vc0:vc0 + nv * 64])]
            if kind == "A":
                Wk4 = Wk.rearrange("p k (a b c) -> p k a b c", a=2, b=2)
                src = win_v[:, :, 512:640].rearrange("p k (a c) -> p k a c", a=2)
                for a_ in range(2):
                    for d_ in range(2):
                        fl.append(lambda e, a_=a_, d_=d_: e.dma_start(out=Wk4[:, :, a_, d_, :], in_=src[:, :, a_, :]))
            else:
                fl.append(lambda e: e.dma_start(out=Wk, in_=win_v[:, :, kc0:kc0 + nkt * 128]))
            kb.dma(pool, fl, writes=[b_W])
            kb.op(dve, lambda e: e.memset(V[:, :, :, 64:65], 1.0), writes=[b_V])
            if kind == "A":
                cos2 = AFr.take(S); sin2 = AFr.take(S)
                t1 = AFr.take(BS); t2 = AFr.take(BS)
                b_cs, b_t1, b_t2 = B_("cs"), B_("t1"), B_("t2")
                kb.dma(sp, [lambda e: e.dma_start(out=cos2, in_=cos_d), lambda e: e.dma_start(out=sin2, in_=sin_d)], writes=[b_cs])
                nrow = NSLOT + 128
                kb.dma(sp, [lambda e, r=r: e.dma_start(out=xs_d[r:r + 128, :], in_=zerob[:]) for r in range(0, nrow, 128)],
                       reads=[consts], writes=[xs_zero])
                kb.dma(sp, lambda e: e.dma_start(out=ys_d[NSLOT:NSLOT + 128, :], in_=zerof[:]), reads=[consts], writes=[ys_zero])
            else:
                bres = AB.take(5 * 4 * 128, [5, 4, 128])
                bdyn = AB.take(5 * 4 * 128, [5, 4, 128])
                bst = AFr.take(4 * 128, [4, 128]); mst = AFr.take(128)
                b_bres, b_bdyn, b_bst = B_("bres"), B_("bdyn"), B_("bst")

                def load_pat(pi, dst, dbuf):
                    kb.dma(sp, [lambda e: e.dma_start(out=bst, in_=rpbx_d[pi, :, 4 * hg:4 * hg + 4, :]),
                                lambda e: e.dma_start(out=mst, in_=maskb_d[pi])], writes=[b_bst])
                    kb.op(dve, lambda e: e.tensor_tensor(out=dst, in0=bst, in1=mst.unsqueeze(1).to_broadcast([128, 4, 128]), op=ALU.add),
                          reads=[b_bst], writes=[dbuf])
                for pi, sl in res_slot.items():
                    load_pat(pi, bres[:, sl, :, :], b_bres)

            if stop_after == 2.1:
                kb.barrier()
                return
            def proj_fm(Wt, ct, col0, N, bk):
                kb.group(pe, [lambda e, k=k: e.matmul(banks[bk][:, 0:N], lhsT=Wt[:, k, ct * 128:(ct + 1) * 128], rhs=hxT[:, k, col0:col0 + N],
                                                      start=(k == 0), stop=(k == 7)) for k in range(8)],
                         reads=[b_W, b_hxT], writes=[bbufs[bk]])

            def rope(bk, bk2, dst, col0, dbuf):
                kb.op(act, lambda e: e.activation(out=qf, in_=banks[bk][:, 0:BS], func=AF.Copy), reads=[bbufs[bk]], writes=[b_qf])
                if stop_after == 2.31:
                    return
                kb.op(pe, lambda e: e.matmul(banks[bk2][:, 0:BS], lhsT=perm2[:], rhs=qf, start=True, stop=True), reads=[b_qf, consts], writes=[bbufs[bk2]])
                if stop_after == 2.32:
                    return
                kb.op(dve, lambda e: e.tensor_tensor(out=t1, in0=banks[bk][:, 0:BS], in1=cos2[:, col0:col0 + BS], op=ALU.mult), reads=[bbufs[bk], b_cs, b_qf], writes=[b_t1])
                if stop_after == 2.33:
                    return
                kb.op(dve, lambda e: e.tensor_tensor(out=t2, in0=banks[bk2][:, 0:BS], in1=sin2[:, col0:col0 + BS], op=ALU.mult), reads=[bbufs[bk2], b_cs], writes=[b_t2])
                if stop_after == 2.34:
                    return
                kb.op(pool, lambda e: e.tensor_tensor(out=dst, in0=t1, in1=t2, op=ALU.add), reads=[b_t1, b_t2], writes=[dbuf])

            n = 0
            for tb in range(NB):
                col0 = tb * BS
                for ct in range(npair):
                    bk = 4 + (n % 2); n += 1
                    proj_fm(Wq, ct, col0, BS, bk)
                    if kind == "A":
                        rope(bk, 6 + (n % 2), QT[:, ct, col0:col0 + BS], col0, b_QT)
                    else:
                        kb.op(act, lambda e, bk=bk, ct=ct, col0=col0: e.activation(out=QT[:, ct, col0:col0 + BS], in_=banks[bk][:, 0:BS], func=AF.Copy, scale=0.125),
                              reads=[bbufs[bk]], writes=[b_QT])
                for ct in range(nkt):
                    bk = 4 + (n % 2); n += 1
                    proj_fm(Wk, ct, col0, BS, bk)
                    if kind == "A":
                        rope(bk, 6 + (n % 2), KT[:, ct, col0:col0 + BS], col0, b_KT)
                    else:
                        kb.op(act, lambda e, bk=bk, ct=ct, col0=col0: e.activation(out=KT[:, ct, col0:col0 + BS], in_=banks[bk][:, 0:BS], func=AF.Copy),
                              reads=[bbufs[bk]], writes=[b_KT])
            for ct in range(nkt):
                bk = 4 + (n % 2); n += 1
                proj_fm(Wk, ct, S, CT, bk)
                kb.op(act, lambda e, bk=bk, ct=ct: e.activation(out=KT[:, ct, S:S + CT], in_=banks[bk][:, 0:CT], func=AF.Copy), reads=[bbufs[bk]], writes=[b_KT])
            for i in range(NTT):
                bk = 4 + (n % 2); n += 1
                kb.group(pe, [lambda e, k=k, bk=bk, i=i: e.matmul(banks[bk][:, 0:nv * 64], lhsT=hxT[:, k, i * 128:(i + 1) * 128], rhs=Wv[:, k, :],
                                                                  start=(k == 0), stop=(k == 7)) for k in range(8)],
                         reads=[b_W, b_hxT], writes=[bbufs[bk]])
                kb.op(dve, lambda e, bk=bk, i=i: e.tensor_copy(out=V[:, i, :, 0:64], in_=banks[bk][:, 0:nv * 64].rearrange("p (a c) -> p a c", a=nv)),
                      reads=[bbufs[bk]], writes=[b_V])

            dump("QT_%s%d" % (kind, hg), QT, BF16); dump("KT_%s%d" % (kind, hg), KT, BF16); dump("V_%s%d" % (kind, hg), V[:, :, :, 0:65], BF16)
            if stop_after == 2.5:
                kb.barrier()
                return
            SP = [(psA, bA), (psB, bB)]
            units = []

            def mk_unit(u, qt, quad, hq, pre):
                qs = slice(qt * 128, (qt + 1) * 128)
                ob, obuf = banks[4 + (u // 4) % 2], bbufs[4 + (u // 4) % 2]
                hi = quad * 4 + hq
                h = heads[hi]
                half = hi % 2
                ps_ = slice(half * 64, half * 64 + 64)
                pair = hi // 2
                spt, sbuf_ = SP[u % 2]
                pt = PT[u % 2]
                st8 = {}

                def scores():
                    if kind == "B" and pre:
                        dyn_slot = {}
                        for c, pi in bchunks[qt]:
                            if pi not in res_slot and pi not in dyn_slot:
                                dyn_slot[pi] = len(dyn_slot)
                                load_pat(pi, bdyn[:, dyn_slot[pi], :, :], b_bdyn)
                    if kind == "A":
                        kti, vi = h // 4, h // 4
                        chunks = []
                        if qt > 0:
                            chunks.append((qt - 1, mA[:, 0, :], consts))
                        chunks.append((qt, None, None))
                        if qt < NT - 1:
                            chunks.append((qt + 1, mA[:, 1, :], consts))
                    else:
                        kti, vi = hi // 2, hi
                        dyn_slot = {}
                        for c, pi in bchunks[qt]:
                            if pi not in res_slot and pi not in dyn_slot:
                                dyn_slot[pi] = len(dyn_slot)
                        chunks = []
                        for c, pi in bchunks[qt]:
                            if pi in res_slot:
                                chunks.append((c, bres[:, res_slot[pi], hi, :], b_bres))
                            else:
                                chunks.append((c, bdyn[:, dyn_slot[pi], hi, :], b_bdyn))
                    for c in range(NCT):
                        chunks.append((NT + c, None, None))
                    ncu = len(chunks)
                    assert ncu <= 7
                    st8["chunks"], st8["vi"], st8["ncu"] = chunks, vi, ncu
                    fns = []
                    rd = [b_QT, b_KT, consts]
                    for ci, (kt_, bias, bb) in enumerate(chunks):
                        o_ = spt[:, ci * 128:(ci + 1) * 128]
                        fns.append(lambda e, o_=o_, kt_=kt_, bias=bias, kti=kti: e.matmul(
                            o_, lhsT=KT[ps_, kti, kt_ * 128:(kt_ + 1) * 128], rhs=QT[ps_, pair, qs], start=True, stop=(bias is None)))
                        if bias is not None:
                            fns.append(lambda e, o_=o_, bias=bias: e.matmul(o_, lhsT=identb[:], rhs=bias, start=False, stop=True))
                            if bb not in rd:
                                rd.append(bb)
                    kb.group(pe, fns, reads=rd, writes=[sbuf_])

                def rest():
                    chunks, vi, ncu = st8["chunks"], st8["vi"], st8["ncu"]
                    kb.op(act, lambda e: e.activation(out=pt[:, 0:ncu * 128], in_=spt[:, 0:ncu * 128], func=AF.Exp, scale=(0.125 if kind == "A" else 1.0)),
                          reads=[sbuf_], writes=[b_PT[u % 2]])
                    kb.group(pe, [lambda e, ci=ci, kt_=kt_: e.matmul(
                        ob[:, hq * 80:hq * 80 + 65], lhsT=pt[:, ci * 128:(ci + 1) * 128], rhs=V[:, kt_, vi, 0:65], start=(ci == 0), stop=(ci == ncu - 1))
                        for ci, (kt_, _, _) in enumerate(chunks)],
                             reads=[b_PT[u % 2], b_V], writes=[obuf] if hq == 0 else [], )
                    if hq > 0:
                        obuf.w = [pe.last()]
                    if hq < 3:
                        return
                    yi = ((u + 1) // 4) % 2
                    ob3 = ob[:, 0:320].rearrange("p (a c) -> p a c", a=4)
                    dn = dnt[:, yi, :]
                    if kind == "A":
                        kb.op(dve, lambda e: e.tensor_tensor(out=dn.unsqueeze(2), in0=ob3[:, :, 64:65], in1=esink[:, quad * 4:quad * 4 + 4].unsqueeze(2), op=ALU.add),
                              reads=[obuf, consts], writes=[b_small])
                        kb.op(dve, lambda e: e.reciprocal(out=dn, in_=dn), reads=[b_small], writes=[b_small])
                    else:
                        kb.op(dve, lambda e: e.reciprocal(out=dn.unsqueeze(2), in_=ob3[:, :, 64:65]), reads=[obuf], writes=[b_small])
                    kb.op(dve, lambda e: e.tensor_tensor(out=ytile[yi], in0=ob3[:, :, 0:64], in1=dn.unsqueeze(2).to_broadcast([128, 4, 64]), op=ALU.mult),
                          reads=[obuf, b_small], writes=[b_yt[yi]])
                    tb_, tbuf = banks[6 + yi], bbufs[6 + yi]
                    tbv = tb_.bitcast(BF16)
                    ytf = ytile[yi].rearrange("p a c -> p (a c)")
                    kb.group(pe, [lambda e, pr=pr: e.transpose(tbv[:, pr * 128:(pr + 1) * 128], ytf[:, pr * 128:(pr + 1) * 128], identb[:]) for pr in range(2)],
                             reads=[b_yt[yi], consts], writes=[tbuf])
                    yTd = yT[0] if kind == "A" else yT[1]
                    p0 = quad * 2 if kind == "A" else hg * 2
                    kb.op(act, lambda e: e.activation(out=yTd[:, p0:p0 + 2, qs], in_=tbv[:, 0:256].rearrange("p (a t) -> p a t", a=2), func=AF.Copy),
                          reads=[tbuf], writes=[b_yT])
                return scores, rest

            u = 0
            for qt in range(NT):
                for quad in range(nh // 4):
                    for hq in range(4):
                        units.append(mk_unit(u, qt, quad, hq, pre=(quad == 0 and hq == 0)))
                        u += 1
            units[0][0]()
            for i in range(len(units)):
                if i + 1 < len(units):
                    units[i + 1][0]()
                units[i][1]()
            kb.barrier()

        attn_branch("A", 0)
        if stop_after in (3, 2.5, 2.6, 2.7, 2.1, 2.2, 2.3, 2.31, 2.32, 2.33, 2.34):
            kb.emit()
            return nc
        attn_branch("B", 0)
        attn_branch("B", 1)
        dump("yaT", yT[0], BF16); dump("ybT", yT[1], BF16)
        if stop_after == 4:
            kb.emit()
            return nc

        AB.reset(AB_base); AFr.reset(0)
        mT = AB.take(8 * S, [8, S])
        AB_3b = AB.off
        wg = [[AB.take(8 * 128, [8, 128]) for _ in range(2)] for _ in range(2)]
        wbr = [[AB.take(4 * 128, [4, 128]) for _ in range(2)] for _ in range(2)]
        sg = [[AB.take(BS) for _ in range(2)] for _ in range(2)]
        m1 = [AFr.take(BS) for _ in range(2)]; m2 = [AFr.take(BS) for _ in range(2)]
        b_wg = [B_("wg0"), B_("wg1")]; b_sg = [[B_("sg00"), B_("sg01")], [B_("sg10"), B_("sg11")]]
        b_mT = B_("mT"); b_m1 = [B_("m10"), B_("m11")]; b_m2 = [B_("m20"), B_("m21")]
        wba_v = wba_d.rearrange("(k p) n -> p k n", p=128); wbb_v = wbb_d.rearrange("(k p) n -> p k n", p=128)

        def load_ct(ct):
            s = ct % 2
            kb.dma(pool, [lambda e: e.dma_start(out=wg[s][0], in_=win_v[:, :, 2304 + ct * 128:2304 + (ct + 1) * 128]),
                          lambda e: e.dma_start(out=wg[s][1], in_=win_v[:, :, 3328 + ct * 128:3328 + (ct + 1) * 128]),
                          lambda e: e.dma_start(out=wbr[s][0], in_=wba_v[:, :, ct * 128:(ct + 1) * 128]),
                          lambda e: e.dma_start(out=wbr[s][1], in_=wbb_v[:, :, ct * 128:(ct + 1) * 128])],
                   writes=[b_wg[s]])

        load_ct(0)
        n3 = 0
        for ct in range(8):
            s = ct % 2
            if ct + 1 < 8:
                load_ct(ct + 1)
            for tb in range(NB):
                cs_ = slice(tb * BS, (tb + 1) * BS)
                u3 = n3 % 2; n3 += 1
                for ab in range(2):
                    bk = 4 * u3 + ab
                    kb.group(pe, [lambda e, k=k, s=s, ab=ab, bk=bk, cs_=cs_: e.matmul(banks[bk][:, 0:BS], lhsT=wg[s][ab][:, k, :], rhs=hxT[:, k, cs_], start=(k == 0), stop=(k == 7))
                                  for k in range(8)], reads=[b_wg[s], b_hxT], writes=[bbufs[bk]])
                    kb.op(act, lambda e, u3=u3, ab=ab, bk=bk: e.activation(out=sg[u3][ab], in_=banks[bk][:, 0:BS], func=AF.Sigmoid), reads=[bbufs[bk]], writes=[b_sg[u3][ab]])
                    bk2 = 4 * u3 + 2 + ab
                    kb.group(pe, [lambda e, k=k, s=s, ab=ab, bk2=bk2, cs_=cs_: e.matmul(banks[bk2][:, 0:BS], lhsT=wbr[s][ab][:, k, :], rhs=yT[ab][:, k, cs_], start=(k == 0), stop=(k == 3))
                                  for k in range(4)], reads=[b_wg[s]], writes=[bbufs[bk2]])
                    mm, bmm = (m1[u3], b_m1[u3]) if ab == 0 else (m2[u3], b_m2[u3])
                    kb.op(dve, lambda e, u3=u3, ab=ab, bk2=bk2, mm=mm: e.tensor_tensor(out=mm, in0=banks[bk2][:, 0:BS], in1=sg[u3][ab], op=ALU.mult),
                          reads=[bbufs[bk2], b_sg[u3][ab]], writes=[bmm])
                kb.op(dve, lambda e, ct=ct, cs_=cs_, u3=u3: e.tensor_tensor(out=mT[:, ct, cs_], in0=m1[u3], in1=m2[u3], op=ALU.add), reads=[b_m1[u3], b_m2[u3]], writes=[b_mT])
        kb.barrier()

        AB.reset(AB_3b); AFr.reset(0)
        Wout = AB.take(8 * D, [8, D])
        hx2b = [AB.take(D) for _ in range(2)]
        Mb = AB.take(E)
        xt3 = [AFr.take(D) for _ in range(2)]
        x1t = [AFr.take(D) for _ in range(2)]
        t3 = AFr.take(D); hx2f = [AFr.take(D) for _ in range(2)]
        hx2T = AFr.take(8 * 128, [8, 128])
        lg = AFr.take(E); mx8 = AFr.take(8); ew = AFr.take(4); dfull = AFr.take(E); spos = AFr.take(E); pen = AFr.take(E)
        junkE = AFr.take(E); destf = AFr.take(4); negm = AFr.take(1); sumw = AFr.take(1)
        b_Wout = B_("Wout")
        b_xt3 = [B_("xt30"), B_("xt31")]; b_x1t = [B_("x1t0"), B_("x1t1")]; b_t3 = B_("t3"); b_hx2f = [B_("hx2f0"), B_("hx2f1")]
        b_hx2b = [B_("hx2b0"), B_("hx2b1")]; b_hx2T = B_("hx2T"); b_r = B_("route"); b_Mb = B_("Mb"); b_cb = B_("cb")
        b_route_out = B_("routeout"); b_outd = Buf("outd", nowaw=True)
        kb.dma(pool, lambda e: e.dma_start(out=Wout, in_=wout_d.rearrange("(k p) n -> p k n", p=128)), writes=[b_Wout])

        def stage1(ti):
            s = ti % 2
            js = slice(ti * 128, (ti + 1) * 128)
            rows = js
            kb.dma(sp, lambda e: e.dma_start(out=xt3[s], in_=x_d[rows, :]), writes=[b_xt3[s]])
            for half in range(2):
                kb.group(pe, [lambda e, k=k, half=half: e.matmul(psC[:, half * 512:(half + 1) * 512], lhsT=mT[:, k, js], rhs=Wout[:, k, half * 512:(half + 1) * 512],
                                                                 start=(k == 0), stop=(k == 7)) for k in range(8)],
                         reads=[b_mT, b_Wout], writes=[bC] if half == 0 else [])
            bC.w = [pe.last()]
            r = rms_rstd(psC[:, :], [bC], psum=True)
            kb.op(dve, lambda e: e.scalar_tensor_tensor(out=t3, in0=psC[:, :], scalar=r, in1=P1g[:], op0=ALU.mult, op1=ALU.mult), reads=[bC, b_small], writes=[b_t3])
            kb.op(pool, lambda e: e.tensor_tensor(out=x1t[s], in0=t3, in1=xt3[s], op=ALU.add), reads=[b_t3, b_xt3[s]], writes=[b_x1t[s]])
            kb.dma(sp, lambda e: e.dma_start(out=out_d[rows, :], in_=x1t[s]), reads=[b_x1t[s]], writes=[b_outd], semof=b_x1t[s])
            r2 = rms_rstd(x1t[s], [b_x1t[s]])
            kb.op(dve, lambda e: e.scalar_tensor_tensor(out=t3, in0=x1t[s], scalar=r2, in1=G2[:], op0=ALU.mult, op1=ALU.mult), reads=[b_x1t[s], b_small], writes=[b_t3])
            kb.op(pool, lambda e: e.tensor_tensor(out=hx2f[s], in0=t3, in1=sh2[:], op=ALU.add), reads=[b_t3], writes=[b_hx2f[s]])
            kb.op(act, lambda e: e.activation(out=hx2b[s], in_=hx2f[s], func=AF.Copy), reads=[b_hx2f[s]], writes=[b_hx2b[s]])

        def stage2(ti):
            s = ti % 2
            kb.group(pe, [lambda e, k=k: e.transpose(psD[:, k * 128:(k + 1) * 128], hx2f[s][:, k * 128:(k + 1) * 128], identf[:]) for k in range(8)],
                     reads=[b_hx2f[s], consts], writes=[bD])
            kb.op(dve, lambda e: e.tensor_copy(out=hx2T, in_=psD[:, :].rearrange("p (k t) -> p k t", k=8)), reads=[bD], writes=[b_hx2T])
            kb.group(pe, [lambda e, k=k: e.matmul(banks[0][:, 0:E], lhsT=hx2T[:, k, :], rhs=wr_sb[:, k, :], start=(k == 0), stop=(k == 7)) for k in range(8)],
                     reads=[b_hx2T, consts], writes=[bbufs[0]])
            R = [b_r]
            kb.op(dve, lambda e: e.tensor_tensor(out=lg, in0=banks[0][:, 0:E], in1=brep[:], op=ALU.add), reads=[bbufs[0], consts], writes=R)
            kb.op(dve, lambda e: e.max(out=mx8, in_=lg), reads=R, writes=R)
            kb.op(dve, lambda e: e.tensor_scalar(out=negm, in0=mx8[:, 0:1], scalar1=-1.0, scalar2=None, op0=ALU.mult), reads=R, writes=R)
            kb.op(act, lambda e: e.activation(out=ew, in_=mx8[:, 0:4], func=AF.Exp, bias=negm, scale=1.0, accum_out=sumw), reads=R, writes=R)
            kb.op(dve, lambda e: e.reciprocal(out=sumw, in_=sumw), reads=R, writes=R)
            kb.op(dve, lambda e: e.tensor_scalar(out=wts[:, ti, :], in0=ew, scalar1=sumw, scalar2=None, op0=ALU.mult), reads=R, writes=[b_route_out])
            kb.op(dve, lambda e: e.tensor_scalar(out=Mb, in0=lg, scalar1=mx8[:, 3:4], scalar2=None, op0=ALU.is_ge), reads=R, writes=[b_Mb])
            kb.op(pe, lambda e: e.matmul(banks[1][:, 0:E], lhsT=tri[:], rhs=Mb, start=True, stop=True), reads=[b_Mb, consts], writes=[bbufs[1]])
            kb.op(pe, lambda e: e.matmul(banks[2][:, 0:E], lhsT=onesb[:], rhs=Mb, start=True, stop=True), reads=[b_Mb, consts], writes=[bbufs[2]])
            kb.op(dve, lambda e: e.tensor_tensor(out=spos, in0=banks[1][:, 0:E], in1=cb[:], op=ALU.add), reads=[bbufs[1], b_cb], writes=R)
            kb.op(dve, lambda e: e.tensor_tensor(out=cb[:], in0=banks[2][:, 0:E], in1=cb[:], op=ALU.add), reads=[bbufs[2], b_r], writes=[b_cb])
            kb.op(dve, lambda e: e.tensor_scalar(out=pen, in0=spos, scalar1=float(CAP), scalar2=1.0e6, op0=ALU.is_ge, op1=ALU.mult), reads=R, writes=R)
            kb.op(dve, lambda e: e.tensor_tensor(out=dfull, in0=spos, in1=baseE[:], op=ALU.add), reads=R + [consts], writes=R)
            kb.op(dve, lambda e: e.tensor_tensor(out=dfull, in0=dfull, in1=pen, op=ALU.add), reads=R, writes=R)
            for k in range(4):
                kb.op(dve, lambda e, k=k: e.scalar_tensor_tensor(out=junkE, in0=lg, scalar=mx8[:, k:k + 1], in1=dfull, op0=ALU.is_equal, op1=ALU.mult,
                                                                 accum_out=destf[:, k:k + 1]), reads=R, writes=R)
            kb.op(dve, lambda e: e.tensor_scalar(out=desti[:, ti, :], in0=destf, scalar1=float(NSLOT), scalar2=None, op0=ALU.min), reads=R, writes=[b_route_out])
            kb.dma(pool, [lambda e, k=k: e.indirect_dma_start(out=xs_d[:, :], out_offset=bass.IndirectOffsetOnAxis(ap=desti[:, ti, k:k + 1], axis=0),
                                                             in_=hx2b[s], in_offset=None)
                          for k in range(4)], reads=[b_hx2b[s], b_route_out, xs_zero], writes=[xsb], semof=b_hx2b[s])

        stage1(0)
        for ti in range(NT):
            if ti + 1 < NT:
                stage1(ti + 1)
            stage2(ti)
        kb.barrier()
        dump("wts", wts[:], F32); dump("desti", desti[:], I32)
        if stop_after == 5:
            kb.emit()
            return nc

        AB.reset(0); AFr.reset(0)
        Wgu = [AB.take(8 * 2 * D, [8, 2 * D]) for _ in range(2)]
        Wd = [AB.take(8 * D, [8, D]) for _ in range(2)]
        XT = [AB.take(8 * CAP, [8, CAP]) for _ in range(2)]
        xrows = AB.take(NJ * D, [NJ, D])
        hT = AB.take(8 * CAP, [8, CAP])
        gt = [AFr.take(CAP) for _ in range(2)]; sgm = [AFr.take(CAP) for _ in range(2)]
        ua = [AFr.take(CAP) for _ in range(2)]; glu = [AFr.take(CAP) for _ in range(2)]
        ysb = [AFr.take(512) for _ in range(4)]
        bdr = [AFr.take(D) for _ in range(2)]
        b_Wgu = [B_("Wgu0"), B_("Wgu1")]; b_Wd = [B_("Wd0"), B_("Wd1")]; b_XT = [B_("XT0"), B_("XT1")]; b_xr, b_hT = B_("xrows"), B_("hT")
        b_gt = [B_("gt0"), B_("gt1")]; b_sgm = [B_("sgm0"), B_("sgm1")]; b_ua = [B_("ua0"), B_("ua1")]; b_glu = [B_("glu0"), B_("glu1")]
        b_ysb = [B_("ysb%d" % i) for i in range(4)]; b_bdr = [B_("bdr0"), B_("bdr1")]

        def load_w(ex):
            s = ex % 2
            wgv = wgu_d[ex].rearrange("(k p) n -> p k n", p=128)
            wdv = wd_d[ex].rearrange("(k p) n -> p k n", p=128)
            kb.dma(pool, [lambda e, q4=q4: e.dma_start(out=Wgu[s][:, 2 * q4:2 * q4 + 2, :], in_=wgv[:, 2 * q4:2 * q4 + 2, :]) for q4 in range(4)],
                   writes=[b_Wgu[s]])
            kb.dma(pool, [lambda e, q2=q2: e.dma_start(out=Wd[s][:, 4 * q2:4 * q2 + 4, :], in_=wdv[:, 4 * q2:4 * q2 + 4, :]) for q2 in range(2)],
                   writes=[b_Wd[s]])
            kb.dma(sp, lambda e: e.dma_start(out=bdr[s], in_=bd_d[ex:ex + 1, :].partition_broadcast(128)), writes=[b_bdr[s]])

        def prep(ex):
            s = ex % 2
            kb.dma(sp, lambda e: e.dma_start(out=xrows, in_=xs_d[ex * CAP:(ex + 1) * CAP, :].rearrange("(j p) d -> p j d", p=128)), reads=[xsb], writes=[b_xr])
            for j in range(NJ):
                bk = j % 2
                pbv = banks[bk].bitcast(BF16)
                kb.group(pe, [lambda e, k=k, j=j, pbv=pbv: e.transpose(pbv[:, k * 128:(k + 1) * 128], xrows[:, j, k * 128:(k + 1) * 128], identb[:]) for k in range(8)],
                         reads=[b_xr, consts], writes=[bbufs[bk]])
                kb.op(act, lambda e, j=j, pbv=pbv: e.activation(out=XT[s][:, :, j * 128:(j + 1) * 128], in_=pbv.rearrange("p (k t) -> p k t", k=8), func=AF.Copy),
                      reads=[bbufs[bk]], writes=[b_XT[s]])

        load_w(0)
        prep(0)
        ny = 0
        for ex in range(E):
            s = ex % 2
            if ex + 1 < E:
                load_w(ex + 1)
            for f in range(8):
                gb_, ub_ = (0, 1) if f % 2 == 0 else (2, 3)
                fs = f % 2
                kb.group(pe, [lambda e, k=k, f=f, s=s, gb_=gb_: e.matmul(banks[gb_][:, 0:CAP], lhsT=Wgu[s][:, k, f * 128:(f + 1) * 128], rhs=XT[s][:, k, :], start=(k == 0), stop=(k == 7))
                              for k in range(8)], reads=[b_Wgu[s], b_XT[s]], writes=[bbufs[gb_]])
                kb.group(pe, [lambda e, k=k, f=f, s=s, ub_=ub_: e.matmul(banks[ub_][:, 0:CAP], lhsT=Wgu[s][:, k, D + f * 128:D + (f + 1) * 128], rhs=XT[s][:, k, :], start=(k == 0), stop=(k == 7))
                              for k in range(8)], reads=[b_Wgu[s], b_XT[s]], writes=[bbufs[ub_]])
                kb.op(dve, lambda e, f=f, ex=ex, gb_=gb_, fs=fs: e.tensor_scalar(out=gt[fs], in0=banks[gb_][:, 0:CAP], scalar1=bguT[:, f, ex:ex + 1], scalar2=7.0, op0=ALU.add, op1=ALU.min),
                      reads=[bbufs[gb_], consts], writes=[b_gt[fs]])
                kb.op(act, lambda e, fs=fs: e.activation(out=sgm[fs], in_=gt[fs], func=AF.Sigmoid, scale=1.702), reads=[b_gt[fs]], writes=[b_sgm[fs]])
                kb.op(dve, lambda e, f=f, ex=ex, ub_=ub_, fs=fs: e.tensor_scalar(out=ua[fs], in0=banks[ub_][:, 0:CAP], scalar1=bguT[:, 8 + f, ex:ex + 1], scalar2=8.0, op0=ALU.add, op1=ALU.min),
                      reads=[bbufs[ub_], consts], writes=[b_ua[fs]])
                kb.op(pool, lambda e, fs=fs: e.tensor_tensor(out=glu[fs], in0=gt[fs], in1=sgm[fs], op=ALU.mult), reads=[b_gt[fs], b_sgm[fs]], writes=[b_glu[fs]])
                kb.op(dve, lambda e, f=f, fs=fs: e.scalar_tensor_tensor(out=hT[:, f, :], in0=ua[fs], scalar=-6.0, in1=glu[fs], op0=ALU.max, op1=ALU.mult), reads=[b_ua[fs], b_glu[fs]], writes=[b_hT])
            if ex + 1 < E:
                prep(ex + 1)
            for j in range(NJ):
                js = slice(j * 128, (j + 1) * 128)
                r0 = ex * CAP + j * 128
                for half in range(2):
                    yb_ = ny % 4; ny += 1
                    bk = 4 + yb_
                    kb.group(pe, [lambda e, k=k, half=half, s=s, js=js, bk=bk: e.matmul(banks[bk], lhsT=hT[:, k, js], rhs=Wd[s][:, k, half * 512:(half + 1) * 512],
                                                                                      start=(k == 0), stop=(k == 7)) for k in range(8)],
                             reads=[b_hT, b_Wd[s]], writes=[bbufs[bk]])
                    kb.op(dve, lambda e, yb_=yb_, s=s, bk=bk, half=half: e.tensor_tensor(out=ysb[yb_], in0=banks[bk], in1=bdr[s][:, half * 512:(half + 1) * 512], op=ALU.add),
                          reads=[bbufs[bk], b_bdr[s]], writes=[b_ysb[yb_]])
                    kb.dma(sp, lambda e, yb_=yb_, r0=r0, half=half: e.dma_start(out=ys_d[r0:r0 + 128, half * 512:(half + 1) * 512], in_=ysb[yb_]), reads=[b_ysb[yb_]], writes=[ysb_b], semof=b_ysb[yb_])
        kb.barrier()
        if stop_after == 6:
            kb.emit()
            return nc

        AFr.reset(0); AB.reset(0)
        Yk = [[AB.take(2 * D).bitcast(F32) for _ in range(4)] for _ in range(2)]
        x1r = [AFr.take(D) for _ in range(2)]; acc = AFr.take(D); ot = [AFr.take(D) for _ in range(2)]
        b_Yk = [[B_("Yk%d%d" % (st_, k)) for k in range(4)] for st_ in range(2)]
        b_x1r = [B_("x1r0"), B_("x1r1")]; b_acc = B_("acc"); b_ot = [B_("ot0"), B_("ot1")]
        b_fin = Buf("fin", nowaw=True)

        def gath(ti):
            st_ = ti % 2
            rows = slice(ti * 128, (ti + 1) * 128)
            for k in range(4):
                kb.dma(pool, lambda e, k=k: e.indirect_dma_start(out=Yk[st_][k], out_offset=None, in_=ys_d[:, :],
                                                                 in_offset=bass.IndirectOffsetOnAxis(ap=desti[:, ti, k:k + 1], axis=0)),
                       reads=[ysb_b, ys_zero, b_route_out], writes=[b_Yk[st_][k]])
            kb.dma(sp, lambda e: e.dma_start(out=x1r[st_], in_=out_d[rows, :]), reads=[b_outd], writes=[b_x1r[st_]])

        gath(0)
        for ti in range(NT):
            st_ = ti % 2
            rows = slice(ti * 128, (ti + 1) * 128)
            if ti + 1 < NT:
                gath(ti + 1)
            kb.op(dve, lambda e, ti=ti, st_=st_: e.tensor_scalar(out=acc, in0=Yk[st_][0], scalar1=wts[:, ti, 0:1], scalar2=None, op0=ALU.mult), reads=[b_Yk[st_][0], b_route_out], writes=[b_acc])
            for k in range(1, 4):
                kb.op(dve, lambda e, ti=ti, k=k, st_=st_: e.scalar_tensor_tensor(out=acc, in0=Yk[st_][k], scalar=wts[:, ti, k:k + 1], in1=acc, op0=ALU.mult, op1=ALU.add),
                      reads=[b_Yk[st_][k], b_route_out, b_acc], writes=[b_acc])
            r = rms_rstd(acc, [b_acc])
            kb.op(dve, lambda e, r=r: e.scalar_tensor_tensor(out=acc, in0=acc, scalar=r, in1=P2g[:], op0=ALU.mult, op1=ALU.mult), reads=[b_acc, b_small], writes=[b_acc])
            kb.op(dve, lambda e, st_=st_: e.tensor_tensor(out=ot[st_], in0=acc, in1=x1r[st_], op=ALU.add), reads=[b_acc, b_x1r[st_]], writes=[b_ot[st_]])
            kb.dma(sp, lambda e, rows=rows, st_=st_: e.dma_start(out=out_d[rows, :], in_=ot[st_]), reads=[b_ot[st_], b_x1r[st_]], writes=[b_fin], semof=b_ot[st_])
        kb.barrier()
        kb.emit()
    return nc


def host_inputs(cfg, inp, b):
    S, CT, E, CAP = cfg["S"], cfg["CT"], cfg["E"], cfg["CAP"]
    f = np.float32
    c = np.asarray(inp["c"][b], f); cc = np.asarray(inp["c_ctx"], f)
    cT = np.concatenate([c.reshape(8, 128).T, cc.reshape(8, 128).T], axis=1)
    pats, _ = nbr_patterns(S)
    rpb = np.asarray(inp["b_rpb"][0], f)
    rpbx = np.stack([rpb[:, dr, dc].transpose(1, 0, 2) for ok, dr, dc in pats]).astype(f)
    maskb = np.stack([np.where(ok, 0.0, NEG) for ok, dr, dc in pats]).astype(f)
    cos2, sin2, perm2 = rope_tables(S)
    kj = np.arange(128)[:, None]; qi = np.arange(128)[None, :]
    maskA = np.stack([np.where(qi <= kj, 0.0, NEG), np.where(kj <= qi, 0.0, NEG)]).astype(ml_dtypes.bfloat16)
    tri = (kj < qi).astype(ml_dtypes.bfloat16)
    return {
        "x": np.ascontiguousarray(inp["x"][b], f), "ctx": np.ascontiguousarray(inp["ctx"][b], f), "cT": np.ascontiguousarray(cT, f),
        "w_mod": np.asarray(inp["w_mod"][0], f), "b_mod": np.asarray(inp["b_mod"], f).reshape(1, -1),
        "g_pre_mix": np.asarray(inp["g_pre_mix"], f).reshape(1, -1), "g_post_mix": np.asarray(inp["g_post_mix"], f).reshape(1, -1),
        "g_pre_ffn": np.asarray(inp["g_pre_ffn"], f).reshape(1, -1), "g_post_ffn": np.asarray(inp["g_post_ffn"], f).reshape(1, -1),
        "w_in": np.asarray(inp["w_in"][0], f), "a_sink": np.asarray(inp["a_sink"], f).reshape(1, 8),
        "rpbx": rpbx, "maskb": maskb,
        "w_branch_a": np.asarray(inp["w_branch_a"][0], f), "w_branch_b": np.asarray(inp["w_branch_b"][0], f), "w_out": np.asarray(inp["w_out"][0], f),
        "w_router": np.asarray(inp["w_router"][0], f), "b_router": np.asarray(inp["b_router"], f).reshape(1, -1),
        "w_gate_up": np.asarray(inp["w_gate_up"][0], f), "b_gate_up": np.asarray(inp["b_gate_up"][0], f),
        "w_down": np.asarray(inp["w_down"][0], f), "b_down": np.asarray(inp["b_down"][0], f),
        "identf": np.eye(128, dtype=f), "tri": tri, "identb": np.eye(128).astype(ml_dtypes.bfloat16),
        "maskA": maskA, "perm2": perm2.astype(ml_dtypes.bfloat16), "cos2": cos2, "sin2": sin2,
        "baseE": np.tile((np.arange(E, dtype=f) * CAP)[None, :], (128, 1)).astype(f),
    }


def kernel(**inputs):
    cfg = FULL
    nb = inputs["x"].shape[0]
    nc = build(cfg)
    in_maps = [host_inputs(cfg, inputs, b) for b in range(nb)]
    res = run_bass_kernel_spmd(nc, in_maps, core_ids=list(range(nb)))
    return np.stack([np.asarray(r["out"], np.float32) for r in res.results], axis=0)
```

```python
import numpy as np
import ml_dtypes
from contextlib import ExitStack
import concourse.bass as bass
import concourse.mybir as mybir
from concourse.bass_utils import run_bass_kernel_spmd

F32 = mybir.dt.float32
BF16 = mybir.dt.bfloat16
I32 = mybir.dt.int32
ALU = mybir.AluOpType
AF = mybir.ActivationFunctionType

D = 1024
GRID_W = 64
HD = 64
TOPK = 4
NEG = -30000.0
EPS = 1e-6
FULL = dict(S=2048, CT=256, E=32, CAP=512)


class Q:
    def __init__(self, kb, name):
        self.name = name
        self.ops = []
        self.sem = kb.new_sem("q_" + name)
        self.count = 0
        self.seen = {}

    def wait(self, *tickets):
        for t in tickets:
            if t is None:
                continue
            sem, val = t
            key = id(sem)
            if self.seen.get(key, 0) >= val:
                continue
            self.seen[key] = val
            self.ops.append(lambda e, sem=sem, val=val: e.wait_ge(sem, val))

    def last(self):
        return (self.sem, self.count) if self.count else None


class Buf:
    def __init__(self, name, nowaw=False):
        self.name = name
        self.w = []
        self.r = []
        self.ds = None
        self.nowaw = nowaw


class KB:
    def __init__(self, nc, stack):
        self.nc = nc
        self.stack = stack
        self.dsems = []
        self.pe, self.act, self.dve, self.pool, self.sp = (Q(self, n) for n in ("pe", "act", "dve", "pool", "sp"))
        self.qs = [self.pe, self.act, self.dve, self.pool, self.sp]

    def new_sem(self, name):
        self.nsem = getattr(self, "nsem", 0) + 1
        return self.stack.enter_context(self.nc.semaphore("%s_%d" % (name, self.nsem)))

    def sb(self, name, shape, dt):
        return self.stack.enter_context(self.nc.sbuf_tensor("s_" + name, shape, dt))

    def ps(self, name, shape, dt):
        return self.stack.enter_context(self.nc.psum_tensor(name, shape, dt))

    def _pre(self, q, reads, writes):
        for b in reads:
            q.wait(*b.w)
        for b in writes:
            if not b.nowaw:
                q.wait(*b.w)
            q.wait(*b.r)

    def _post(self, t, reads, writes):
        for b in reads:
            b.r.append(t)
        for b in writes:
            if b.nowaw:
                b.w = [x for x in b.w if x[0] is not t[0]] + [t]
            else:
                b.w = [t]
            b.r = []

    def op(self, q, fn, reads=(), writes=()):
        self._pre(q, reads, writes)
        q.count += 1
        sem = q.sem
        q.ops.append(lambda e, fn=fn, sem=sem: fn(e).then_inc(sem, 1))
        t = (sem, q.count)
        self._post(t, reads, writes)
        return t

    def group(self, q, fns, reads=(), writes=()):
        self._pre(q, reads, writes)
        for fn in fns[:-1]:
            q.ops.append(lambda e, fn=fn: fn(e))
        q.count += 1
        sem = q.sem
        fn = fns[-1]
        q.ops.append(lambda e, fn=fn, sem=sem: fn(e).then_inc(sem, 1))
        t = (sem, q.count)
        self._post(t, reads, writes)
        return t

    def dma(self, q, fns, reads=(), writes=(), semof=None):
        if not isinstance(fns, (list, tuple)):
            fns = [fns]
        self._pre(q, reads, writes)
        b = semof if semof is not None else writes[0]
        if b.ds is None:
            b.ds = {}
        if q.name not in b.ds:
            b.ds[q.name] = [self.new_sem("d_" + b.name), 0]
            self.dsems.append(b.ds[q.name])
        d = b.ds[q.name]
        for fn in fns:
            d[1] += 16
            s = d[0]
            q.ops.append(lambda e, fn=fn, s=s: fn(e).then_inc(s, 16))
        t = (d[0], d[1])
        self._post(t, reads, writes)
        return t

    def barrier(self):
        ts = [q.last() for q in self.qs] + [(d[0], d[1]) for d in self.dsems if d[1]]
        for q in self.qs:
            q.wait(*ts)

    def emit(self):
        with self.nc.Block() as block:
            @block.tensor
            def _(e):
                for f in self.pe.ops:
                    f(e)

            @block.scalar
            def _(e):
                for f in self.act.ops:
                    f(e)

            @block.vector
            def _(e):
                for f in self.dve.ops:
                    f(e)

            @block.gpsimd
            def _(e):
                for f in self.pool.ops:
                    f(e)

            @block.sync
            def _(e):
                for f in self.sp.ops:
                    f(e)


class Arena:
    def __init__(self, t, n, dt):
        self.t, self.n, self.dt, self.off = t, n, dt, 0

    def reset(self, off=0):
        self.off = off

    def take(self, n, shape=None):
        assert self.off + n <= self.n, ("arena overflow", self.dt, self.off, n, self.n)
        v = self.t[:, self.off:self.off + n]
        self.off += (n + 31) // 32 * 32
        if shape is not None:
            names = " ".join("d%d" % i for i in range(len(shape)))
            v = v.rearrange("p (%s) -> p %s" % (names, names), **{"d%d" % i: s for i, s in enumerate(shape)})
        return v


def rope_tables(S):
    t = np.arange(S, dtype=np.int32)
    row = (t // GRID_W).astype(np.float32)
    col = (t % GRID_W).astype(np.float32)
    nf = HD // 4
    inv = (np.float32(10000.0) ** (-np.arange(nf, dtype=np.float32) / np.float32(nf))).astype(np.float32)
    ang = np.concatenate([row[:, None] * inv, col[:, None] * inv], axis=-1).astype(np.float32)
    cos, sin = np.cos(ang).astype(np.float32), np.sin(ang).astype(np.float32)
    cosT = np.zeros((HD, S), np.float32)
    sinT = np.zeros((HD, S), np.float32)
    perm = np.zeros((HD, HD), np.float32)
    for d in range(HD):
        axis, half, f = d // 32, (d % 32) // 16, d % 16
        cosT[d] = cos[:, axis * nf + f]
        if half == 0:
            sinT[d] = -sin[:, axis * nf + f]
            perm[d + 16, d] = 1.0
        else:
            sinT[d] = sin[:, axis * nf + f]
            perm[d - 16, d] = 1.0
    cos2 = np.concatenate([cosT, cosT], 0)
    sin2 = np.concatenate([sinT, sinT], 0)
    perm2 = np.zeros((128, 128), np.float32)
    perm2[:64, :64] = perm
    perm2[64:, 64:] = perm
    return cos2, sin2, perm2


def nbr_patterns(S):
    rows = S // GRID_W
    kr = min(8, rows)
    kc = 16
    NT = S // 128
    pats, pat_idx, chunks = [], {}, []
    qi = np.arange(128)
    kj = np.arange(128)
    qcol = (qi % 64)[None, :]
    kcol = (kj % 64)[:, None]
    col_start = np.clip(qcol - kc // 2, 0, GRID_W - kc)
    col_ok = (kcol >= col_start) & (kcol < col_start + kc)
    dc = np.clip(kcol - qcol, -(kc - 1), kc - 1) + 15
    for p in range(NT):
        lst = []
        for c in range(NT):
            qrow = (2 * p + qi // 64)[None, :]
            krow = (2 * c + kj // 64)[:, None]
            rs = np.clip(qrow - kr // 2, 0, rows - kr)
            ok = (krow >= rs) & (krow < rs + kr) & col_ok
            if not ok.any():
                continue
            dr = np.where(ok, krow - qrow + 7, 0)
            dcc = np.where(ok, dc, 0)
            key = (ok.tobytes(), dr.astype(np.int16).tobytes(), dcc.astype(np.int16).tobytes())
            if key not in pat_idx:
                pat_idx[key] = len(pats)
                pats.append((ok, dr, dcc))
            lst.append((c, pat_idx[key]))
        chunks.append(lst)
    return pats, chunks


def build(cfg, stop_after=99, debug=False):
    S, CT, E, CAP = cfg["S"], cfg["CT"], cfg["E"], cfg["CAP"]
    NT, NCT = S // 128, CT // 128
    NTT = NT + NCT
    BS = min(512, S)
    NB = S // BS
    NSLOT = E * CAP
    NJ = CAP // 128
    pats, bchunks = nbr_patterns(S)
    NPAT = len(pats)
    use_cnt = np.zeros(NPAT, int)
    for lst in bchunks:
        for _, pi in lst:
            use_cnt[pi] += 1
    res_p = list(np.argsort(-use_cnt)[:5])
    res_slot = {int(p): i for i, p in enumerate(res_p)}

    nc = bass.Bass("TRN2", target_bir_lowering=False)

    def din(name, shape, dt=F32):
        return nc.dram_tensor(name, list(shape), dt, kind="ExternalInput").ap()

    x_d = din("x", [S, D]); ctx_d = din("ctx", [CT, D]); cT_d = din("cT", [128, 16])
    wmod_d = din("w_mod", [D, 6 * D]); bmod_d = din("b_mod", [1, 6 * D])
    g_d = [din(n, [1, D]) for n in ("g_pre_mix", "g_post_mix", "g_pre_ffn", "g_post_ffn")]
    win_d = din("w_in", [D, 4352]); sink_d = din("a_sink", [1, 8])
    rpbx_d = din("rpbx", [NPAT, 128, 8, 128]); maskb_d = din("maskb", [NPAT, 128, 128])
    wba_d = din("w_branch_a", [512, D]); wbb_d = din("w_branch_b", [512, D]); wout_d = din("w_out", [D, D])
    wr_d = din("w_router", [D, E]); br_d = din("b_router", [1, E])
    wgu_d = din("w_gate_up", [E, D, 2 * D]); bgu_d = din("b_gate_up", [E, 2 * D])
    wd_d = din("w_down", [E, D, D]); bd_d = din("b_down", [E, D])
    identf_d = din("identf", [128, 128]); tri_d = din("tri", [128, 128], BF16); identb_d = din("identb", [128, 128], BF16)
    mA_d = din("maskA", [2, 128, 128], BF16); perm_d = din("perm2", [128, 128], BF16)
    cos_d = din("cos2", [128, S]); sin_d = din("sin2", [128, S]); base_d = din("baseE", [128, E])
    out_d = nc.dram_tensor("out", [S, D], F32, kind="ExternalOutput").ap()
    xs_d = nc.dram_tensor("xs", [NSLOT + 128, D], BF16, kind="Internal").ap()
    ys_d = nc.dram_tensor("ys", [NSLOT + 128, D], F32, kind="Internal").ap()
    win_v = win_d.rearrange("(k p) n -> p k n", p=128)
    wmod_v = wmod_d.rearrange("(k p) n -> p k n", p=128)

    st = ExitStack()
    with st:
        kb = KB(nc, st)
        pe, act, dve, pool, sp = kb.pe, kb.act, kb.dve, kb.pool, kb.sp

        def dump(name, ap, dt):
            if not debug:
                return
            kb.barrier()
            shp = list(ap.shape)
            flat = [shp[0], int(np.prod(shp[1:]))]
            dd = nc.dram_tensor("dbg_" + name, flat, dt, kind="ExternalOutput").ap()
            src = ap
            if len(shp) == 3:
                dd = dd.rearrange("p (a b) -> p a b", a=shp[1])
            elif len(shp) == 4:
                dd = dd.rearrange("p (a b c) -> p a b c", a=shp[1], b=shp[2])
            kb.dma(sp, lambda e: e.dma_start(out=dd, in_=src), writes=[Buf("dbg_" + name)])
            kb.barrier()
        NBF = 66 * 1024
        NF = 10 * 1024
        AB = Arena(kb.sb("arenaB", [128, NBF], BF16), NBF, BF16)
        AFr = Arena(kb.sb("arenaF", [128, NF], F32), NF, F32)
        P1g = kb.sb("P1g", [128, D], F32); G2 = kb.sb("G2", [128, D], F32)
        sh2 = kb.sb("sh2", [128, D], F32); P2g = kb.sb("P2g", [128, D], F32)
        identb = kb.sb("identb", [128, 128], BF16); identf = kb.sb("identf", [128, 128], F32)
        tri = kb.sb("tri", [128, 128], BF16); onesb = kb.sb("onesb", [128, 128], BF16)
        mA = kb.sb("mA", [128, 2, 128], BF16); perm2 = kb.sb("perm2", [128, 128], BF16)
        esink = kb.sb("esink", [128, 8], F32)
        small = kb.sb("small", [128, 64 + 4 * NTT], F32)
        wts = kb.sb("wts", [128, NT, 4], F32); desti = kb.sb("desti", [128, NT, 4], I32)
        bguT = kb.sb("bguT", [128, 16, E], F32)
        baseE = kb.sb("baseE", [128, E], F32); cb = kb.sb("cb", [128, E], F32); brep = kb.sb("brep", [128, E], F32)
        wr_sb = kb.sb("wr", [128, 8, E], F32)
        junkb = kb.sb("junkb", [128, D], BF16)
        dnt = kb.sb("dnt", [128, 2, 4], F32)
        zerob = kb.sb("zerob", [128, D], BF16); zerof = kb.sb("zerof", [128, D], F32)
        psA = kb.ps("psA", [128, 1024], F32); psB = kb.ps("psB", [128, 1024], F32)
        psC = kb.ps("psC", [128, 1024], F32); psD = kb.ps("psD", [128, 1024], F32)
        bA, bB, bC, bD = Buf("psA"), Buf("psB"), Buf("psC"), Buf("psD")
        banks = []
        bbufs = [Buf("bank%d" % i) for i in range(8)]
        for i, t in enumerate((psA, psB, psC, psD)):
            banks.append(t[:, 0:512]); banks.append(t[:, 512:1024])
        ssq_i = [0]

        def col():
            c = ssq_i[0] % 64
            ssq_i[0] += 1
            return small[:, c:c + 1]

        B_ = lambda n: Buf(n)
        consts = B_("consts")
        cl = [
            (identb, identb_d), (identf, identf_d), (tri, tri_d), (perm2, perm_d),
            (baseE, base_d),
        ]
        kb.dma(sp, [lambda e, o=o, i=i: e.dma_start(out=o[:], in_=i) for o, i in cl] +
               [lambda e: e.dma_start(out=mA[:], in_=mA_d.rearrange("a p q -> p a q")),
                lambda e: e.dma_start(out=esink[:], in_=sink_d.partition_broadcast(128)),
                lambda e: e.dma_start(out=brep[:], in_=br_d.partition_broadcast(128)),
                lambda e: e.dma_start(out=wr_sb[:], in_=wr_d.rearrange("(k p) n -> p k n", p=128))],
               writes=[consts])
        kb.op(dve, lambda e: e.memset(onesb[:], 1.0), writes=[consts])
        kb.op(dve, lambda e: e.memset(cb[:], 0.0), writes=[consts])
        kb.op(dve, lambda e: e.memset(zerob[:], 0.0), writes=[consts])
        kb.op(dve, lambda e: e.memset(zerof[:], 0.0), writes=[consts])
        kb.op(act, lambda e: e.activation(out=esink[:], in_=esink[:], func=AF.Exp), reads=[], writes=[consts])
        xsb, ysb_b = Buf("xs", nowaw=True), Buf("ys", nowaw=True)
        xs_zero, ys_zero = B_("xsz"), B_("ysz")
        AFr.reset()
        bgu_rows = AFr.take(2 * D)
        bgr = B_("bgr")
        kb.dma(sp, lambda e: e.dma_start(out=bgu_rows[0:E, :], in_=bgu_d), writes=[bgr])
        for c in range(16):
            bk = c % 2
            kb.op(pe, lambda e, c=c, bk=bk: e.transpose(banks[bk][:, 0:E], bgu_rows[0:E, c * 128:(c + 1) * 128], identf[0:E, 0:E]),
                  reads=[bgr, consts], writes=[bbufs[bk]])
            if c < 8:
                kb.op(dve, lambda e, c=c, bk=bk: e.tensor_copy(out=bguT[:, c, :], in_=banks[bk][:, 0:E]), reads=[bbufs[bk]], writes=[consts])
            else:
                kb.op(dve, lambda e, c=c, bk=bk: e.tensor_scalar(out=bguT[:, c, :], in0=banks[bk][:, 0:E], scalar1=1.0, scalar2=None, op0=ALU.add),
                      reads=[bbufs[bk]], writes=[consts])
        kb.barrier()
        if stop_after == 0:
            kb.emit()
            return nc

        AB.reset(); AFr.reset()
        sh1 = AFr.take(D); G1 = AFr.take(D); csh1 = AFr.take(D); G1c = AFr.take(D)
        greps = [AFr.take(D) for _ in range(4)]
        bm = [AFr.take(512) for _ in range(2)]
        tmpm = AFr.take(512)
        cTt = AFr.take(16); sil = AFr.take(16)
        silrep = AB.take(16 * 128, [16, 128])
        wm = [AB.take(8 * 512, [8, 512]) for _ in range(2)]
        b_g, b_c, b_sil, b_tmp = B_("greps"), B_("cT"), B_("silrep"), B_("tmpm")
        b_bm = [B_("bm0"), B_("bm1")]; b_wm = [B_("wm0"), B_("wm1")]
        b_mod = B_("modout")
        kb.dma(sp, [lambda e, j=j: e.dma_start(out=greps[j], in_=g_d[j].partition_broadcast(128)) for j in range(4)], writes=[b_g])
        kb.dma(sp, lambda e: e.dma_start(out=cTt, in_=cT_d), writes=[b_c])
        kb.op(act, lambda e: e.activation(out=sil, in_=cTt, func=AF.Silu), reads=[b_c], writes=[b_sil])
        for j in range(16):
            kb.op(dve, lambda e, j=j: e.tensor_copy(out=silrep[:, j, :], in_=sil[:, j:j + 1].to_broadcast([128, 128])),
                  reads=[b_sil], writes=[b_sil])
        gi = 0
        for j in range(6):
            for half in range(2):
                c0 = j * D + half * 512
                s = gi % 2
                gi += 1
                kb.dma(pool, lambda e, s=s, c0=c0: e.dma_start(out=wm[s], in_=wmod_v[:, :, c0:c0 + 512]), writes=[b_wm[s]])
                kb.dma(sp, lambda e, s=s, c0=c0: e.dma_start(out=bm[s], in_=bmod_d[0:1, c0:c0 + 512].partition_broadcast(128)), writes=[b_bm[s]])
                cs = slice(half * 512, half * 512 + 512)
                for which in range(2 if j < 2 else 1):
                    bk = 2 * s + which
                    kb.group(pe, [lambda e, k=k, bk=bk, s=s, which=which: e.matmul(banks[bk], lhsT=silrep[:, which * 8 + k, :], rhs=wm[s][:, k, :],
                                                                                  start=(k == 0), stop=(k == 7)) for k in range(8)],
                             reads=[b_sil, b_wm[s]], writes=[bbufs[bk]])
                    if j == 0 or j == 3:
                        dst = (sh1 if j == 0 else sh2[:]) if which == 0 else csh1
                        kb.op(dve, lambda e, bk=bk, s=s, dst=dst, cs=cs: e.tensor_tensor(out=dst[:, cs], in0=banks[bk], in1=bm[s], op=ALU.add),
                              reads=[bbufs[bk], b_bm[s]], writes=[b_mod])
                    else:
                        kb.op(dve, lambda e, bk=bk, s=s: e.tensor_tensor(out=tmpm, in0=banks[bk], in1=bm[s], op=ALU.add),
                              reads=[bbufs[bk], b_bm[s]], writes=[b_tmp])
                        if j == 1:
                            dst, gr = (G1, greps[0]) if which == 0 else (G1c, greps[0])
                        elif j == 2:
                            dst, gr = P1g[:], greps[1]
                        elif j == 4:
                            dst, gr = G2[:], greps[2]
                        else:
                            dst, gr = P2g[:], greps[3]
                        if j in (1, 4):
                            kb.op(dve, lambda e, dst=dst, gr=gr, cs=cs: e.scalar_tensor_tensor(out=dst[:, cs], in0=tmpm, scalar=1.0, in1=gr[:, cs], op0=ALU.add, op1=ALU.mult),
                                  reads=[b_tmp, b_g], writes=[b_mod])
                        else:
                            kb.op(dve, lambda e, dst=dst, gr=gr, cs=cs: e.tensor_tensor(out=dst[:, cs], in0=tmpm, in1=gr[:, cs], op=ALU.mult),
                                  reads=[b_tmp, b_g], writes=[b_mod])
        kb.barrier()
        if stop_after == 1:
            kb.emit()
            return nc

        AB.reset()
        hxT = AB.take(8 * (S + CT), [8, S + CT])
        yT = [AB.take(4 * S, [4, S]) for _ in range(2)]
        AB_base = AB.off
        AFr.reset(4 * D)
        xt = [AFr.take(D) for _ in range(2)]
        tmpf = AFr.take(D)
        hxb = [AB.take(D) for _ in range(2)]
        b_xt = [B_("xt0"), B_("xt1")]; b_tmpf = B_("tmpf"); b_hxb = [B_("hxb0"), B_("hxb1")]
        b_small = B_("small"); b_hxT = B_("hxT")

        def rms_rstd(src_ap, src_bufs, psum=False):
            c1, c2 = col(), col()
            if psum:
                kb.op(act, lambda e: e.activation(out=junkb[:], in_=src_ap, func=AF.Square, accum_out=c1), reads=list(src_bufs), writes=[b_small])
            else:
                kb.op(dve, lambda e: e.scalar_tensor_tensor(out=junkb[:], in0=src_ap, scalar=1.0, in1=src_ap, op0=ALU.mult, op1=ALU.mult, accum_out=c1),
                      reads=list(src_bufs), writes=[b_small])
            kb.op(dve, lambda e: e.tensor_scalar(out=c2, in0=c1, scalar1=1.0 / D, scalar2=EPS, op0=ALU.mult, op1=ALU.add), reads=[b_small], writes=[b_small])
            kb.op(act, lambda e: e.activation(out=c2, in_=c2, func=AF.Ln), reads=[b_small], writes=[b_small])
            kb.op(act, lambda e: e.activation(out=c2, in_=c2, func=AF.Exp, scale=-0.5), reads=[b_small], writes=[b_small])
            return c2

        tmpf2 = [tmpf, AFr.take(D)]
        b_tmpf2 = [b_tmpf, B_("tmpf1")]

        def p1_a(i):
            s = i % 2
            src = x_d[i * 128:(i + 1) * 128, :] if i < NT else ctx_d[(i - NT) * 128:(i - NT + 1) * 128, :]
            Gm, shm = (G1, sh1) if i < NT else (G1c, csh1)
            kb.dma(sp, lambda e: e.dma_start(out=xt[s], in_=src), writes=[b_xt[s]])
            r = rms_rstd(xt[s], [b_xt[s]])
            kb.op(dve, lambda e: e.scalar_tensor_tensor(out=tmpf2[s], in0=xt[s], scalar=r, in1=Gm, op0=ALU.mult, op1=ALU.mult),
                  reads=[b_xt[s], b_small, b_mod], writes=[b_tmpf2[s]])
            kb.op(pool, lambda e: e.tensor_tensor(out=hxb[s], in0=tmpf2[s], in1=shm, op=ALU.add), reads=[b_tmpf2[s], b_mod], writes=[b_hxb[s]])

        def p1_b(i):
            s = i % 2
            pb = psA if s == 0 else psB
            pbuf = bA if s == 0 else bB
            pbv = pb[:, 0:512].bitcast(BF16)
            kb.group(pe, [lambda e, k=k: e.transpose(pbv[:, k * 128:(k + 1) * 128], hxb[s][:, k * 128:(k + 1) * 128], identb[:]) for k in range(8)],
                     reads=[b_hxb[s], consts], writes=[pbuf])
            kb.op(act, lambda e: e.activation(out=hxT[:, :, i * 128:(i + 1) * 128], in_=pbv.rearrange("p (k t) -> p k t", k=8), func=AF.Copy),
                  reads=[pbuf], writes=[b_hxT])

        p1_a(0)
        for i in range(NTT):
            if i + 1 < NTT:
                p1_a(i + 1)
            p1_b(i)
        kb.barrier()
        dump("hxT", hxT, BF16)
        if stop_after == 2:
            kb.emit()
            return nc

        def attn_branch(kind, hg):
            AB.reset(AB_base); AFr.reset(0)
            if kind == "A":
                heads = list(range(8)); npair = 4; nkt = 2; nv = 2
                qc0, vc0 = 0, 640
            else:
                heads = list(range(4 * hg, 4 * hg + 4)); npair = 2; nkt = 2; nv = 4
                qc0, kc0, vc0 = 768 + hg * 256, 1280 + hg * 256, 1792 + hg * 256
            nh = len(heads)
            Wq = AB.take(8 * npair * 128, [8, npair * 128])
            Wk = AB.take(8 * nkt * 128, [8, nkt * 128])
            Wv = AB.take(8 * nv * 64, [8, nv * 64])
            QT = AB.take(npair * S, [npair, S])
            KT = AB.take(nkt * (S + CT), [nkt, S + CT])
            V = AB.take(NTT * nv * 80, [NTT, nv, 80])
            PT = [AB.take(7 * 128) for _ in range(2)]
            ytile = [AB.take(4 * 64, [4, 64]) for _ in range(2)]
            qf = AB.take(BS)
            b_W, b_QT, b_KT, b_V = B_("W"), B_("QT"), B_("KT"), B_("V")
            b_PT = [B_("PT0"), B_("PT1")]; b_yt = [B_("yt0"), B_("yt1")]; b_qf = B_("qf")
            b_yT = B_("yT")
            fl = [lambda e: e.dma_start(out=Wq, in_=win_v[:, :, qc0:qc0 + npair * 128]),
                  lambda e: e.dma_start(out=Wv, in_=win_v[:, :, vc0:vc0 + nv * 64])]
            if kind == "A":
                Wk4 = Wk.rearrange("p k (a b c) -> p k a b c", a=2, b=2)
                src = win_v[:, :, 512:640].rearrange("p k (a c) -> p k a c", a=2)
                for a_ in range(2):
                    for d_ in range(2):
                        fl.append(lambda e, a_=a_, d_=d_: e.dma_start(out=Wk4[:, :, a_, d_, :], in_=src[:, :, a_, :]))
            else:
                fl.append(lambda e: e.dma_start(out=Wk, in_=win_v[:, :, kc0:kc0 + nkt * 128]))
            kb.dma(pool, fl, writes=[b_W])
            kb.op(dve, lambda e: e.memset(V[:, :, :, 64:65], 1.0), writes=[b_V])
            if kind == "A":
                cos2 = AFr.take(S); sin2 = AFr.take(S)
                t1 = AFr.take(BS); t2 = AFr.take(BS)
                b_cs, b_t1, b_t2 = B_("cs"), B_("t1"), B_("t2")
                kb.dma(sp, [lambda e: e.dma_start(out=cos2, in_=cos_d), lambda e: e.dma_start(out=sin2, in_=sin_d)], writes=[b_cs])
            else:
                bres = AB.take(5 * 4 * 128, [5, 4, 128])
                bdyn = AB.take(5 * 4 * 128, [5, 4, 128])
                bst = AFr.take(4 * 128, [4, 128]); mst = AFr.take(128)
                b_bres, b_bdyn, b_bst = B_("bres"), B_("bdyn"), B_("bst")

                def load_pat(pi, dst, dbuf):
                    kb.dma(sp, [lambda e: e.dma_start(out=bst, in_=rpbx_d[pi, :, 4 * hg:4 * hg + 4, :]),
                                lambda e: e.dma_start(out=mst, in_=maskb_d[pi])], writes=[b_bst])
                    kb.op(dve, lambda e: e.tensor_tensor(out=dst, in0=bst, in1=mst.unsqueeze(1).to_broadcast([128, 4, 128]), op=ALU.add),
                          reads=[b_bst], writes=[dbuf])
                for pi, sl in res_slot.items():
                    load_pat(pi, bres[:, sl, :, :], b_bres)

            if stop_after == 2.1:
                kb.barrier()
                return
            def proj_fm(Wt, ct, col0, N, bk):
                kb.group(pe, [lambda e, k=k: e.matmul(banks[bk][:, 0:N], lhsT=Wt[:, k, ct * 128:(ct + 1) * 128], rhs=hxT[:, k, col0:col0 + N],
                                                      start=(k == 0), stop=(k == 7)) for k in range(8)],
                         reads=[b_W, b_hxT], writes=[bbufs[bk]])

            def rope(bk, bk2, dst, col0, dbuf):
                kb.op(act, lambda e: e.activation(out=qf, in_=banks[bk][:, 0:BS], func=AF.Copy), reads=[bbufs[bk]], writes=[b_qf])
                if stop_after == 2.31:
                    return
                kb.op(pe, lambda e: e.matmul(banks[bk2][:, 0:BS], lhsT=perm2[:], rhs=qf, start=True, stop=True), reads=[b_qf, consts], writes=[bbufs[bk2]])
                if stop_after == 2.32:
                    return
                kb.op(dve, lambda e: e.tensor_tensor(out=t1, in0=banks[bk][:, 0:BS], in1=cos2[:, col0:col0 + BS], op=ALU.mult), reads=[bbufs[bk], b_cs, b_qf], writes=[b_t1])
                if stop_after == 2.33:
                    return
                kb.op(dve, lambda e: e.tensor_tensor(out=t2, in0=banks[bk2][:, 0:BS], in1=sin2[:, col0:col0 + BS], op=ALU.mult), reads=[bbufs[bk2], b_cs], writes=[b_t2])
                if stop_after == 2.34:
                    return
                kb.op(pool, lambda e: e.tensor_tensor(out=dst, in0=t1, in1=t2, op=ALU.add), reads=[b_t1, b_t2], writes=[dbuf])

            n = 0
            for tb in range(NB):
                col0 = tb * BS
                for ct in range(npair):
                    bk = 4 + (n % 2); n += 1
                    proj_fm(Wq, ct, col0, BS, bk)
                    if kind == "A":
                        rope(bk, 6 + (n % 2), QT[:, ct, col0:col0 + BS], col0, b_QT)
                    else:
                        kb.op(act, lambda e, bk=bk, ct=ct, col0=col0: e.activation(out=QT[:, ct, col0:col0 + BS], in_=banks[bk][:, 0:BS], func=AF.Copy, scale=0.125),
                              reads=[bbufs[bk]], writes=[b_QT])
                for ct in range(nkt):
                    bk = 4 + (n % 2); n += 1
                    proj_fm(Wk, ct, col0, BS, bk)
                    if kind == "A":
                        rope(bk, 6 + (n % 2), KT[:, ct, col0:col0 + BS], col0, b_KT)
                    else:
                        kb.op(act, lambda e, bk=bk, ct=ct, col0=col0: e.activation(out=KT[:, ct, col0:col0 + BS], in_=banks[bk][:, 0:BS], func=AF.Copy),
                              reads=[bbufs[bk]], writes=[b_KT])
            for ct in range(nkt):
                bk = 4 + (n % 2); n += 1
                proj_fm(Wk, ct, S, CT, bk)
                kb.op(act, lambda e, bk=bk, ct=ct: e.activation(out=KT[:, ct, S:S + CT], in_=banks[bk][:, 0:CT], func=AF.Copy), reads=[bbufs[bk]], writes=[b_KT])
            for i in range(NTT):
                bk = 4 + (n % 2); n += 1
                kb.group(pe, [lambda e, k=k, bk=bk, i=i: e.matmul(banks[bk][:, 0:nv * 64], lhsT=hxT[:, k, i * 128:(i + 1) * 128], rhs=Wv[:, k, :],
                                                                  start=(k == 0), stop=(k == 7)) for k in range(8)],
                         reads=[b_W, b_hxT], writes=[bbufs[bk]])
                kb.op(dve, lambda e, bk=bk, i=i: e.tensor_copy(out=V[:, i, :, 0:64], in_=banks[bk][:, 0:nv * 64].rearrange("p (a c) -> p a c", a=nv)),
                      reads=[bbufs[bk]], writes=[b_V])

            dump("QT_%s%d" % (kind, hg), QT, BF16); dump("KT_%s%d" % (kind, hg), KT, BF16); dump("V_%s%d" % (kind, hg), V[:, :, :, 0:65], BF16)
            if stop_after == 2.5:
                kb.barrier()
                return
            if kind == "A":
                nrow = NSLOT + 128
                kb.dma(sp, [lambda e, r=r: e.dma_start(out=xs_d[r:r + 128, :], in_=zerob[:]) for r in range(0, nrow, 128)],
                       reads=[consts, b_V], writes=[xs_zero])
                kb.dma(sp, lambda e: e.dma_start(out=ys_d[NSLOT:NSLOT + 128, :], in_=zerof[:]), reads=[consts], writes=[ys_zero])
            SP = [(psA, bA), (psB, bB)]
            units = []

            def mk_unit(u, qt, quad, hq, pre):
                qs = slice(qt * 128, (qt + 1) * 128)
                ob, obuf = banks[4 + (u // 4) % 2], bbufs[4 + (u // 4) % 2]
                hi = quad * 4 + hq
                h = heads[hi]
                half = hi % 2
                ps_ = slice(half * 64, half * 64 + 64)
                pair = hi // 2
                spt, sbuf_ = SP[u % 2]
                pt = PT[u % 2]
                st8 = {}

                def scores():
                    if kind == "B" and pre:
                        dyn_slot = {}
                        for c, pi in bchunks[qt]:
                            if pi not in res_slot and pi not in dyn_slot:
                                dyn_slot[pi] = len(dyn_slot)
                                load_pat(pi, bdyn[:, dyn_slot[pi], :, :], b_bdyn)
                    if kind == "A":
                        kti, vi = h // 4, h // 4
                        chunks = []
                        if qt > 0:
                            chunks.append((qt - 1, mA[:, 0, :], consts))
                        chunks.append((qt, None, None))
                        if qt < NT - 1:
                            chunks.append((qt + 1, mA[:, 1, :], consts))
                    else:
                        kti, vi = hi // 2, hi
                        dyn_slot = {}
                        for c, pi in bchunks[qt]:
                            if pi not in res_slot and pi not in dyn_slot:
                                dyn_slot[pi] = len(dyn_slot)
                        chunks = []
                        for c, pi in bchunks[qt]:
                            if pi in res_slot:
                                chunks.append((c, bres[:, res_slot[pi], hi, :], b_bres))
                            else:
                                chunks.append((c, bdyn[:, dyn_slot[pi], hi, :], b_bdyn))
                    for c in range(NCT):
                        chunks.append((NT + c, None, None))
                    ncu = len(chunks)
                    assert ncu <= 7
                    st8["chunks"], st8["vi"], st8["ncu"] = chunks, vi, ncu
                    fns = []
                    rd = [b_QT, b_KT, consts]
                    for ci, (kt_, bias, bb) in enumerate(chunks):
                        o_ = spt[:, ci * 128:(ci + 1) * 128]
                        fns.append(lambda e, o_=o_, kt_=kt_, bias=bias, kti=kti: e.matmul(
                            o_, lhsT=KT[ps_, kti, kt_ * 128:(kt_ + 1) * 128], rhs=QT[ps_, pair, qs], start=True, stop=(bias is None)))
                        if bias is not None:
                            fns.append(lambda e, o_=o_, bias=bias: e.matmul(o_, lhsT=identb[:], rhs=bias, start=False, stop=True))
                            if bb not in rd:
                                rd.append(bb)
                    kb.group(pe, fns, reads=rd, writes=[sbuf_])

                def rest():
                    chunks, vi, ncu = st8["chunks"], st8["vi"], st8["ncu"]
                    kb.op(act, lambda e: e.activation(out=pt[:, 0:ncu * 128], in_=spt[:, 0:ncu * 128], func=AF.Exp, scale=(0.125 if kind == "A" else 1.0)),
                          reads=[sbuf_], writes=[b_PT[u % 2]])
                    kb.group(pe, [lambda e, ci=ci, kt_=kt_: e.matmul(
                        ob[:, hq * 80:hq * 80 + 65], lhsT=pt[:, ci * 128:(ci + 1) * 128], rhs=V[:, kt_, vi, 0:65], start=(ci == 0), stop=(ci == ncu - 1))
                        for ci, (kt_, _, _) in enumerate(chunks)],
                             reads=[b_PT[u % 2], b_V], writes=[obuf] if hq == 0 else [], )
                    if hq > 0:
                        obuf.w = [pe.last()]
                    if hq < 3:
                        return
                    yi = ((u + 1) // 4) % 2
                    ob3 = ob[:, 0:320].rearrange("p (a c) -> p a c", a=4)
                    dn = dnt[:, yi, :]
                    if kind == "A":
                        kb.op(dve, lambda e: e.tensor_tensor(out=dn.unsqueeze(2), in0=ob3[:, :, 64:65], in1=esink[:, quad * 4:quad * 4 + 4].unsqueeze(2), op=ALU.add),
                              reads=[obuf, consts], writes=[b_small])
                        kb.op(dve, lambda e: e.reciprocal(out=dn, in_=dn), reads=[b_small], writes=[b_small])
                    else:
                        kb.op(dve, lambda e: e.reciprocal(out=dn.unsqueeze(2), in_=ob3[:, :, 64:65]), reads=[obuf], writes=[b_small])
                    kb.op(dve, lambda e: e.tensor_tensor(out=ytile[yi], in0=ob3[:, :, 0:64], in1=dn.unsqueeze(2).to_broadcast([128, 4, 64]), op=ALU.mult),
                          reads=[obuf, b_small], writes=[b_yt[yi]])
                    tb_, tbuf = banks[6 + yi], bbufs[6 + yi]
                    tbv = tb_.bitcast(BF16)
                    ytf = ytile[yi].rearrange("p a c -> p (a c)")
                    kb.group(pe, [lambda e, pr=pr: e.transpose(tbv[:, pr * 128:(pr + 1) * 128], ytf[:, pr * 128:(pr + 1) * 128], identb[:]) for pr in range(2)],
                             reads=[b_yt[yi], consts], writes=[tbuf])
                    yTd = yT[0] if kind == "A" else yT[1]
                    p0 = quad * 2 if kind == "A" else hg * 2
                    kb.op(act, lambda e: e.activation(out=yTd[:, p0:p0 + 2, qs], in_=tbv[:, 0:256].rearrange("p (a t) -> p a t", a=2), func=AF.Copy),
                          reads=[tbuf], writes=[b_yT])
                return scores, rest

            u = 0
            for qt in range(NT):
                for quad in range(nh // 4):
                    for hq in range(4):
                        units.append(mk_unit(u, qt, quad, hq, pre=(quad == 0 and hq == 0)))
                        u += 1
            units[0][0]()
            for i in range(len(units)):
                if i + 1 < len(units):
                    units[i + 1][0]()
                units[i][1]()
            kb.barrier()

        attn_branch("A", 0)
        if stop_after in (3, 2.5, 2.6, 2.7, 2.1, 2.2, 2.3, 2.31, 2.32, 2.33, 2.34):
            kb.emit()
            return nc
        attn_branch("B", 0)
        attn_branch("B", 1)
        dump("yaT", yT[0], BF16); dump("ybT", yT[1], BF16)
        if stop_after == 4:
            kb.emit()
            return nc

        AB.reset(AB_base); AFr.reset(0)
        mT = AB.take(8 * S, [8, S])
        AB_3b = AB.off
        wg = [[AB.take(8 * 128, [8, 128]) for _ in range(2)] for _ in range(2)]
        wbr = [[AB.take(4 * 128, [4, 128]) for _ in range(2)] for _ in range(2)]
        sg = [[AB.take(BS) for _ in range(2)] for _ in range(2)]
        m1 = [AFr.take(BS) for _ in range(2)]; m2 = [AFr.take(BS) for _ in range(2)]
        b_wg = [B_("wg0"), B_("wg1")]; b_sg = [[B_("sg00"), B_("sg01")], [B_("sg10"), B_("sg11")]]
        b_mT = B_("mT"); b_m1 = [B_("m10"), B_("m11")]; b_m2 = [B_("m20"), B_("m21")]
        wba_v = wba_d.rearrange("(k p) n -> p k n", p=128); wbb_v = wbb_d.rearrange("(k p) n -> p k n", p=128)

        def load_ct(ct):
            s = ct % 2
            kb.dma(pool, [lambda e: e.dma_start(out=wg[s][0], in_=win_v[:, :, 2304 + ct * 128:2304 + (ct + 1) * 128]),
                          lambda e: e.dma_start(out=wg[s][1], in_=win_v[:, :, 3328 + ct * 128:3328 + (ct + 1) * 128]),
                          lambda e: e.dma_start(out=wbr[s][0], in_=wba_v[:, :, ct * 128:(ct + 1) * 128]),
                          lambda e: e.dma_start(out=wbr[s][1], in_=wbb_v[:, :, ct * 128:(ct + 1) * 128])],
                   writes=[b_wg[s]])

        load_ct(0)
        n3 = 0
        for ct in range(8):
            s = ct % 2
            if ct + 1 < 8:
                load_ct(ct + 1)
            for tb in range(NB):
                cs_ = slice(tb * BS, (tb + 1) * BS)
                u3 = n3 % 2; n3 += 1
                for ab in range(2):
                    bk = 4 * u3 + ab
                    kb.group(pe, [lambda e, k=k, s=s, ab=ab, bk=bk, cs_=cs_: e.matmul(banks[bk][:, 0:BS], lhsT=wg[s][ab][:, k, :], rhs=hxT[:, k, cs_], start=(k == 0), stop=(k == 7))
                                  for k in range(8)], reads=[b_wg[s], b_hxT], writes=[bbufs[bk]])
                    kb.op(act, lambda e, u3=u3, ab=ab, bk=bk: e.activation(out=sg[u3][ab], in_=banks[bk][:, 0:BS], func=AF.Sigmoid), reads=[bbufs[bk]], writes=[b_sg[u3][ab]])
                    bk2 = 4 * u3 + 2 + ab
                    kb.group(pe, [lambda e, k=k, s=s, ab=ab, bk2=bk2, cs_=cs_: e.matmul(banks[bk2][:, 0:BS], lhsT=wbr[s][ab][:, k, :], rhs=yT[ab][:, k, cs_], start=(k == 0), stop=(k == 3))
                                  for k in range(4)], reads=[b_wg[s]], writes=[bbufs[bk2]])
                    mm, bmm = (m1[u3], b_m1[u3]) if ab == 0 else (m2[u3], b_m2[u3])
                    kb.op(dve, lambda e, u3=u3, ab=ab, bk2=bk2, mm=mm: e.tensor_tensor(out=mm, in0=banks[bk2][:, 0:BS], in1=sg[u3][ab], op=ALU.mult),
                          reads=[bbufs[bk2], b_sg[u3][ab]], writes=[bmm])
                kb.op(dve, lambda e, ct=ct, cs_=cs_, u3=u3: e.tensor_tensor(out=mT[:, ct, cs_], in0=m1[u3], in1=m2[u3], op=ALU.add), reads=[b_m1[u3], b_m2[u3]], writes=[b_mT])
        kb.barrier()

        AB.reset(AB_3b); AFr.reset(0)
        Wout = AB.take(8 * D, [8, D])
        hx2b = [AB.take(D) for _ in range(2)]
        Mb = AB.take(E)
        xt3 = [AFr.take(D) for _ in range(2)]
        x1t = [AFr.take(D) for _ in range(2)]
        t3 = AFr.take(D); hx2f = [AFr.take(D) for _ in range(2)]
        hx2T = AFr.take(8 * 128, [8, 128])
        lg = AFr.take(E); mx8 = AFr.take(8); ew = AFr.take(4); dfull = AFr.take(E); spos = AFr.take(E); pen = AFr.take(E)
        junkE = AFr.take(E); destf = AFr.take(4); negm = AFr.take(1); sumw = AFr.take(1)
        b_Wout = B_("Wout")
        b_xt3 = [B_("xt30"), B_("xt31")]; b_x1t = [B_("x1t0"), B_("x1t1")]; b_t3 = B_("t3"); b_hx2f = [B_("hx2f0"), B_("hx2f1")]
        b_hx2b = [B_("hx2b0"), B_("hx2b1")]; b_hx2T = B_("hx2T"); b_r = B_("route"); b_Mb = B_("Mb"); b_cb = B_("cb")
        b_route_out = B_("routeout"); b_outd = Buf("outd", nowaw=True)
        kb.dma(pool, lambda e: e.dma_start(out=Wout, in_=wout_d.rearrange("(k p) n -> p k n", p=128)), writes=[b_Wout])

        def stage1(ti):
            s = ti % 2
            js = slice(ti * 128, (ti + 1) * 128)
            rows = js
            kb.dma(sp, lambda e: e.dma_start(out=xt3[s], in_=x_d[rows, :]), writes=[b_xt3[s]])
            for half in range(2):
                kb.group(pe, [lambda e, k=k, half=half: e.matmul(psC[:, half * 512:(half + 1) * 512], lhsT=mT[:, k, js], rhs=Wout[:, k, half * 512:(half + 1) * 512],
                                                                 start=(k == 0), stop=(k == 7)) for k in range(8)],
                         reads=[b_mT, b_Wout], writes=[bC] if half == 0 else [])
            bC.w = [pe.last()]
            r = rms_rstd(psC[:, :], [bC], psum=True)
            kb.op(dve, lambda e: e.scalar_tensor_tensor(out=t3, in0=psC[:, :], scalar=r, in1=P1g[:], op0=ALU.mult, op1=ALU.mult), reads=[bC, b_small], writes=[b_t3])
            kb.op(dve, lambda e: e.tensor_tensor(out=x1t[s], in0=t3, in1=xt3[s], op=ALU.add), reads=[b_t3, b_xt3[s]], writes=[b_x1t[s]])
            kb.dma(sp, lambda e: e.dma_start(out=out_d[rows, :], in_=x1t[s]), reads=[b_x1t[s]], writes=[b_outd], semof=b_x1t[s])
            r2 = rms_rstd(x1t[s], [b_x1t[s]])
            kb.op(dve, lambda e: e.scalar_tensor_tensor(out=t3, in0=x1t[s], scalar=r2, in1=G2[:], op0=ALU.mult, op1=ALU.mult), reads=[b_x1t[s], b_small], writes=[b_t3])
            kb.op(dve, lambda e: e.tensor_tensor(out=hx2f[s], in0=t3, in1=sh2[:], op=ALU.add), reads=[b_t3], writes=[b_hx2f[s]])
            kb.op(act, lambda e: e.activation(out=hx2b[s], in_=hx2f[s], func=AF.Copy), reads=[b_hx2f[s]], writes=[b_hx2b[s]])

        def stage2(ti):
            s = ti % 2
            kb.group(pe, [lambda e, k=k: e.transpose(psD[:, k * 128:(k + 1) * 128], hx2f[s][:, k * 128:(k + 1) * 128], identf[:]) for k in range(8)],
                     reads=[b_hx2f[s], consts], writes=[bD])
            kb.op(dve, lambda e: e.tensor_copy(out=hx2T, in_=psD[:, :].rearrange("p (k t) -> p k t", k=8)), reads=[bD], writes=[b_hx2T])
            kb.group(pe, [lambda e, k=k: e.matmul(banks[0][:, 0:E], lhsT=hx2T[:, k, :], rhs=wr_sb[:, k, :], start=(k == 0), stop=(k == 7)) for k in range(8)],
                     reads=[b_hx2T, consts], writes=[bbufs[0]])
            R = [b_r]
            kb.op(dve, lambda e: e.tensor_tensor(out=lg, in0=banks[0][:, 0:E], in1=brep[:], op=ALU.add), reads=[bbufs[0], consts], writes=R)
            kb.op(dve, lambda e: e.max(out=mx8, in_=lg), reads=R, writes=R)
            kb.op(dve, lambda e: e.tensor_scalar(out=negm, in0=mx8[:, 0:1], scalar1=-1.0, scalar2=None, op0=ALU.mult), reads=R, writes=R)
            kb.op(act, lambda e: e.activation(out=ew, in_=mx8[:, 0:4], func=AF.Exp, bias=negm, scale=1.0, accum_out=sumw), reads=R, writes=R)
            kb.op(dve, lambda e: e.reciprocal(out=sumw, in_=sumw), reads=R, writes=R)
            kb.op(dve, lambda e: e.tensor_scalar(out=wts[:, ti, :], in0=ew, scalar1=sumw, scalar2=None, op0=ALU.mult), reads=R, writes=[b_route_out])
            kb.op(dve, lambda e: e.tensor_scalar(out=Mb, in0=lg, scalar1=mx8[:, 3:4], scalar2=None, op0=ALU.is_ge), reads=R, writes=[b_Mb])
            kb.op(pe, lambda e: e.matmul(banks[1][:, 0:E], lhsT=tri[:], rhs=Mb, start=True, stop=True), reads=[b_Mb, consts], writes=[bbufs[1]])
            kb.op(pe, lambda e: e.matmul(banks[2][:, 0:E], lhsT=onesb[:], rhs=Mb, start=True, stop=True), reads=[b_Mb, consts], writes=[bbufs[2]])
            kb.op(dve, lambda e: e.tensor_tensor(out=spos, in0=banks[1][:, 0:E], in1=cb[:], op=ALU.add), reads=[bbufs[1], b_cb], writes=R)
            kb.op(dve, lambda e: e.tensor_tensor(out=cb[:], in0=banks[2][:, 0:E], in1=cb[:], op=ALU.add), reads=[bbufs[2], b_r], writes=[b_cb])
            kb.op(dve, lambda e: e.tensor_scalar(out=pen, in0=spos, scalar1=float(CAP), scalar2=1.0e6, op0=ALU.is_ge, op1=ALU.mult), reads=R, writes=R)
            kb.op(dve, lambda e: e.tensor_tensor(out=dfull, in0=spos, in1=baseE[:], op=ALU.add), reads=R + [consts], writes=R)
            kb.op(dve, lambda e: e.tensor_tensor(out=dfull, in0=dfull, in1=pen, op=ALU.add), reads=R, writes=R)
            for k in range(4):
                kb.op(dve, lambda e, k=k: e.scalar_tensor_tensor(out=junkE, in0=lg, scalar=mx8[:, k:k + 1], in1=dfull, op0=ALU.is_equal, op1=ALU.mult,
                                                                 accum_out=destf[:, k:k + 1]), reads=R, writes=R)
            kb.op(dve, lambda e: e.tensor_scalar(out=desti[:, ti, :], in0=destf, scalar1=float(NSLOT), scalar2=None, op0=ALU.min), reads=R, writes=[b_route_out])
            kb.dma(pool, [lambda e, k=k: e.indirect_dma_start(out=xs_d[:, :], out_offset=bass.IndirectOffsetOnAxis(ap=desti[:, ti, k:k + 1], axis=0),
                                                             in_=hx2b[s], in_offset=None)
                          for k in range(4)], reads=[b_hx2b[s], b_route_out, xs_zero], writes=[xsb], semof=b_hx2b[s])

        stage1(0)
        for ti in range(NT):
            if ti + 1 < NT:
                stage1(ti + 1)
            stage2(ti)
        kb.barrier()
        dump("wts", wts[:], F32); dump("desti", desti[:], I32)
        if stop_after == 5:
            kb.emit()
            return nc

        AB.reset(0); AFr.reset(0)
        Wgu = [AB.take(8 * 2 * D, [8, 2 * D]) for _ in range(2)]
        Wd = [AB.take(8 * D, [8, D]) for _ in range(2)]
        XT = [AB.take(8 * CAP, [8, CAP]) for _ in range(2)]
        xrows = AB.take(NJ * D, [NJ, D])
        hT = AB.take(8 * CAP, [8, CAP])
        gt = [AFr.take(CAP) for _ in range(2)]; sgm = [AFr.take(CAP) for _ in range(2)]
        ua = [AFr.take(CAP) for _ in range(2)]; glu = [AFr.take(CAP) for _ in range(2)]
        ysb = [AFr.take(512) for _ in range(4)]
        bdr = [AFr.take(D) for _ in range(2)]
        b_Wgu = [B_("Wgu0"), B_("Wgu1")]; b_Wd = [B_("Wd0"), B_("Wd1")]; b_XT = [B_("XT0"), B_("XT1")]; b_xr, b_hT = B_("xrows"), B_("hT")
        b_gt = [B_("gt0"), B_("gt1")]; b_sgm = [B_("sgm0"), B_("sgm1")]; b_ua = [B_("ua0"), B_("ua1")]; b_glu = [B_("glu0"), B_("glu1")]
        b_ysb = [B_("ysb%d" % i) for i in range(4)]; b_bdr = [B_("bdr0"), B_("bdr1")]

        def load_w(ex):
            s = ex % 2
            wgv = wgu_d[ex].rearrange("(k p) n -> p k n", p=128)
            wdv = wd_d[ex].rearrange("(k p) n -> p k n", p=128)
            kb.dma(pool, [lambda e, q4=q4: e.dma_start(out=Wgu[s][:, 2 * q4:2 * q4 + 2, :], in_=wgv[:, 2 * q4:2 * q4 + 2, :]) for q4 in range(4)],
                   writes=[b_Wgu[s]])
            kb.dma(pool, [lambda e, q2=q2: e.dma_start(out=Wd[s][:, 4 * q2:4 * q2 + 4, :], in_=wdv[:, 4 * q2:4 * q2 + 4, :]) for q2 in range(2)],
                   writes=[b_Wd[s]])
            kb.dma(sp, lambda e: e.dma_start(out=bdr[s], in_=bd_d[ex:ex + 1, :].partition_broadcast(128)), writes=[b_bdr[s]])

        def prep(ex):
            s = ex % 2
            kb.dma(sp, lambda e: e.dma_start(out=xrows, in_=xs_d[ex * CAP:(ex + 1) * CAP, :].rearrange("(j p) d -> p j d", p=128)), reads=[xsb], writes=[b_xr])
            for j in range(NJ):
                bk = j % 2
                pbv = banks[bk].bitcast(BF16)
                kb.group(pe, [lambda e, k=k, j=j, pbv=pbv: e.transpose(pbv[:, k * 128:(k + 1) * 128], xrows[:, j, k * 128:(k + 1) * 128], identb[:]) for k in range(8)],
                         reads=[b_xr, consts], writes=[bbufs[bk]])
                kb.op(act, lambda e, j=j, pbv=pbv: e.activation(out=XT[s][:, :, j * 128:(j + 1) * 128], in_=pbv.rearrange("p (k t) -> p k t", k=8), func=AF.Copy),
                      reads=[bbufs[bk]], writes=[b_XT[s]])

        load_w(0)
        prep(0)
        ny = 0
        for ex in range(E):
            s = ex % 2
            if ex + 1 < E:
                load_w(ex + 1)
            for f in range(8):
                gb_, ub_ = (0, 1) if f % 2 == 0 else (2, 3)
                fs = f % 2
                kb.group(pe, [lambda e, k=k, f=f, s=s, gb_=gb_: e.matmul(banks[gb_][:, 0:CAP], lhsT=Wgu[s][:, k, f * 128:(f + 1) * 128], rhs=XT[s][:, k, :], start=(k == 0), stop=(k == 7))
                              for k in range(8)], reads=[b_Wgu[s], b_XT[s]], writes=[bbufs[gb_]])
                kb.group(pe, [lambda e, k=k, f=f, s=s, ub_=ub_: e.matmul(banks[ub_][:, 0:CAP], lhsT=Wgu[s][:, k, D + f * 128:D + (f + 1) * 128], rhs=XT[s][:, k, :], start=(k == 0), stop=(k == 7))
                              for k in range(8)], reads=[b_Wgu[s], b_XT[s]], writes=[bbufs[ub_]])
                kb.op(dve, lambda e, f=f, ex=ex, gb_=gb_, fs=fs: e.tensor_scalar(out=gt[fs], in0=banks[gb_][:, 0:CAP], scalar1=bguT[:, f, ex:ex + 1], scalar2=7.0, op0=ALU.add, op1=ALU.min),
                      reads=[bbufs[gb_], consts], writes=[b_gt[fs]])
                kb.op(act, lambda e, fs=fs: e.activation(out=sgm[fs], in_=gt[fs], func=AF.Sigmoid, scale=1.702), reads=[b_gt[fs]], writes=[b_sgm[fs]])
                kb.op(dve, lambda e, f=f, ex=ex, ub_=ub_, fs=fs: e.tensor_scalar(out=ua[fs], in0=banks[ub_][:, 0:CAP], scalar1=bguT[:, 8 + f, ex:ex + 1], scalar2=8.0, op0=ALU.add, op1=ALU.min),
                      reads=[bbufs[ub_], consts], writes=[b_ua[fs]])
                kb.op(pool, lambda e, fs=fs: e.tensor_tensor(out=glu[fs], in0=gt[fs], in1=sgm[fs], op=ALU.mult), reads=[b_gt[fs], b_sgm[fs]], writes=[b_glu[fs]])
                kb.op(dve, lambda e, f=f, fs=fs: e.scalar_tensor_tensor(out=hT[:, f, :], in0=ua[fs], scalar=-6.0, in1=glu[fs], op0=ALU.max, op1=ALU.mult), reads=[b_ua[fs], b_glu[fs]], writes=[b_hT])
            if ex + 1 < E:
                prep(ex + 1)
            for j in range(NJ):
                js = slice(j * 128, (j + 1) * 128)
                r0 = ex * CAP + j * 128
                for half in range(2):
                    yb_ = ny % 4; ny += 1
                    bk = 4 + yb_
                    kb.group(pe, [lambda e, k=k, half=half, s=s, js=js, bk=bk: e.matmul(banks[bk], lhsT=hT[:, k, js], rhs=Wd[s][:, k, half * 512:(half + 1) * 512],
                                                                                      start=(k == 0), stop=(k == 7)) for k in range(8)],
                             reads=[b_hT, b_Wd[s]], writes=[bbufs[bk]])
                    kb.op(dve, lambda e, yb_=yb_, s=s, bk=bk, half=half: e.tensor_tensor(out=ysb[yb_], in0=banks[bk], in1=bdr[s][:, half * 512:(half + 1) * 512], op=ALU.add),
                          reads=[bbufs[bk], b_bdr[s]], writes=[b_ysb[yb_]])
                    kb.dma(sp, lambda e, yb_=yb_, r0=r0, half=half: e.dma_start(out=ys_d[r0:r0 + 128, half * 512:(half + 1) * 512], in_=ysb[yb_]), reads=[b_ysb[yb_]], writes=[ysb_b], semof=b_ysb[yb_])
        kb.barrier()
        if stop_after == 6:
            kb.emit()
            return nc

        AFr.reset(0); AB.reset(0)
        Yk = [[AB.take(2 * D).bitcast(F32) for _ in range(4)] for _ in range(2)]
        x1r = [AFr.take(D) for _ in range(2)]; acc = AFr.take(D); ot = [AFr.take(D) for _ in range(2)]
        b_Yk = [[B_("Yk%d%d" % (st_, k)) for k in range(4)] for st_ in range(2)]
        b_x1r = [B_("x1r0"), B_("x1r1")]; b_acc = B_("acc"); b_ot = [B_("ot0"), B_("ot1")]
        b_fin = Buf("fin", nowaw=True)

        def gath(ti):
            st_ = ti % 2
            rows = slice(ti * 128, (ti + 1) * 128)
            for k in range(4):
                kb.dma(pool, lambda e, k=k: e.indirect_dma_start(out=Yk[st_][k], out_offset=None, in_=ys_d[:, :],
                                                                 in_offset=bass.IndirectOffsetOnAxis(ap=desti[:, ti, k:k + 1], axis=0)),
                       reads=[ysb_b, ys_zero, b_route_out], writes=[b_Yk[st_][k]])
            kb.dma(sp, lambda e: e.dma_start(out=x1r[st_], in_=out_d[rows, :]), reads=[b_outd], writes=[b_x1r[st_]])

        gath(0)
        for ti in range(NT):
            st_ = ti % 2
            rows = slice(ti * 128, (ti + 1) * 128)
            if ti + 1 < NT:
                gath(ti + 1)
            kb.op(dve, lambda e, ti=ti, st_=st_: e.tensor_scalar(out=acc, in0=Yk[st_][0], scalar1=wts[:, ti, 0:1], scalar2=None, op0=ALU.mult), reads=[b_Yk[st_][0], b_route_out], writes=[b_acc])
            for k in range(1, 4):
                kb.op(dve, lambda e, ti=ti, k=k, st_=st_: e.scalar_tensor_tensor(out=acc, in0=Yk[st_][k], scalar=wts[:, ti, k:k + 1], in1=acc, op0=ALU.mult, op1=ALU.add),
                      reads=[b_Yk[st_][k], b_route_out, b_acc], writes=[b_acc])
            r = rms_rstd(acc, [b_acc])
            kb.op(dve, lambda e, r=r: e.scalar_tensor_tensor(out=acc, in0=acc, scalar=r, in1=P2g[:], op0=ALU.mult, op1=ALU.mult), reads=[b_acc, b_small], writes=[b_acc])
            kb.op(dve, lambda e, st_=st_: e.tensor_tensor(out=ot[st_], in0=acc, in1=x1r[st_], op=ALU.add), reads=[b_acc, b_x1r[st_]], writes=[b_ot[st_]])
            kb.dma(sp, lambda e, rows=rows, st_=st_: e.dma_start(out=out_d[rows, :], in_=ot[st_]), reads=[b_ot[st_], b_x1r[st_]], writes=[b_fin], semof=b_ot[st_])
        kb.barrier()
        kb.emit()
    return nc


def host_inputs(cfg, inp, b):
    S, CT, E, CAP = cfg["S"], cfg["CT"], cfg["E"], cfg["CAP"]
    f = np.float32
    c = np.asarray(inp["c"][b], f); cc = np.asarray(inp["c_ctx"], f)
    cT = np.concatenate([c.reshape(8, 128).T, cc.reshape(8, 128).T], axis=1)
    pats, _ = nbr_patterns(S)
    rpb = np.asarray(inp["b_rpb"][0], f)
    rpbx = np.stack([rpb[:, dr, dc].transpose(1, 0, 2) for ok, dr, dc in pats]).astype(f)
    maskb = np.stack([np.where(ok, 0.0, NEG) for ok, dr, dc in pats]).astype(f)
    cos2, sin2, perm2 = rope_tables(S)
    kj = np.arange(128)[:, None]; qi = np.arange(128)[None, :]
    maskA = np.stack([np.where(qi <= kj, 0.0, NEG), np.where(kj <= qi, 0.0, NEG)]).astype(ml_dtypes.bfloat16)
    tri = (kj < qi).astype(ml_dtypes.bfloat16)
    return {
        "x": np.ascontiguousarray(inp["x"][b], f), "ctx": np.ascontiguousarray(inp["ctx"][b], f), "cT": np.ascontiguousarray(cT, f),
        "w_mod": np.asarray(inp["w_mod"][0], f), "b_mod": np.asarray(inp["b_mod"], f).reshape(1, -1),
        "g_pre_mix": np.asarray(inp["g_pre_mix"], f).reshape(1, -1), "g_post_mix": np.asarray(inp["g_post_mix"], f).reshape(1, -1),
        "g_pre_ffn": np.asarray(inp["g_pre_ffn"], f).reshape(1, -1), "g_post_ffn": np.asarray(inp["g_post_ffn"], f).reshape(1, -1),
        "w_in": np.asarray(inp["w_in"][0], f), "a_sink": np.asarray(inp["a_sink"], f).reshape(1, 8),
        "rpbx": rpbx, "maskb": maskb,
        "w_branch_a": np.asarray(inp["w_branch_a"][0], f), "w_branch_b": np.asarray(inp["w_branch_b"][0], f), "w_out": np.asarray(inp["w_out"][0], f),
        "w_router": np.asarray(inp["w_router"][0], f), "b_router": np.asarray(inp["b_router"], f).reshape(1, -1),
        "w_gate_up": np.asarray(inp["w_gate_up"][0], f), "b_gate_up": np.asarray(inp["b_gate_up"][0], f),
        "w_down": np.asarray(inp["w_down"][0], f), "b_down": np.asarray(inp["b_down"][0], f),
        "identf": np.eye(128, dtype=f), "tri": tri, "identb": np.eye(128).astype(ml_dtypes.bfloat16),
        "maskA": maskA, "perm2": perm2.astype(ml_dtypes.bfloat16), "cos2": cos2, "sin2": sin2,
        "baseE": np.tile((np.arange(E, dtype=f) * CAP)[None, :], (128, 1)).astype(f),
    }


def kernel(**inputs):
    cfg = FULL
    nb = inputs["x"].shape[0]
    nc = build(cfg)
    in_maps = [host_inputs(cfg, inputs, b) for b in range(nb)]
    res = run_bass_kernel_spmd(nc, in_maps, core_ids=list(range(nb)))
    return np.stack([np.asarray(r["out"], np.float32) for r in res.results], axis=0)
```

```python
import numpy as np
import ml_dtypes
from contextlib import ExitStack
import concourse.bass as bass
import concourse.mybir as mybir
from concourse.bass_utils import run_bass_kernel_spmd

F32 = mybir.dt.float32
BF16 = mybir.dt.bfloat16
I32 = mybir.dt.int32
ALU = mybir.AluOpType
AF = mybir.ActivationFunctionType

D = 1024
GRID_W = 64
HD = 64
TOPK = 4
NEG = -30000.0
EPS = 1e-6
FULL = dict(S=2048, CT=256, E=32, CAP=512)


class Q:
    def __init__(self, kb, name):
        self.name = name
        self.ops = []
        self.sem = kb.new_sem("q_" + name)
        self.count = 0
        self.seen = {}

    def wait(self, *tickets):
        for t in tickets:
            if t is None:
                continue
            sem, val = t
            key = id(sem)
            if self.seen.get(key, 0) >= val:
                continue
            self.seen[key] = val
            self.ops.append(lambda e, sem=sem, val=val: e.wait_ge(sem, val))

    def last(self):
        return (self.sem, self.count) if self.count else None


class Buf:
    def __init__(self, name, nowaw=False):
        self.name = name
        self.w = []
        self.r = []
        self.ds = None
        self.nowaw = nowaw


class KB:
    def __init__(self, nc, stack):
        self.nc = nc
        self.stack = stack
        self.dsems = []
        self.pe, self.act, self.dve, self.pool, self.sp = (Q(self, n) for n in ("pe", "act", "dve", "pool", "sp"))
        self.qs = [self.pe, self.act, self.dve, self.pool, self.sp]

    def new_sem(self, name):
        self.nsem = getattr(self, "nsem", 0) + 1
        return self.stack.enter_context(self.nc.semaphore("%s_%d" % (name, self.nsem)))

    def sb(self, name, shape, dt):
        return self.stack.enter_context(self.nc.sbuf_tensor("s_" + name, shape, dt))

    def ps(self, name, shape, dt):
        return self.stack.enter_context(self.nc.psum_tensor(name, shape, dt))

    def _pre(self, q, reads, writes):
        for b in reads:
            q.wait(*b.w)
        for b in writes:
            if not b.nowaw:
                q.wait(*b.w)
            q.wait(*b.r)

    def _post(self, t, reads, writes):
        for b in reads:
            b.r.append(t)
        for b in writes:
            if b.nowaw:
                b.w = [x for x in b.w if x[0] is not t[0]] + [t]
            else:
                b.w = [t]
            b.r = []

    def op(self, q, fn, reads=(), writes=()):
        self._pre(q, reads, writes)
        q.count += 1
        sem = q.sem
        q.ops.append(lambda e, fn=fn, sem=sem: fn(e).then_inc(sem, 1))
        t = (sem, q.count)
        self._post(t, reads, writes)
        return t

    def group(self, q, fns, reads=(), writes=()):
        self._pre(q, reads, writes)
        for fn in fns[:-1]:
            q.ops.append(lambda e, fn=fn: fn(e))
        q.count += 1
        sem = q.sem
        fn = fns[-1]
        q.ops.append(lambda e, fn=fn, sem=sem: fn(e).then_inc(sem, 1))
        t = (sem, q.count)
        self._post(t, reads, writes)
        return t

    def dma(self, q, fns, reads=(), writes=(), semof=None):
        if not isinstance(fns, (list, tuple)):
            fns = [fns]
        self._pre(q, reads, writes)
        b = semof if semof is not None else writes[0]
        if b.ds is None:
            b.ds = {}
        if q.name not in b.ds:
            b.ds[q.name] = [self.new_sem("d_" + b.name), 0]
            self.dsems.append(b.ds[q.name])
        d = b.ds[q.name]
        for fn in fns:
            d[1] += 16
            s = d[0]
            q.ops.append(lambda e, fn=fn, s=s: fn(e).then_inc(s, 16))
        t = (d[0], d[1])
        self._post(t, reads, writes)
        return t

    def barrier(self):
        ts = [q.last() for q in self.qs] + [(d[0], d[1]) for d in self.dsems if d[1]]
        for q in self.qs:
            q.wait(*ts)

    def emit(self):
        with self.nc.Block() as block:
            @block.tensor
            def _(e):
                for f in self.pe.ops:
                    f(e)

            @block.scalar
            def _(e):
                for f in self.act.ops:
                    f(e)

            @block.vector
            def _(e):
                for f in self.dve.ops:
                    f(e)

            @block.gpsimd
            def _(e):
                for f in self.pool.ops:
                    f(e)

            @block.sync
            def _(e):
                for f in self.sp.ops:
                    f(e)


class Arena:
    def __init__(self, t, n, dt):
        self.t, self.n, self.dt, self.off = t, n, dt, 0

    def reset(self, off=0):
        self.off = off

    def take(self, n, shape=None):
        assert self.off + n <= self.n, ("arena overflow", self.dt, self.off, n, self.n)
        v = self.t[:, self.off:self.off + n]
        self.off += (n + 31) // 32 * 32
        if shape is not None:
            names = " ".join("d%d" % i for i in range(len(shape)))
            v = v.rearrange("p (%s) -> p %s" % (names, names), **{"d%d" % i: s for i, s in enumerate(shape)})
        return v


def rope_tables(S):
    t = np.arange(S, dtype=np.int32)
    row = (t // GRID_W).astype(np.float32)
    col = (t % GRID_W).astype(np.float32)
    nf = HD // 4
    inv = (np.float32(10000.0) ** (-np.arange(nf, dtype=np.float32) / np.float32(nf))).astype(np.float32)
    ang = np.concatenate([row[:, None] * inv, col[:, None] * inv], axis=-1).astype(np.float32)
    cos, sin = np.cos(ang).astype(np.float32), np.sin(ang).astype(np.float32)
    cosT = np.zeros((HD, S), np.float32)
    sinT = np.zeros((HD, S), np.float32)
    perm = np.zeros((HD, HD), np.float32)
    for d in range(HD):
        axis, half, f = d // 32, (d % 32) // 16, d % 16
        cosT[d] = cos[:, axis * nf + f]
        if half == 0:
            sinT[d] = -sin[:, axis * nf + f]
            perm[d + 16, d] = 1.0
        else:
            sinT[d] = sin[:, axis * nf + f]
            perm[d - 16, d] = 1.0
    cos2 = np.concatenate([cosT, cosT], 0)
    sin2 = np.concatenate([sinT, sinT], 0)
    perm2 = np.zeros((128, 128), np.float32)
    perm2[:64, :64] = perm
    perm2[64:, 64:] = perm
    return cos2, sin2, perm2


def nbr_patterns(S):
    rows = S // GRID_W
    kr = min(8, rows)
    kc = 16
    NT = S // 128
    pats, pat_idx, chunks = [], {}, []
    qi = np.arange(128)
    kj = np.arange(128)
    qcol = (qi % 64)[None, :]
    kcol = (kj % 64)[:, None]
    col_start = np.clip(qcol - kc // 2, 0, GRID_W - kc)
    col_ok = (kcol >= col_start) & (kcol < col_start + kc)
    dc = np.clip(kcol - qcol, -(kc - 1), kc - 1) + 15
    for p in range(NT):
        lst = []
        for c in range(NT):
            qrow = (2 * p + qi // 64)[None, :]
            krow = (2 * c + kj // 64)[:, None]
            rs = np.clip(qrow - kr // 2, 0, rows - kr)
            ok = (krow >= rs) & (krow < rs + kr) & col_ok
            if not ok.any():
                continue
            dr = np.where(ok, krow - qrow + 7, 0)
            dcc = np.where(ok, dc, 0)
            key = (ok.tobytes(), dr.astype(np.int16).tobytes(), dcc.astype(np.int16).tobytes())
            if key not in pat_idx:
                pat_idx[key] = len(pats)
                pats.append((ok, dr, dcc))
            lst.append((c, pat_idx[key]))
        chunks.append(lst)
    return pats, chunks


def build(cfg, stop_after=99, debug=False):
    S, CT, E, CAP = cfg["S"], cfg["CT"], cfg["E"], cfg["CAP"]
    NT, NCT = S // 128, CT // 128
    NTT = NT + NCT
    BS = min(512, S)
    NB = S // BS
    NSLOT = E * CAP
    NJ = CAP // 128
    pats, bchunks = nbr_patterns(S)
    NPAT = len(pats)
    use_cnt = np.zeros(NPAT, int)
    for lst in bchunks:
        for _, pi in lst:
            use_cnt[pi] += 1
    res_p = list(np.argsort(-use_cnt)[:5])
    res_slot = {int(p): i for i, p in enumerate(res_p)}

    nc = bass.Bass("TRN2", target_bir_lowering=False)

    def din(name, shape, dt=F32):
        return nc.dram_tensor(name, list(shape), dt, kind="ExternalInput").ap()

    x_d = din("x", [S, D]); ctx_d = din("ctx", [CT, D]); cT_d = din("cT", [128, 16])
    wmod_d = din("w_mod", [D, 6 * D]); bmod_d = din("b_mod", [1, 6 * D])
    g_d = [din(n, [1, D]) for n in ("g_pre_mix", "g_post_mix", "g_pre_ffn", "g_post_ffn")]
    win_d = din("w_in", [D, 4352]); sink_d = din("a_sink", [1, 8])
    rpbx_d = din("rpbx", [NPAT, 128, 8, 128]); maskb_d = din("maskb", [NPAT, 128, 128])
    wba_d = din("w_branch_a", [512, D]); wbb_d = din("w_branch_b", [512, D]); wout_d = din("w_out", [D, D])
    wr_d = din("w_router", [D, E]); br_d = din("b_router", [1, E])
    wgu_d = din("w_gate_up", [E, D, 2 * D]); bgu_d = din("b_gate_up", [E, 2 * D])
    wd_d = din("w_down", [E, D, D]); bd_d = din("b_down", [E, D])
    identf_d = din("identf", [128, 128]); tri_d = din("tri", [128, 128], BF16); identb_d = din("identb", [128, 128], BF16)
    mA_d = din("maskA", [2, 128, 128], BF16); perm_d = din("perm2", [128, 128], BF16)
    cos_d = din("cos2", [128, S]); sin_d = din("sin2", [128, S]); base_d = din("baseE", [128, E])
    out_d = nc.dram_tensor("out", [S, D], F32, kind="ExternalOutput").ap()
    xs_d = nc.dram_tensor("xs", [NSLOT + 128, D], BF16, kind="Internal").ap()
    ys_d = nc.dram_tensor("ys", [NSLOT + 128, D], F32, kind="Internal").ap()
    win_v = win_d.rearrange("(k p) n -> p k n", p=128)
    wmod_v = wmod_d.rearrange("(k p) n -> p k n", p=128)

    st = ExitStack()
    with st:
        kb = KB(nc, st)
        pe, act, dve, pool, sp = kb.pe, kb.act, kb.dve, kb.pool, kb.sp

        def dump(name, ap, dt):
            if not debug:
                return
            kb.barrier()
            shp = list(ap.shape)
            flat = [shp[0], int(np.prod(shp[1:]))]
            dd = nc.dram_tensor("dbg_" + name, flat, dt, kind="ExternalOutput").ap()
            src = ap
            if len(shp) == 3:
                dd = dd.rearrange("p (a b) -> p a b", a=shp[1])
            elif len(shp) == 4:
                dd = dd.rearrange("p (a b c) -> p a b c", a=shp[1], b=shp[2])
            kb.dma(sp, lambda e: e.dma_start(out=dd, in_=src), writes=[Buf("dbg_" + name)])
            kb.barrier()
        NBF = 66 * 1024
        NF = 10 * 1024
        AB = Arena(kb.sb("arenaB", [128, NBF], BF16), NBF, BF16)
        AFr = Arena(kb.sb("arenaF", [128, NF], F32), NF, F32)
        P1g = kb.sb("P1g", [128, D], F32); G2 = kb.sb("G2", [128, D], F32)
        sh2 = kb.sb("sh2", [128, D], F32); P2g = kb.sb("P2g", [128, D], F32)
        identb = kb.sb("identb", [128, 128], BF16); identf = kb.sb("identf", [128, 128], F32)
        tri = kb.sb("tri", [128, 128], BF16); onesb = kb.sb("onesb", [128, 128], BF16)
        mA = kb.sb("mA", [128, 2, 128], BF16); perm2 = kb.sb("perm2", [128, 128], BF16)
        esink = kb.sb("esink", [128, 8], F32)
        small = kb.sb("small", [128, 64 + 4 * NTT], F32)
        wts = kb.sb("wts", [128, NT, 4], F32); desti = kb.sb("desti", [128, NT, 4], I32)
        bguT = kb.sb("bguT", [128, 16, E], F32)
        baseE = kb.sb("baseE", [128, E], F32); cb = kb.sb("cb", [128, E], F32); brep = kb.sb("brep", [128, E], F32)
        wr_sb = kb.sb("wr", [128, 8, E], F32)
        junkb = kb.sb("junkb", [128, D], BF16)
        dnt = kb.sb("dnt", [128, 2, 4], F32)
        zerob = kb.sb("zerob", [128, D], BF16); zerof = kb.sb("zerof", [128, D], F32)
        psA = kb.ps("psA", [128, 1024], F32); psB = kb.ps("psB", [128, 1024], F32)
        psC = kb.ps("psC", [128, 1024], F32); psD = kb.ps("psD", [128, 1024], F32)
        bA, bB, bC, bD = Buf("psA"), Buf("psB"), Buf("psC"), Buf("psD")
        banks = []
        bbufs = [Buf("bank%d" % i) for i in range(8)]
        for i, t in enumerate((psA, psB, psC, psD)):
            banks.append(t[:, 0:512]); banks.append(t[:, 512:1024])
        ssq_i = [0]

        def col():
            c = ssq_i[0] % 64
            ssq_i[0] += 1
            return small[:, c:c + 1]

        B_ = lambda n: Buf(n)
        consts = B_("consts")
        cl = [
            (identb, identb_d), (identf, identf_d), (tri, tri_d), (perm2, perm_d),
            (baseE, base_d),
        ]
        kb.dma(sp, [lambda e, o=o, i=i: e.dma_start(out=o[:], in_=i) for o, i in cl] +
               [lambda e: e.dma_start(out=mA[:], in_=mA_d.rearrange("a p q -> p a q")),
                lambda e: e.dma_start(out=esink[:], in_=sink_d.partition_broadcast(128)),
                lambda e: e.dma_start(out=brep[:], in_=br_d.partition_broadcast(128)),
                lambda e: e.dma_start(out=wr_sb[:], in_=wr_d.rearrange("(k p) n -> p k n", p=128))],
               writes=[consts])
        kb.op(dve, lambda e: e.memset(onesb[:], 1.0), writes=[consts])
        kb.op(dve, lambda e: e.memset(cb[:], 0.0), writes=[consts])
        kb.op(dve, lambda e: e.memset(zerob[:], 0.0), writes=[consts])
        kb.op(dve, lambda e: e.memset(zerof[:], 0.0), writes=[consts])
        kb.op(act, lambda e: e.activation(out=esink[:], in_=esink[:], func=AF.Exp), reads=[], writes=[consts])
        xsb, ysb_b = Buf("xs", nowaw=True), Buf("ys", nowaw=True)
        xs_zero, ys_zero = B_("xsz"), B_("ysz")
        AFr.reset()
        bgu_rows = AFr.take(2 * D)
        bgr = B_("bgr")
        kb.dma(sp, lambda e: e.dma_start(out=bgu_rows[0:E, :], in_=bgu_d), writes=[bgr])
        for c in range(16):
            bk = c % 2
            kb.op(pe, lambda e, c=c, bk=bk: e.transpose(banks[bk][:, 0:E], bgu_rows[0:E, c * 128:(c + 1) * 128], identf[0:E, 0:E]),
                  reads=[bgr, consts], writes=[bbufs[bk]])
            if c < 8:
                kb.op(dve, lambda e, c=c, bk=bk: e.tensor_copy(out=bguT[:, c, :], in_=banks[bk][:, 0:E]), reads=[bbufs[bk]], writes=[consts])
            else:
                kb.op(dve, lambda e, c=c, bk=bk: e.tensor_scalar(out=bguT[:, c, :], in0=banks[bk][:, 0:E], scalar1=1.0, scalar2=None, op0=ALU.add),
                      reads=[bbufs[bk]], writes=[consts])
        kb.barrier()
        if stop_after == 0:
            kb.emit()
            return nc

        AB.reset(); AFr.reset()
        sh1 = AFr.take(D); G1 = AFr.take(D); csh1 = AFr.take(D); G1c = AFr.take(D)
        greps = [AFr.take(D) for _ in range(4)]
        bm = [AFr.take(512) for _ in range(2)]
        tmpm = AFr.take(512)
        cTt = AFr.take(16); sil = AFr.take(16)
        silrep = AB.take(16 * 128, [16, 128])
        wm = [AB.take(8 * 512, [8, 512]) for _ in range(2)]
        b_g, b_c, b_sil, b_tmp = B_("greps"), B_("cT"), B_("silrep"), B_("tmpm")
        b_bm = [B_("bm0"), B_("bm1")]; b_wm = [B_("wm0"), B_("wm1")]
        b_mod = B_("modout")
        kb.dma(sp, [lambda e, j=j: e.dma_start(out=greps[j], in_=g_d[j].partition_broadcast(128)) for j in range(4)], writes=[b_g])
        kb.dma(sp, lambda e: e.dma_start(out=cTt, in_=cT_d), writes=[b_c])
        kb.op(act, lambda e: e.activation(out=sil, in_=cTt, func=AF.Silu), reads=[b_c], writes=[b_sil])
        for j in range(16):
            kb.op(dve, lambda e, j=j: e.tensor_copy(out=silrep[:, j, :], in_=sil[:, j:j + 1].to_broadcast([128, 128])),
                  reads=[b_sil], writes=[b_sil])
        gi = 0
        for j in range(6):
            for half in range(2):
                c0 = j * D + half * 512
                s = gi % 2
                gi += 1
                kb.dma(pool, lambda e, s=s, c0=c0: e.dma_start(out=wm[s], in_=wmod_v[:, :, c0:c0 + 512]), writes=[b_wm[s]])
                kb.dma(sp, lambda e, s=s, c0=c0: e.dma_start(out=bm[s], in_=bmod_d[0:1, c0:c0 + 512].partition_broadcast(128)), writes=[b_bm[s]])
                cs = slice(half * 512, half * 512 + 512)
                for which in range(2 if j < 2 else 1):
                    bk = 2 * s + which
                    kb.group(pe, [lambda e, k=k, bk=bk, s=s, which=which: e.matmul(banks[bk], lhsT=silrep[:, which * 8 + k, :], rhs=wm[s][:, k, :],
                                                                                  start=(k == 0), stop=(k == 7)) for k in range(8)],
                             reads=[b_sil, b_wm[s]], writes=[bbufs[bk]])
                    if j == 0 or j == 3:
                        dst = (sh1 if j == 0 else sh2[:]) if which == 0 else csh1
                        kb.op(dve, lambda e, bk=bk, s=s, dst=dst, cs=cs: e.tensor_tensor(out=dst[:, cs], in0=banks[bk], in1=bm[s], op=ALU.add),
                              reads=[bbufs[bk], b_bm[s]], writes=[b_mod])
                    else:
                        kb.op(dve, lambda e, bk=bk, s=s: e.tensor_tensor(out=tmpm, in0=banks[bk], in1=bm[s], op=ALU.add),
                              reads=[bbufs[bk], b_bm[s]], writes=[b_tmp])
                        if j == 1:
                            dst, gr = (G1, greps[0]) if which == 0 else (G1c, greps[0])
                        elif j == 2:
                            dst, gr = P1g[:], greps[1]
                        elif j == 4:
                            dst, gr = G2[:], greps[2]
                        else:
                            dst, gr = P2g[:], greps[3]
                        if j in (1, 4):
                            kb.op(dve, lambda e, dst=dst, gr=gr, cs=cs: e.scalar_tensor_tensor(out=dst[:, cs], in0=tmpm, scalar=1.0, in1=gr[:, cs], op0=ALU.add, op1=ALU.mult),
                                  reads=[b_tmp, b_g], writes=[b_mod])
                        else:
                            kb.op(dve, lambda e, dst=dst, gr=gr, cs=cs: e.tensor_tensor(out=dst[:, cs], in0=tmpm, in1=gr[:, cs], op=ALU.mult),
                                  reads=[b_tmp, b_g], writes=[b_mod])
        kb.barrier()
        if stop_after == 1:
            kb.emit()
            return nc

        AB.reset()
        hxT = AB.take(8 * (S + CT), [8, S + CT])
        yT = [AB.take(4 * S, [4, S]) for _ in range(2)]
        AB_base = AB.off
        AFr.reset(4 * D)
        xt = [AFr.take(D) for _ in range(2)]
        tmpf = AFr.take(D)
        hxb = [AB.take(D) for _ in range(2)]
        b_xt = [B_("xt0"), B_("xt1")]; b_tmpf = B_("tmpf"); b_hxb = [B_("hxb0"), B_("hxb1")]
        b_small = B_("small"); b_hxT = B_("hxT")

        def rms_rstd(src_ap, src_bufs, psum=False):
            c1, c2 = col(), col()
            if psum:
                kb.op(act, lambda e: e.activation(out=junkb[:], in_=src_ap, func=AF.Square, accum_out=c1), reads=list(src_bufs), writes=[b_small])
            else:
                kb.op(dve, lambda e: e.scalar_tensor_tensor(out=junkb[:], in0=src_ap, scalar=1.0, in1=src_ap, op0=ALU.mult, op1=ALU.mult, accum_out=c1),
                      reads=list(src_bufs), writes=[b_small])
            kb.op(dve, lambda e: e.tensor_scalar(out=c2, in0=c1, scalar1=1.0 / D, scalar2=EPS, op0=ALU.mult, op1=ALU.add), reads=[b_small], writes=[b_small])
            kb.op(act, lambda e: e.activation(out=c2, in_=c2, func=AF.Ln), reads=[b_small], writes=[b_small])
            kb.op(act, lambda e: e.activation(out=c2, in_=c2, func=AF.Exp, scale=-0.5), reads=[b_small], writes=[b_small])
            return c2

        tmpf2 = [tmpf, AFr.take(D)]
        b_tmpf2 = [b_tmpf, B_("tmpf1")]

        def p1_a(i):
            s = i % 2
            src = x_d[i * 128:(i + 1) * 128, :] if i < NT else ctx_d[(i - NT) * 128:(i - NT + 1) * 128, :]
            Gm, shm = (G1, sh1) if i < NT else (G1c, csh1)
            kb.dma(sp, lambda e: e.dma_start(out=xt[s], in_=src), writes=[b_xt[s]])
            r = rms_rstd(xt[s], [b_xt[s]])
            kb.op(dve, lambda e: e.scalar_tensor_tensor(out=tmpf2[s], in0=xt[s], scalar=r, in1=Gm, op0=ALU.mult, op1=ALU.mult),
                  reads=[b_xt[s], b_small, b_mod], writes=[b_tmpf2[s]])
            kb.op(pool, lambda e: e.tensor_tensor(out=hxb[s], in0=tmpf2[s], in1=shm, op=ALU.add), reads=[b_tmpf2[s], b_mod], writes=[b_hxb[s]])

        def p1_b(i):
            s = i % 2
            pb = psA if s == 0 else psB
            pbuf = bA if s == 0 else bB
            pbv = pb[:, 0:512].bitcast(BF16)
            kb.group(pe, [lambda e, k=k: e.transpose(pbv[:, k * 128:(k + 1) * 128], hxb[s][:, k * 128:(k + 1) * 128], identb[:]) for k in range(8)],
                     reads=[b_hxb[s], consts], writes=[pbuf])
            kb.op(act, lambda e: e.activation(out=hxT[:, :, i * 128:(i + 1) * 128], in_=pbv.rearrange("p (k t) -> p k t", k=8), func=AF.Copy),
                  reads=[pbuf], writes=[b_hxT])

        p1_a(0)
        for i in range(NTT):
            if i + 1 < NTT:
                p1_a(i + 1)
            p1_b(i)
        kb.barrier()
        dump("hxT", hxT, BF16)
        if stop_after == 2:
            kb.emit()
            return nc

        def attn_branch(kind, hg):
            AB.reset(AB_base); AFr.reset(0)
            if kind == "A":
                heads = list(range(8)); npair = 4; nkt = 2; nv = 2
                qc0, vc0 = 0, 640
            else:
                heads = list(range(4 * hg, 4 * hg + 4)); npair = 2; nkt = 2; nv = 4
                qc0, kc0, vc0 = 768 + hg * 256, 1280 + hg * 256, 1792 + hg * 256
            nh = len(heads)
            Wq = AB.take(8 * npair * 128, [8, npair * 128])
            Wk = AB.take(8 * nkt * 128, [8, nkt * 128])
            Wv = AB.take(8 * nv * 64, [8, nv * 64])
            QT = AB.take(npair * S, [npair, S])
            KT = AB.take(nkt * (S + CT), [nkt, S + CT])
            V = AB.take(NTT * nv * 80, [NTT, nv, 80])
            PT = [AB.take(7 * 128) for _ in range(2)]
            ytile = [AB.take(4 * 64, [4, 64]) for _ in range(2)]
            qf = AB.take(BS)
            b_W, b_QT, b_KT, b_V = B_("W"), B_("QT"), B_("KT"), B_("V")
            b_PT = [B_("PT0"), B_("PT1")]; b_yt = [B_("yt0"), B_("yt1")]; b_qf = B_("qf")
            b_yT = B_("yT")
            fl = [lambda e: e.dma_start(out=Wq, in_=win_v[:, :, qc0:qc0 + npair * 128]),
                  lambda e: e.dma_start(out=Wv, in_=win_v[:, :, vc0:vc0 + nv * 64])]
            if kind == "A":
                Wk4 = Wk.rearrange("p k (a b c) -> p k a b c", a=2, b=2)
                src = win_v[:, :, 512:640].rearrange("p k (a c) -> p k a c", a=2)
                for a_ in range(2):
                    for d_ in range(2):
                        fl.append(lambda e, a_=a_, d_=d_: e.dma_start(out=Wk4[:, :, a_, d_, :], in_=src[:, :, a_, :]))
            else:
                fl.append(lambda e: e.dma_start(out=Wk, in_=win_v[:, :, kc0:kc0 + nkt * 128]))
            kb.dma(pool, fl, writes=[b_W])
            kb.op(dve, lambda e: e.memset(V[:, :, :, 64:65], 1.0), writes=[b_V])
            if kind == "A":
                cos2 = AFr.take(S); sin2 = AFr.take(S)
                t1 = AFr.take(BS); t2 = AFr.take(BS)
                b_cs, b_t1, b_t2 = B_("cs"), B_("t1"), B_("t2")
                kb.dma(sp, [lambda e: e.dma_start(out=cos2, in_=cos_d), lambda e: e.dma_start(out=sin2, in_=sin_d)], writes=[b_cs])
            else:
                bres = AB.take(5 * 4 * 128, [5, 4, 128])
                bdyn = AB.take(5 * 4 * 128, [5, 4, 128])
                bst = AFr.take(4 * 128, [4, 128]); mst = AFr.take(128)
                b_bres, b_bdyn, b_bst = B_("bres"), B_("bdyn"), B_("bst")

                def load_pat(pi, dst, dbuf):
                    kb.dma(sp, [lambda e: e.dma_start(out=bst, in_=rpbx_d[pi, :, 4 * hg:4 * hg + 4, :]),
                                lambda e: e.dma_start(out=mst, in_=maskb_d[pi])], writes=[b_bst])
                    kb.op(dve, lambda e: e.tensor_tensor(out=dst, in0=bst, in1=mst.unsqueeze(1).to_broadcast([128, 4, 128]), op=ALU.add),
                          reads=[b_bst], writes=[dbuf])
                for pi, sl in res_slot.items():
                    load_pat(pi, bres[:, sl, :, :], b_bres)

            if stop_after == 2.1:
                kb.barrier()
                return
            def proj_fm(Wt, ct, col0, N, bk):
                kb.group(pe, [lambda e, k=k: e.matmul(banks[bk][:, 0:N], lhsT=Wt[:, k, ct * 128:(ct + 1) * 128], rhs=hxT[:, k, col0:col0 + N],
                                                      start=(k == 0), stop=(k == 7)) for k in range(8)],
                         reads=[b_W, b_hxT], writes=[bbufs[bk]])

            def rope(bk, bk2, dst, col0, dbuf):
                kb.op(act, lambda e: e.activation(out=qf, in_=banks[bk][:, 0:BS], func=AF.Copy), reads=[bbufs[bk]], writes=[b_qf])
                if stop_after == 2.31:
                    return
                kb.op(pe, lambda e: e.matmul(banks[bk2][:, 0:BS], lhsT=perm2[:], rhs=qf, start=True, stop=True), reads=[b_qf, consts], writes=[bbufs[bk2]])
                if stop_after == 2.32:
                    return
                kb.op(dve, lambda e: e.tensor_tensor(out=t1, in0=banks[bk][:, 0:BS], in1=cos2[:, col0:col0 + BS], op=ALU.mult), reads=[bbufs[bk], b_cs, b_qf], writes=[b_t1])
                if stop_after == 2.33:
                    return
                kb.op(dve, lambda e: e.tensor_tensor(out=t2, in0=banks[bk2][:, 0:BS], in1=sin2[:, col0:col0 + BS], op=ALU.mult), reads=[bbufs[bk2], b_cs], writes=[b_t2])
                if stop_after == 2.34:
                    return
                kb.op(pool, lambda e: e.tensor_tensor(out=dst, in0=t1, in1=t2, op=ALU.add), reads=[b_t1, b_t2], writes=[dbuf])

            n = 0
            for tb in range(NB):
                col0 = tb * BS
                for ct in range(npair):
                    bk = 4 + (n % 2); n += 1
                    proj_fm(Wq, ct, col0, BS, bk)
                    if kind == "A":
                        rope(bk, 6 + (n % 2), QT[:, ct, col0:col0 + BS], col0, b_QT)
                    else:
                        kb.op(act, lambda e, bk=bk, ct=ct, col0=col0: e.activation(out=QT[:, ct, col0:col0 + BS], in_=banks[bk][:, 0:BS], func=AF.Copy, scale=0.125),
                              reads=[bbufs[bk]], writes=[b_QT])
                for ct in range(nkt):
                    bk = 4 + (n % 2); n += 1
                    proj_fm(Wk, ct, col0, BS, bk)
                    if kind == "A":
                        rope(bk, 6 + (n % 2), KT[:, ct, col0:col0 + BS], col0, b_KT)
                    else:
                        kb.op(act, lambda e, bk=bk, ct=ct, col0=col0: e.activation(out=KT[:, ct, col0:col0 + BS], in_=banks[bk][:, 0:BS], func=AF.Copy),
                              reads=[bbufs[bk]], writes=[b_KT])
            for ct in range(nkt):
                bk = 4 + (n % 2); n += 1
                proj_fm(Wk, ct, S, CT, bk)
                kb.op(act, lambda e, bk=bk, ct=ct: e.activation(out=KT[:, ct, S:S + CT], in_=banks[bk][:, 0:CT], func=AF.Copy), reads=[bbufs[bk]], writes=[b_KT])
            for i in range(NTT):
                bk = 4 + (n % 2); n += 1
                kb.group(pe, [lambda e, k=k, bk=bk, i=i: e.matmul(banks[bk][:, 0:nv * 64], lhsT=hxT[:, k, i * 128:(i + 1) * 128], rhs=Wv[:, k, :],
                                                                  start=(k == 0), stop=(k == 7)) for k in range(8)],
                         reads=[b_W, b_hxT], writes=[bbufs[bk]])
                kb.op(dve, lambda e, bk=bk, i=i: e.tensor_copy(out=V[:, i, :, 0:64], in_=banks[bk][:, 0:nv * 64].rearrange("p (a c) -> p a c", a=nv)),
                      reads=[bbufs[bk]], writes=[b_V])

            dump("QT_%s%d" % (kind, hg), QT, BF16); dump("KT_%s%d" % (kind, hg), KT, BF16); dump("V_%s%d" % (kind, hg), V[:, :, :, 0:65], BF16)
            if stop_after == 2.5:
                kb.barrier()
                return
            if kind == "A":
                nrow = NSLOT + 128
                kb.dma(sp, [lambda e, r=r: e.dma_start(out=xs_d[r:r + 128, :], in_=zerob[:]) for r in range(0, nrow, 128)],
                       reads=[consts, b_V], writes=[xs_zero])
                kb.dma(sp, lambda e: e.dma_start(out=ys_d[NSLOT:NSLOT + 128, :], in_=zerof[:]), reads=[consts], writes=[ys_zero])
            SP = [(psA, bA), (psB, bB)]
            units = []

            def mk_unit(u, qt, quad, hq, pre):
                qs = slice(qt * 128, (qt + 1) * 128)
                ob, obuf = banks[4 + (u // 4) % 2], bbufs[4 + (u // 4) % 2]
                hi = quad * 4 + hq
                h = heads[hi]
                half = hi % 2
                ps_ = slice(half * 64, half * 64 + 64)
                pair = hi // 2
                spt, sbuf_ = SP[u % 2]
                pt = PT[u % 2]
                st8 = {}

                def scores():
                    if kind == "B" and pre:
                        dyn_slot = {}
                        for c, pi in bchunks[qt]:
                            if pi not in res_slot and pi not in dyn_slot:
                                dyn_slot[pi] = len(dyn_slot)
                                load_pat(pi, bdyn[:, dyn_slot[pi], :, :], b_bdyn)
                    if kind == "A":
                        kti, vi = h // 4, h // 4
                        chunks = []
                        if qt > 0:
                            chunks.append((qt - 1, mA[:, 0, :], consts))
                        chunks.append((qt, None, None))
                        if qt < NT - 1:
                            chunks.append((qt + 1, mA[:, 1, :], consts))
                    else:
                        kti, vi = hi // 2, hi
                        dyn_slot = {}
                        for c, pi in bchunks[qt]:
                            if pi not in res_slot and pi not in dyn_slot:
                                dyn_slot[pi] = len(dyn_slot)
                        chunks = []
                        for c, pi in bchunks[qt]:
                            if pi in res_slot:
                                chunks.append((c, bres[:, res_slot[pi], hi, :], b_bres))
                            else:
                                chunks.append((c, bdyn[:, dyn_slot[pi], hi, :], b_bdyn))
                    for c in range(NCT):
                        chunks.append((NT + c, None, None))
                    ncu = len(chunks)
                    assert ncu <= 7
                    st8["chunks"], st8["vi"], st8["ncu"] = chunks, vi, ncu
                    fns = []
                    rd = [b_QT, b_KT, consts]
                    for ci, (kt_, bias, bb) in enumerate(chunks):
                        o_ = spt[:, ci * 128:(ci + 1) * 128]
                        fns.append(lambda e, o_=o_, kt_=kt_, bias=bias, kti=kti: e.matmul(
                            o_, lhsT=KT[ps_, kti, kt_ * 128:(kt_ + 1) * 128], rhs=QT[ps_, pair, qs], start=True, stop=(bias is None)))
                        if bias is not None:
                            fns.append(lambda e, o_=o_, bias=bias: e.matmul(o_, lhsT=identb[:], rhs=bias, start=False, stop=True))
                            if bb not in rd:
                                rd.append(bb)
                    kb.group(pe, fns, reads=rd, writes=[sbuf_])

                def rest():
                    chunks, vi, ncu = st8["chunks"], st8["vi"], st8["ncu"]
                    kb.op(act, lambda e: e.activation(out=pt[:, 0:ncu * 128], in_=spt[:, 0:ncu * 128], func=AF.Exp, scale=(0.125 if kind == "A" else 1.0)),
                          reads=[sbuf_], writes=[b_PT[u % 2]])
                    kb.group(pe, [lambda e, ci=ci, kt_=kt_: e.matmul(
                        ob[:, hq * 80:hq * 80 + 65], lhsT=pt[:, ci * 128:(ci + 1) * 128], rhs=V[:, kt_, vi, 0:65], start=(ci == 0), stop=(ci == ncu - 1))
                        for ci, (kt_, _, _) in enumerate(chunks)],
                             reads=[b_PT[u % 2], b_V], writes=[obuf] if hq == 0 else [], )
                    if hq > 0:
                        obuf.w = [pe.last()]
                    if hq < 3:
                        return
                    yi = ((u + 1) // 4) % 2
                    ob3 = ob[:, 0:320].rearrange("p (a c) -> p a c", a=4)
                    dn = dnt[:, yi, :]
                    if kind == "A":
                        kb.op(dve, lambda e: e.tensor_tensor(out=dn.unsqueeze(2), in0=ob3[:, :, 64:65], in1=esink[:, quad * 4:quad * 4 + 4].unsqueeze(2), op=ALU.add),
                              reads=[obuf, consts], writes=[b_small])
                        kb.op(dve, lambda e: e.reciprocal(out=dn, in_=dn), reads=[b_small], writes=[b_small])
                    else:
                        kb.op(dve, lambda e: e.reciprocal(out=dn.unsqueeze(2), in_=ob3[:, :, 64:65]), reads=[obuf], writes=[b_small])
                    kb.op(dve, lambda e: e.tensor_tensor(out=ytile[yi], in0=ob3[:, :, 0:64], in1=dn.unsqueeze(2).to_broadcast([128, 4, 64]), op=ALU.mult),
                          reads=[obuf, b_small], writes=[b_yt[yi]])
                    tb_, tbuf = banks[6 + yi], bbufs[6 + yi]
                    tbv = tb_.bitcast(BF16)
                    ytf = ytile[yi].rearrange("p a c -> p (a c)")
                    kb.group(pe, [lambda e, pr=pr: e.transpose(tbv[:, pr * 128:(pr + 1) * 128], ytf[:, pr * 128:(pr + 1) * 128], identb[:]) for pr in range(2)],
                             reads=[b_yt[yi], consts], writes=[tbuf])
                    yTd = yT[0] if kind == "A" else yT[1]
                    p0 = quad * 2 if kind == "A" else hg * 2
                    kb.op(act, lambda e: e.activation(out=yTd[:, p0:p0 + 2, qs], in_=tbv[:, 0:256].rearrange("p (a t) -> p a t", a=2), func=AF.Copy),
                          reads=[tbuf], writes=[b_yT])
                return scores, rest

            u = 0
            for qt in range(NT):
                for quad in range(nh // 4):
                    for hq in range(4):
                        units.append(mk_unit(u, qt, quad, hq, pre=(quad == 0 and hq == 0)))
                        u += 1
            units[0][0]()
            for i in range(len(units)):
                if i + 1 < len(units):
                    units[i + 1][0]()
                units[i][1]()
            kb.barrier()

        attn_branch("A", 0)
        if stop_after in (3, 2.5, 2.6, 2.7, 2.1, 2.2, 2.3, 2.31, 2.32, 2.33, 2.34):
            kb.emit()
            return nc
        attn_branch("B", 0)
        attn_branch("B", 1)
        dump("yaT", yT[0], BF16); dump("ybT", yT[1], BF16)
        if stop_after == 4:
            kb.emit()
            return nc

        AB.reset(AB_base); AFr.reset(0)
        mT = AB.take(8 * S, [8, S])
        AB_3b = AB.off
        wg = [[AB.take(8 * 128, [8, 128]) for _ in range(2)] for _ in range(2)]
        wbr = [[AB.take(4 * 128, [4, 128]) for _ in range(2)] for _ in range(2)]
        sg = [[AB.take(BS) for _ in range(2)] for _ in range(2)]
        m1 = [AFr.take(BS) for _ in range(2)]; m2 = [AFr.take(BS) for _ in range(2)]
        b_wg = [B_("wg0"), B_("wg1")]; b_sg = [[B_("sg00"), B_("sg01")], [B_("sg10"), B_("sg11")]]
        b_mT = B_("mT"); b_m1 = [B_("m10"), B_("m11")]; b_m2 = [B_("m20"), B_("m21")]
        wba_v = wba_d.rearrange("(k p) n -> p k n", p=128); wbb_v = wbb_d.rearrange("(k p) n -> p k n", p=128)

        def load_ct(ct):
            s = ct % 2
            kb.dma(pool, [lambda e: e.dma_start(out=wg[s][0], in_=win_v[:, :, 2304 + ct * 128:2304 + (ct + 1) * 128]),
                          lambda e: e.dma_start(out=wg[s][1], in_=win_v[:, :, 3328 + ct * 128:3328 + (ct + 1) * 128]),
                          lambda e: e.dma_start(out=wbr[s][0], in_=wba_v[:, :, ct * 128:(ct + 1) * 128]),
                          lambda e: e.dma_start(out=wbr[s][1], in_=wbb_v[:, :, ct * 128:(ct + 1) * 128])],
                   writes=[b_wg[s]])

        load_ct(0)
        n3 = 0
        for ct in range(8):
            s = ct % 2
            if ct + 1 < 8:
                load_ct(ct + 1)
            for tb in range(NB):
                cs_ = slice(tb * BS, (tb + 1) * BS)
                u3 = n3 % 2; n3 += 1
                for ab in range(2):
                    bk = 4 * u3 + ab
                    kb.group(pe, [lambda e, k=k, s=s, ab=ab, bk=bk, cs_=cs_: e.matmul(banks[bk][:, 0:BS], lhsT=wg[s][ab][:, k, :], rhs=hxT[:, k, cs_], start=(k == 0), stop=(k == 7))
                                  for k in range(8)], reads=[b_wg[s], b_hxT], writes=[bbufs[bk]])
                    kb.op(act, lambda e, u3=u3, ab=ab, bk=bk: e.activation(out=sg[u3][ab], in_=banks[bk][:, 0:BS], func=AF.Sigmoid), reads=[bbufs[bk]], writes=[b_sg[u3][ab]])
                    bk2 = 4 * u3 + 2 + ab
                    kb.group(pe, [lambda e, k=k, s=s, ab=ab, bk2=bk2, cs_=cs_: e.matmul(banks[bk2][:, 0:BS], lhsT=wbr[s][ab][:, k, :], rhs=yT[ab][:, k, cs_], start=(k == 0), stop=(k == 3))
                                  for k in range(4)], reads=[b_wg[s]], writes=[bbufs[bk2]])
                    mm, bmm = (m1[u3], b_m1[u3]) if ab == 0 else (m2[u3], b_m2[u3])
                    kb.op(dve, lambda e, u3=u3, ab=ab, bk2=bk2, mm=mm: e.tensor_tensor(out=mm, in0=banks[bk2][:, 0:BS], in1=sg[u3][ab], op=ALU.mult),
                          reads=[bbufs[bk2], b_sg[u3][ab]], writes=[bmm])
                kb.op(dve, lambda e, ct=ct, cs_=cs_, u3=u3: e.tensor_tensor(out=mT[:, ct, cs_], in0=m1[u3], in1=m2[u3], op=ALU.add), reads=[b_m1[u3], b_m2[u3]], writes=[b_mT])
        kb.barrier()

        AB.reset(AB_3b); AFr.reset(0)
        Wout = AB.take(8 * D, [8, D])
        hx2b = [AB.take(D) for _ in range(2)]
        Mb = AB.take(E)
        xt3 = [AFr.take(D) for _ in range(2)]
        x1t = [AFr.take(D) for _ in range(2)]
        t3 = AFr.take(D); hx2f = [AFr.take(D) for _ in range(2)]
        hx2T = AFr.take(8 * 128, [8, 128])
        lg = AFr.take(E); mx8 = AFr.take(8); ew = AFr.take(4); dfull = AFr.take(E); spos = AFr.take(E); pen = AFr.take(E)
        junkE = AFr.take(E); destf = AFr.take(4); negm = AFr.take(1); sumw = AFr.take(1)
        b_Wout = B_("Wout")
        b_xt3 = [B_("xt30"), B_("xt31")]; b_x1t = [B_("x1t0"), B_("x1t1")]; b_t3 = B_("t3"); b_hx2f = [B_("hx2f0"), B_("hx2f1")]
        b_hx2b = [B_("hx2b0"), B_("hx2b1")]; b_hx2T = B_("hx2T"); b_r = B_("route"); b_Mb = B_("Mb"); b_cb = B_("cb")
        b_route_out = B_("routeout"); b_outd = Buf("outd", nowaw=True)
        kb.dma(pool, lambda e: e.dma_start(out=Wout, in_=wout_d.rearrange("(k p) n -> p k n", p=128)), writes=[b_Wout])

        def stage1(ti):
            s = ti % 2
            js = slice(ti * 128, (ti + 1) * 128)
            rows = js
            kb.dma(sp, lambda e: e.dma_start(out=xt3[s], in_=x_d[rows, :]), writes=[b_xt3[s]])
            yield
            for half in range(2):
                kb.group(pe, [lambda e, k=k, half=half: e.matmul(psC[:, half * 512:(half + 1) * 512], lhsT=mT[:, k, js], rhs=Wout[:, k, half * 512:(half + 1) * 512],
                                                                 start=(k == 0), stop=(k == 7)) for k in range(8)],
                         reads=[b_mT, b_Wout], writes=[bC] if half == 0 else [])
                yield
            bC.w = [pe.last()]
            r = rms_rstd(psC[:, :], [bC], psum=True)
            kb.op(dve, lambda e: e.scalar_tensor_tensor(out=t3, in0=psC[:, :], scalar=r, in1=P1g[:], op0=ALU.mult, op1=ALU.mult), reads=[bC, b_small], writes=[b_t3])
            yield
            kb.op(dve, lambda e: e.tensor_tensor(out=x1t[s], in0=t3, in1=xt3[s], op=ALU.add), reads=[b_t3, b_xt3[s]], writes=[b_x1t[s]])
            yield
            kb.dma(sp, lambda e: e.dma_start(out=out_d[rows, :], in_=x1t[s]), reads=[b_x1t[s]], writes=[b_outd], semof=b_x1t[s])
            yield
            r2 = rms_rstd(x1t[s], [b_x1t[s]])
            kb.op(dve, lambda e: e.scalar_tensor_tensor(out=t3, in0=x1t[s], scalar=r2, in1=G2[:], op0=ALU.mult, op1=ALU.mult), reads=[b_x1t[s], b_small], writes=[b_t3])
            yield
            kb.op(dve, lambda e: e.tensor_tensor(out=hx2f[s], in0=t3, in1=sh2[:], op=ALU.add), reads=[b_t3], writes=[b_hx2f[s]])
            yield
            kb.op(act, lambda e: e.activation(out=hx2b[s], in_=hx2f[s], func=AF.Copy), reads=[b_hx2f[s]], writes=[b_hx2b[s]])
            yield

        def stage2(ti):
            s = ti % 2
            kb.group(pe, [lambda e, k=k: e.transpose(psD[:, k * 128:(k + 1) * 128], hx2f[s][:, k * 128:(k + 1) * 128], identf[:]) for k in range(8)],
                     reads=[b_hx2f[s], consts], writes=[bD])
            yield
            kb.op(dve, lambda e: e.tensor_copy(out=hx2T, in_=psD[:, :].rearrange("p (k t) -> p k t", k=8)), reads=[bD], writes=[b_hx2T])
            yield
            kb.group(pe, [lambda e, k=k: e.matmul(banks[0][:, 0:E], lhsT=hx2T[:, k, :], rhs=wr_sb[:, k, :], start=(k == 0), stop=(k == 7)) for k in range(8)],
                     reads=[b_hx2T, consts], writes=[bbufs[0]])
            yield
            R = [b_r]
            kb.op(dve, lambda e: e.tensor_tensor(out=lg, in0=banks[0][:, 0:E], in1=brep[:], op=ALU.add), reads=[bbufs[0], consts], writes=R)
            yield
            kb.op(dve, lambda e: e.max(out=mx8, in_=lg), reads=R, writes=R)
            yield
            kb.op(dve, lambda e: e.tensor_scalar(out=negm, in0=mx8[:, 0:1], scalar1=-1.0, scalar2=None, op0=ALU.mult), reads=R, writes=R)
            yield
            kb.op(act, lambda e: e.activation(out=ew, in_=mx8[:, 0:4], func=AF.Exp, bias=negm, scale=1.0, accum_out=sumw), reads=R, writes=R)
            yield
            kb.op(dve, lambda e: e.reciprocal(out=sumw, in_=sumw), reads=R, writes=R)
            yield
            kb.op(dve, lambda e: e.tensor_scalar(out=wts[:, ti, :], in0=ew, scalar1=sumw, scalar2=None, op0=ALU.mult), reads=R, writes=[b_route_out])
            yield
            kb.op(dve, lambda e: e.tensor_scalar(out=Mb, in0=lg, scalar1=mx8[:, 3:4], scalar2=None, op0=ALU.is_ge), reads=R, writes=[b_Mb])
            yield
            kb.op(pe, lambda e: e.matmul(banks[1][:, 0:E], lhsT=tri[:], rhs=Mb, start=True, stop=True), reads=[b_Mb, consts], writes=[bbufs[1]])
            yield
            kb.op(pe, lambda e: e.matmul(banks[2][:, 0:E], lhsT=onesb[:], rhs=Mb, start=True, stop=True), reads=[b_Mb, consts], writes=[bbufs[2]])
            yield
            kb.op(dve, lambda e: e.tensor_tensor(out=spos, in0=banks[1][:, 0:E], in1=cb[:], op=ALU.add), reads=[bbufs[1], b_cb], writes=R)
            yield
            kb.op(dve, lambda e: e.tensor_tensor(out=cb[:], in0=banks[2][:, 0:E], in1=cb[:], op=ALU.add), reads=[bbufs[2], b_r], writes=[b_cb])
            yield
            kb.op(dve, lambda e: e.tensor_scalar(out=pen, in0=spos, scalar1=float(CAP), scalar2=1.0e6, op0=ALU.is_ge, op1=ALU.mult), reads=R, writes=R)
            yield
            kb.op(dve, lambda e: e.tensor_tensor(out=dfull, in0=spos, in1=baseE[:], op=ALU.add), reads=R + [consts], writes=R)
            yield
            kb.op(dve, lambda e: e.tensor_tensor(out=dfull, in0=dfull, in1=pen, op=ALU.add), reads=R, writes=R)
            yield
            for k in range(4):
                kb.op(dve, lambda e, k=k: e.scalar_tensor_tensor(out=junkE, in0=lg, scalar=mx8[:, k:k + 1], in1=dfull, op0=ALU.is_equal, op1=ALU.mult,
                                                                 accum_out=destf[:, k:k + 1]), reads=R, writes=R)
                yield
            kb.op(dve, lambda e: e.tensor_scalar(out=desti[:, ti, :], in0=destf, scalar1=float(NSLOT), scalar2=None, op0=ALU.min), reads=R, writes=[b_route_out])
            yield
            kb.dma(pool, [lambda e, k=k: e.indirect_dma_start(out=xs_d[:, :], out_offset=bass.IndirectOffsetOnAxis(ap=desti[:, ti, k:k + 1], axis=0),
                                                             in_=hx2b[s], in_offset=None)
                          for k in range(4)], reads=[b_hx2b[s], b_route_out, xs_zero], writes=[xsb], semof=b_hx2b[s])
            yield

        def run_g(g):
            for _ in g:
                pass

        def zip_run(g1, g2):
            d1 = d2 = False
            while not (d1 and d2):
                if not d1:
                    try:
                        next(g1)
                    except StopIteration:
                        d1 = True
                if not d2:
                    try:
                        next(g2)
                    except StopIteration:
                        d2 = True

        run_g(stage1(0))
        for ti in range(NT):
            if ti + 1 < NT:
                zip_run(stage1(ti + 1), stage2(ti))
            else:
                run_g(stage2(ti))
        kb.barrier()
        dump("wts", wts[:], F32); dump("desti", desti[:], I32)
        if stop_after == 5:
            kb.emit()
            return nc

        AB.reset(0); AFr.reset(0)
        Wgu = [AB.take(8 * 2 * D, [8, 2 * D]) for _ in range(2)]
        Wd = [AB.take(8 * D, [8, D]) for _ in range(2)]
        XT = [AB.take(8 * CAP, [8, CAP]) for _ in range(2)]
        xrows = AB.take(NJ * D, [NJ, D])
        hT = AB.take(8 * CAP, [8, CAP])
        gt = [AFr.take(CAP) for _ in range(2)]; sgm = [AFr.take(CAP) for _ in range(2)]
        ua = [AFr.take(CAP) for _ in range(2)]; glu = [AFr.take(CAP) for _ in range(2)]
        ysb = [AFr.take(512) for _ in range(4)]
        bdr = [AFr.take(D) for _ in range(2)]
        b_Wgu = [B_("Wgu0"), B_("Wgu1")]; b_Wd = [B_("Wd0"), B_("Wd1")]; b_XT = [B_("XT0"), B_("XT1")]; b_xr, b_hT = B_("xrows"), B_("hT")
        b_gt = [B_("gt0"), B_("gt1")]; b_sgm = [B_("sgm0"), B_("sgm1")]; b_ua = [B_("ua0"), B_("ua1")]; b_glu = [B_("glu0"), B_("glu1")]
        b_ysb = [B_("ysb%d" % i) for i in range(4)]; b_bdr = [B_("bdr0"), B_("bdr1")]

        def load_w(ex):
            s = ex % 2
            wgv = wgu_d[ex].rearrange("(k p) n -> p k n", p=128)
            wdv = wd_d[ex].rearrange("(k p) n -> p k n", p=128)
            kb.dma(pool, [lambda e, q4=q4: e.dma_start(out=Wgu[s][:, 2 * q4:2 * q4 + 2, :], in_=wgv[:, 2 * q4:2 * q4 + 2, :]) for q4 in range(4)],
                   writes=[b_Wgu[s]])
            kb.dma(pool, [lambda e, q2=q2: e.dma_start(out=Wd[s][:, 4 * q2:4 * q2 + 4, :], in_=wdv[:, 4 * q2:4 * q2 + 4, :]) for q2 in range(2)],
                   writes=[b_Wd[s]])
            kb.dma(sp, lambda e: e.dma_start(out=bdr[s], in_=bd_d[ex:ex + 1, :].partition_broadcast(128)), writes=[b_bdr[s]])

        def prep(ex):
            s = ex % 2
            kb.dma(sp, lambda e: e.dma_start(out=xrows, in_=xs_d[ex * CAP:(ex + 1) * CAP, :].rearrange("(j p) d -> p j d", p=128)), reads=[xsb], writes=[b_xr])
            for j in range(NJ):
                bk = j % 2
                pbv = banks[bk].bitcast(BF16)
                kb.group(pe, [lambda e, k=k, j=j, pbv=pbv: e.transpose(pbv[:, k * 128:(k + 1) * 128], xrows[:, j, k * 128:(k + 1) * 128], identb[:]) for k in range(8)],
                         reads=[b_xr, consts], writes=[bbufs[bk]])
                kb.op(act, lambda e, j=j, pbv=pbv: e.activation(out=XT[s][:, :, j * 128:(j + 1) * 128], in_=pbv.rearrange("p (k t) -> p k t", k=8), func=AF.Copy),
                      reads=[bbufs[bk]], writes=[b_XT[s]])

        load_w(0)
        prep(0)
        ny = 0
        for ex in range(E):
            s = ex % 2
            if ex + 1 < E:
                load_w(ex + 1)
            for f in range(8):
                gb_, ub_ = (0, 1) if f % 2 == 0 else (2, 3)
                fs = f % 2
                kb.group(pe, [lambda e, k=k, f=f, s=s, gb_=gb_: e.matmul(banks[gb_][:, 0:CAP], lhsT=Wgu[s][:, k, f * 128:(f + 1) * 128], rhs=XT[s][:, k, :], start=(k == 0), stop=(k == 7))
                              for k in range(8)], reads=[b_Wgu[s], b_XT[s]], writes=[bbufs[gb_]])
                kb.group(pe, [lambda e, k=k, f=f, s=s, ub_=ub_: e.matmul(banks[ub_][:, 0:CAP], lhsT=Wgu[s][:, k, D + f * 128:D + (f + 1) * 128], rhs=XT[s][:, k, :], start=(k == 0), stop=(k == 7))
                              for k in range(8)], reads=[b_Wgu[s], b_XT[s]], writes=[bbufs[ub_]])
                kb.op(dve, lambda e, f=f, ex=ex, gb_=gb_, fs=fs: e.tensor_scalar(out=gt[fs], in0=banks[gb_][:, 0:CAP], scalar1=bguT[:, f, ex:ex + 1], scalar2=7.0, op0=ALU.add, op1=ALU.min),
                      reads=[bbufs[gb_], consts], writes=[b_gt[fs]])
                kb.op(act, lambda e, fs=fs: e.activation(out=sgm[fs], in_=gt[fs], func=AF.Sigmoid, scale=1.702), reads=[b_gt[fs]], writes=[b_sgm[fs]])
                kb.op(dve, lambda e, f=f, ex=ex, ub_=ub_, fs=fs: e.tensor_scalar(out=ua[fs], in0=banks[ub_][:, 0:CAP], scalar1=bguT[:, 8 + f, ex:ex + 1], scalar2=8.0, op0=ALU.add, op1=ALU.min),
                      reads=[bbufs[ub_], consts], writes=[b_ua[fs]])
                kb.op(pool, lambda e, fs=fs: e.tensor_tensor(out=glu[fs], in0=gt[fs], in1=sgm[fs], op=ALU.mult), reads=[b_gt[fs], b_sgm[fs]], writes=[b_glu[fs]])
                kb.op(dve, lambda e, f=f, fs=fs: e.scalar_tensor_tensor(out=hT[:, f, :], in0=ua[fs], scalar=-6.0, in1=glu[fs], op0=ALU.max, op1=ALU.mult), reads=[b_ua[fs], b_glu[fs]], writes=[b_hT])
            if ex + 1 < E:
                prep(ex + 1)
            for j in range(NJ):
                js = slice(j * 128, (j + 1) * 128)
                r0 = ex * CAP + j * 128
                for half in range(2):
                    yb_ = ny % 4; ny += 1
                    bk = 4 + yb_
                    kb.group(pe, [lambda e, k=k, half=half, s=s, js=js, bk=bk: e.matmul(banks[bk], lhsT=hT[:, k, js], rhs=Wd[s][:, k, half * 512:(half + 1) * 512],
                                                                                      start=(k == 0), stop=(k == 7)) for k in range(8)],
                             reads=[b_hT, b_Wd[s]], writes=[bbufs[bk]])
                    kb.op(dve, lambda e, yb_=yb_, s=s, bk=bk, half=half: e.tensor_tensor(out=ysb[yb_], in0=banks[bk], in1=bdr[s][:, half * 512:(half + 1) * 512], op=ALU.add),
                          reads=[bbufs[bk], b_bdr[s]], writes=[b_ysb[yb_]])
                    kb.dma(sp, lambda e, yb_=yb_, r0=r0, half=half: e.dma_start(out=ys_d[r0:r0 + 128, half * 512:(half + 1) * 512], in_=ysb[yb_]), reads=[b_ysb[yb_]], writes=[ysb_b], semof=b_ysb[yb_])
        kb.barrier()
        if stop_after == 6:
            kb.emit()
            return nc

        AFr.reset(0); AB.reset(0)
        Yk = [[AB.take(2 * D).bitcast(F32) for _ in range(4)] for _ in range(2)]
        x1r = [AFr.take(D) for _ in range(2)]; acc = AFr.take(D); ot = [AFr.take(D) for _ in range(2)]
        b_Yk = [[B_("Yk%d%d" % (st_, k)) for k in range(4)] for st_ in range(2)]
        b_x1r = [B_("x1r0"), B_("x1r1")]; b_acc = B_("acc"); b_ot = [B_("ot0"), B_("ot1")]
        b_fin = Buf("fin", nowaw=True)

        def gath(ti):
            st_ = ti % 2
            rows = slice(ti * 128, (ti + 1) * 128)
            for k in range(4):
                kb.dma(pool, lambda e, k=k: e.indirect_dma_start(out=Yk[st_][k], out_offset=None, in_=ys_d[:, :],
                                                                 in_offset=bass.IndirectOffsetOnAxis(ap=desti[:, ti, k:k + 1], axis=0)),
                       reads=[ysb_b, ys_zero, b_route_out], writes=[b_Yk[st_][k]])
            kb.dma(sp, lambda e: e.dma_start(out=x1r[st_], in_=out_d[rows, :]), reads=[b_outd], writes=[b_x1r[st_]])

        gath(0)
        for ti in range(NT):
            st_ = ti % 2
            rows = slice(ti * 128, (ti + 1) * 128)
            if ti + 1 < NT:
                gath(ti + 1)
            kb.op(dve, lambda e, ti=ti, st_=st_: e.tensor_scalar(out=acc, in0=Yk[st_][0], scalar1=wts[:, ti, 0:1], scalar2=None, op0=ALU.mult), reads=[b_Yk[st_][0], b_route_out], writes=[b_acc])
            for k in range(1, 4):
                kb.op(dve, lambda e, ti=ti, k=k, st_=st_: e.scalar_tensor_tensor(out=acc, in0=Yk[st_][k], scalar=wts[:, ti, k:k + 1], in1=acc, op0=ALU.mult, op1=ALU.add),
                      reads=[b_Yk[st_][k], b_route_out, b_acc], writes=[b_acc])
            r = rms_rstd(acc, [b_acc])
            kb.op(dve, lambda e, r=r: e.scalar_tensor_tensor(out=acc, in0=acc, scalar=r, in1=P2g[:], op0=ALU.mult, op1=ALU.mult), reads=[b_acc, b_small], writes=[b_acc])
            kb.op(dve, lambda e, st_=st_: e.tensor_tensor(out=ot[st_], in0=acc, in1=x1r[st_], op=ALU.add), reads=[b_acc, b_x1r[st_]], writes=[b_ot[st_]])
            kb.dma(sp, lambda e, rows=rows, st_=st_: e.dma_start(out=out_d[rows, :], in_=ot[st_]), reads=[b_ot[st_], b_x1r[st_]], writes=[b_fin], semof=b_ot[st_])
        kb.barrier()
        kb.emit()
    return nc


def host_inputs(cfg, inp, b):
    S, CT, E, CAP = cfg["S"], cfg["CT"], cfg["E"], cfg["CAP"]
    f = np.float32
    c = np.asarray(inp["c"][b], f); cc = np.asarray(inp["c_ctx"], f)
    cT = np.concatenate([c.reshape(8, 128).T, cc.reshape(8, 128).T], axis=1)
    pats, _ = nbr_patterns(S)
    rpb = np.asarray(inp["b_rpb"][0], f)
    rpbx = np.stack([rpb[:, dr, dc].transpose(1, 0, 2) for ok, dr, dc in pats]).astype(f)
    maskb = np.stack([np.where(ok, 0.0, NEG) for ok, dr, dc in pats]).astype(f)
    cos2, sin2, perm2 = rope_tables(S)
    kj = np.arange(128)[:, None]; qi = np.arange(128)[None, :]
    maskA = np.stack([np.where(qi <= kj, 0.0, NEG), np.where(kj <= qi, 0.0, NEG)]).astype(ml_dtypes.bfloat16)
    tri = (kj < qi).astype(ml_dtypes.bfloat16)
    return {
        "x": np.ascontiguousarray(inp["x"][b], f), "ctx": np.ascontiguousarray(inp["ctx"][b], f), "cT": np.ascontiguousarray(cT, f),
        "w_mod": np.asarray(inp["w_mod"][0], f), "b_mod": np.asarray(inp["b_mod"], f).reshape(1, -1),
        "g_pre_mix": np.asarray(inp["g_pre_mix"], f).reshape(1, -1), "g_post_mix": np.asarray(inp["g_post_mix"], f).reshape(1, -1),
        "g_pre_ffn": np.asarray(inp["g_pre_ffn"], f).reshape(1, -1), "g_post_ffn": np.asarray(inp["g_post_ffn"], f).reshape(1, -1),
        "w_in": np.asarray(inp["w_in"][0], f), "a_sink": np.asarray(inp["a_sink"], f).reshape(1, 8),
        "rpbx": rpbx, "maskb": maskb,
        "w_branch_a": np.asarray(inp["w_branch_a"][0], f), "w_branch_b": np.asarray(inp["w_branch_b"][0], f), "w_out": np.asarray(inp["w_out"][0], f),
        "w_router": np.asarray(inp["w_router"][0], f), "b_router": np.asarray(inp["b_router"], f).reshape(1, -1),
        "w_gate_up": np.asarray(inp["w_gate_up"][0], f), "b_gate_up": np.asarray(inp["b_gate_up"][0], f),
        "w_down": np.asarray(inp["w_down"][0], f), "b_down": np.asarray(inp["b_down"][0], f),
        "identf": np.eye(128, dtype=f), "tri": tri, "identb": np.eye(128).astype(ml_dtypes.bfloat16),
        "maskA": maskA, "perm2": perm2.astype(ml_dtypes.bfloat16), "cos2": cos2, "sin2": sin2,
        "baseE": np.tile((np.arange(E, dtype=f) * CAP)[None, :], (128, 1)).astype(f),
    }


def kernel(**inputs):
    cfg = FULL
    nb = inputs["x"].shape[0]
    nc = build(cfg)
    in_maps = [host_inputs(cfg, inputs, b) for b in range(nb)]
    res = run_bass_kernel_spmd(nc, in_maps, core_ids=list(range(nb)))
    return np.stack([np.asarray(r["out"], np.float32) for r in res.results], axis=0)
```

```python
import numpy as np
import ml_dtypes
from contextlib import ExitStack
import concourse.bass as bass
import concourse.mybir as mybir
from concourse.bass_utils import run_bass_kernel_spmd

F32 = mybir.dt.float32
BF16 = mybir.dt.bfloat16
I32 = mybir.dt.int32
ALU = mybir.AluOpType
AF = mybir.ActivationFunctionType

D = 1024
GRID_W = 64
HD = 64
TOPK = 4
NEG = -30000.0
EPS = 1e-6
FULL = dict(S=2048, CT=256, E=32, CAP=512)


class Q:
    def __init__(self, kb, name):
        self.name = name
        self.ops = []
        self.sem = kb.new_sem("q_" + name)
        self.count = 0
        self.seen = {}

    def wait(self, *tickets):
        for t in tickets:
            if t is None:
                continue
            sem, val = t
            key = id(sem)
            if self.seen.get(key, 0) >= val:
                continue
            self.seen[key] = val
            self.ops.append(lambda e, sem=sem, val=val: e.wait_ge(sem, val))

    def last(self):
        return (self.sem, self.count) if self.count else None


class Buf:
    def __init__(self, name, nowaw=False):
        self.name = name
        self.w = []
        self.r = []
        self.ds = None
        self.nowaw = nowaw


class KB:
    def __init__(self, nc, stack):
        self.nc = nc
        self.stack = stack
        self.dsems = []
        self.pe, self.act, self.dve, self.pool, self.sp = (Q(self, n) for n in ("pe", "act", "dve", "pool", "sp"))
        self.qs = [self.pe, self.act, self.dve, self.pool, self.sp]

    def new_sem(self, name):
        self.nsem = getattr(self, "nsem", 0) + 1
        return self.stack.enter_context(self.nc.semaphore("%s_%d" % (name, self.nsem)))

    def sb(self, name, shape, dt):
        return self.stack.enter_context(self.nc.sbuf_tensor("s_" + name, shape, dt))

    def ps(self, name, shape, dt):
        return self.stack.enter_context(self.nc.psum_tensor(name, shape, dt))

    def _pre(self, q, reads, writes):
        for b in reads:
            q.wait(*b.w)
        for b in writes:
            if not b.nowaw:
                q.wait(*b.w)
            q.wait(*b.r)

    def _post(self, t, reads, writes):
        for b in reads:
            b.r.append(t)
        for b in writes:
            if b.nowaw:
                b.w = [x for x in b.w if x[0] is not t[0]] + [t]
            else:
                b.w = [t]
            b.r = []

    def op(self, q, fn, reads=(), writes=()):
        self._pre(q, reads, writes)
        q.count += 1
        sem = q.sem
        q.ops.append(lambda e, fn=fn, sem=sem: fn(e).then_inc(sem, 1))
        t = (sem, q.count)
        self._post(t, reads, writes)
        return t

    def group(self, q, fns, reads=(), writes=()):
        self._pre(q, reads, writes)
        for fn in fns[:-1]:
            q.ops.append(lambda e, fn=fn: fn(e))
        q.count += 1
        sem = q.sem
        fn = fns[-1]
        q.ops.append(lambda e, fn=fn, sem=sem: fn(e).then_inc(sem, 1))
        t = (sem, q.count)
        self._post(t, reads, writes)
        return t

    def dma(self, q, fns, reads=(), writes=(), semof=None):
        if not isinstance(fns, (list, tuple)):
            fns = [fns]
        self._pre(q, reads, writes)
        b = semof if semof is not None else writes[0]
        if b.ds is None:
            b.ds = {}
        if q.name not in b.ds:
            b.ds[q.name] = [self.new_sem("d_" + b.name), 0]
            self.dsems.append(b.ds[q.name])
        d = b.ds[q.name]
        for fn in fns:
            d[1] += 16
            s = d[0]
            q.ops.append(lambda e, fn=fn, s=s: fn(e).then_inc(s, 16))
        t = (d[0], d[1])
        self._post(t, reads, writes)
        return t

    def barrier(self):
        ts = [q.last() for q in self.qs] + [(d[0], d[1]) for d in self.dsems if d[1]]
        for q in self.qs:
            q.wait(*ts)

    def emit(self):
        with self.nc.Block() as block:
            @block.tensor
            def _(e):
                for f in self.pe.ops:
                    f(e)

            @block.scalar
            def _(e):
                for f in self.act.ops:
                    f(e)

            @block.vector
            def _(e):
                for f in self.dve.ops:
                    f(e)

            @block.gpsimd
            def _(e):
                for f in self.pool.ops:
                    f(e)

            @block.sync
            def _(e):
                for f in self.sp.ops:
                    f(e)


class Arena:
    def __init__(self, t, n, dt):
        self.t, self.n, self.dt, self.off = t, n, dt, 0

    def reset(self, off=0):
        self.off = off

    def take(self, n, shape=None):
        assert self.off + n <= self.n, ("arena overflow", self.dt, self.off, n, self.n)
        v = self.t[:, self.off:self.off + n]
        self.off += (n + 31) // 32 * 32
        if shape is not None:
            names = " ".join("d%d" % i for i in range(len(shape)))
            v = v.rearrange("p (%s) -> p %s" % (names, names), **{"d%d" % i: s for i, s in enumerate(shape)})
        return v


def rope_tables(S):
    t = np.arange(S, dtype=np.int32)
    row = (t // GRID_W).astype(np.float32)
    col = (t % GRID_W).astype(np.float32)
    nf = HD // 4
    inv = (np.float32(10000.0) ** (-np.arange(nf, dtype=np.float32) / np.float32(nf))).astype(np.float32)
    ang = np.concatenate([row[:, None] * inv, col[:, None] * inv], axis=-1).astype(np.float32)
    cos, sin = np.cos(ang).astype(np.float32), np.sin(ang).astype(np.float32)
    cosT = np.zeros((HD, S), np.float32)
    sinT = np.zeros((HD, S), np.float32)
    perm = np.zeros((HD, HD), np.float32)
    for d in range(HD):
        axis, half, f = d // 32, (d % 32) // 16, d % 16
        cosT[d] = cos[:, axis * nf + f]
        if half == 0:
            sinT[d] = -sin[:, axis * nf + f]
            perm[d + 16, d] = 1.0
        else:
            sinT[d] = sin[:, axis * nf + f]
            perm[d - 16, d] = 1.0
    cos2 = np.concatenate([cosT, cosT], 0)
    sin2 = np.concatenate([sinT, sinT], 0)
    perm2 = np.zeros((128, 128), np.float32)
    perm2[:64, :64] = perm
    perm2[64:, 64:] = perm
    return cos2, sin2, perm2


def nbr_patterns(S):
    rows = S // GRID_W
    kr = min(8, rows)
    kc = 16
    NT = S // 128
    pats, pat_idx, chunks = [], {}, []
    qi = np.arange(128)
    kj = np.arange(128)
    qcol = (qi % 64)[None, :]
    kcol = (kj % 64)[:, None]
    col_start = np.clip(qcol - kc // 2, 0, GRID_W - kc)
    col_ok = (kcol >= col_start) & (kcol < col_start + kc)
    dc = np.clip(kcol - qcol, -(kc - 1), kc - 1) + 15
    for p in range(NT):
        lst = []
        for c in range(NT):
            qrow = (2 * p + qi // 64)[None, :]
            krow = (2 * c + kj // 64)[:, None]
            rs = np.clip(qrow - kr // 2, 0, rows - kr)
            ok = (krow >= rs) & (krow < rs + kr) & col_ok
            if not ok.any():
                continue
            dr = np.where(ok, krow - qrow + 7, 0)
            dcc = np.where(ok, dc, 0)
            key = (ok.tobytes(), dr.astype(np.int16).tobytes(), dcc.astype(np.int16).tobytes())
            if key not in pat_idx:
                pat_idx[key] = len(pats)
                pats.append((ok, dr, dcc))
            lst.append((c, pat_idx[key]))
        chunks.append(lst)
    return pats, chunks


def build(cfg, stop_after=99, debug=False):
    S, CT, E, CAP = cfg["S"], cfg["CT"], cfg["E"], cfg["CAP"]
    NT, NCT = S // 128, CT // 128
    NTT = NT + NCT
    BS = min(512, S)
    NB = S // BS
    NSLOT = E * CAP
    NJ = CAP // 128
    pats, bchunks = nbr_patterns(S)
    NPAT = len(pats)
    use_cnt = np.zeros(NPAT, int)
    for lst in bchunks:
        for _, pi in lst:
            use_cnt[pi] += 1
    res_p = list(np.argsort(-use_cnt)[:5])
    res_slot = {int(p): i for i, p in enumerate(res_p)}

    nc = bass.Bass("TRN2", target_bir_lowering=False)

    def din(name, shape, dt=F32):
        return nc.dram_tensor(name, list(shape), dt, kind="ExternalInput").ap()

    x_d = din("x", [S, D]); ctx_d = din("ctx", [CT, D]); cT_d = din("cT", [128, 16])
    wmod_d = din("w_mod", [D, 6 * D]); bmod_d = din("b_mod", [1, 6 * D])
    g_d = [din(n, [1, D]) for n in ("g_pre_mix", "g_post_mix", "g_pre_ffn", "g_post_ffn")]
    win_d = din("w_in", [D, 4352]); sink_d = din("a_sink", [1, 8])
    rpbx_d = din("rpbx", [NPAT, 128, 8, 128]); maskb_d = din("maskb", [NPAT, 128, 128])
    wba_d = din("w_branch_a", [512, D]); wbb_d = din("w_branch_b", [512, D]); wout_d = din("w_out", [D, D])
    wr_d = din("w_router", [D, E]); br_d = din("b_router", [1, E])
    wgu_d = din("w_gate_up", [E, D, 2 * D]); bgu_d = din("b_gate_up", [E, 2 * D])
    wd_d = din("w_down", [E, D, D]); bd_d = din("b_down", [E, D])
    identf_d = din("identf", [128, 128]); tri_d = din("tri", [128, 128], BF16); identb_d = din("identb", [128, 128], BF16)
    mA_d = din("maskA", [2, 128, 128], BF16); perm_d = din("perm2", [128, 128], BF16)
    cos_d = din("cos2", [128, S]); sin_d = din("sin2", [128, S]); base_d = din("baseE", [128, E]); trash_d = din("trash", [128, 4])
    out_d = nc.dram_tensor("out", [S, D], F32, kind="ExternalOutput").ap()
    xs_d = nc.dram_tensor("xs", [NSLOT + 512, D], BF16, kind="Internal").ap()
    ys_d = nc.dram_tensor("ys", [NSLOT + 512, D], F32, kind="Internal").ap()
    win_v = win_d.rearrange("(k p) n -> p k n", p=128)
    wmod_v = wmod_d.rearrange("(k p) n -> p k n", p=128)

    st = ExitStack()
    with st:
        kb = KB(nc, st)
        pe, act, dve, pool, sp = kb.pe, kb.act, kb.dve, kb.pool, kb.sp

        def dump(name, ap, dt):
            if not debug:
                return
            kb.barrier()
            shp = list(ap.shape)
            flat = [shp[0], int(np.prod(shp[1:]))]
            dd = nc.dram_tensor("dbg_" + name, flat, dt, kind="ExternalOutput").ap()
            src = ap
            if len(shp) == 3:
                dd = dd.rearrange("p (a b) -> p a b", a=shp[1])
            elif len(shp) == 4:
                dd = dd.rearrange("p (a b c) -> p a b c", a=shp[1], b=shp[2])
            kb.dma(sp, lambda e: e.dma_start(out=dd, in_=src), writes=[Buf("dbg_" + name)])
            kb.barrier()
        NBF = 66 * 1024
        NF = 10 * 1024
        AB = Arena(kb.sb("arenaB", [128, NBF], BF16), NBF, BF16)
        AFr = Arena(kb.sb("arenaF", [128, NF], F32), NF, F32)
        P1g = kb.sb("P1g", [128, D], F32); G2 = kb.sb("G2", [128, D], F32)
        sh2 = kb.sb("sh2", [128, D], F32); P2g = kb.sb("P2g", [128, D], F32)
        identb = kb.sb("identb", [128, 128], BF16); identf = kb.sb("identf", [128, 128], F32)
        tri = kb.sb("tri", [128, 128], BF16); onesb = kb.sb("onesb", [128, 128], BF16)
        mA = kb.sb("mA", [128, 2, 128], BF16); perm2 = kb.sb("perm2", [128, 128], BF16)
        esink = kb.sb("esink", [128, 8], F32)
        small = kb.sb("small", [128, 64 + 4 * NTT], F32)
        wts = kb.sb("wts", [128, NT, 4], F32); desti = kb.sb("desti", [128, NT, 4], I32)
        bguT = kb.sb("bguT", [128, 16, E], F32)
        baseE = kb.sb("baseE", [128, E], F32); cb = kb.sb("cb", [128, E], F32); brep = kb.sb("brep", [128, E], F32)
        wr_sb = kb.sb("wr", [128, 8, E], F32)
        trash = kb.sb("trash", [128, 4], F32)
        junkb = kb.sb("junkb", [128, D], BF16)
        dnt = kb.sb("dnt", [128, 2, 4], F32)
        zerob = kb.sb("zerob", [128, D], BF16); zerof = kb.sb("zerof", [128, D], F32)
        psA = kb.ps("psA", [128, 1024], F32); psB = kb.ps("psB", [128, 1024], F32)
        psC = kb.ps("psC", [128, 1024], F32); psD = kb.ps("psD", [128, 1024], F32)
        bA, bB, bC, bD = Buf("psA"), Buf("psB"), Buf("psC"), Buf("psD")
        banks = []
        bbufs = [Buf("bank%d" % i) for i in range(8)]
        for i, t in enumerate((psA, psB, psC, psD)):
            banks.append(t[:, 0:512]); banks.append(t[:, 512:1024])
        ssq_i = [0]

        def col():
            c = ssq_i[0] % 64
            ssq_i[0] += 1
            return small[:, c:c + 1]

        B_ = lambda n: Buf(n)
        consts = B_("consts")
        cl = [
            (identb, identb_d), (identf, identf_d), (tri, tri_d), (perm2, perm_d),
            (baseE, base_d), (trash, trash_d),
        ]
        kb.dma(sp, [lambda e, o=o, i=i: e.dma_start(out=o[:], in_=i) for o, i in cl] +
               [lambda e: e.dma_start(out=mA[:], in_=mA_d.rearrange("a p q -> p a q")),
                lambda e: e.dma_start(out=esink[:], in_=sink_d.partition_broadcast(128)),
                lambda e: e.dma_start(out=brep[:], in_=br_d.partition_broadcast(128)),
                lambda e: e.dma_start(out=wr_sb[:], in_=wr_d.rearrange("(k p) n -> p k n", p=128))],
               writes=[consts])
        kb.op(dve, lambda e: e.memset(onesb[:], 1.0), writes=[consts])
        kb.op(dve, lambda e: e.memset(cb[:], 0.0), writes=[consts])
        kb.op(dve, lambda e: e.memset(zerob[:], 0.0), writes=[consts])
        kb.op(dve, lambda e: e.memset(zerof[:], 0.0), writes=[consts])
        kb.op(act, lambda e: e.activation(out=esink[:], in_=esink[:], func=AF.Exp), reads=[], writes=[consts])
        xsb, ysb_b = Buf("xs", nowaw=True), Buf("ys", nowaw=True)
        xs_zero, ys_zero = B_("xsz"), B_("ysz")
        AFr.reset()
        bgu_rows = AFr.take(2 * D)
        bgr = B_("bgr")
        kb.dma(sp, lambda e: e.dma_start(out=bgu_rows[0:E, :], in_=bgu_d), writes=[bgr])
        for c in range(16):
            bk = c % 2
            kb.op(pe, lambda e, c=c, bk=bk: e.transpose(banks[bk][:, 0:E], bgu_rows[0:E, c * 128:(c + 1) * 128], identf[0:E, 0:E]),
                  reads=[bgr, consts], writes=[bbufs[bk]])
            if c < 8:
                kb.op(dve, lambda e, c=c, bk=bk: e.tensor_copy(out=bguT[:, c, :], in_=banks[bk][:, 0:E]), reads=[bbufs[bk]], writes=[consts])
            else:
                kb.op(dve, lambda e, c=c, bk=bk: e.tensor_scalar(out=bguT[:, c, :], in0=banks[bk][:, 0:E], scalar1=1.0, scalar2=None, op0=ALU.add),
                      reads=[bbufs[bk]], writes=[consts])
        kb.barrier()
        if stop_after == 0:
            kb.emit()
            return nc

        AB.reset(); AFr.reset()
        sh1 = AFr.take(D); G1 = AFr.take(D); csh1 = AFr.take(D); G1c = AFr.take(D)
        greps = [AFr.take(D) for _ in range(4)]
        bm = [AFr.take(512) for _ in range(2)]
        tmpm = AFr.take(512)
        cTt = AFr.take(16); sil = AFr.take(16)
        silrep = AB.take(16 * 128, [16, 128])
        wm = [AB.take(8 * 512, [8, 512]) for _ in range(2)]
        b_g, b_c, b_sil, b_tmp = B_("greps"), B_("cT"), B_("silrep"), B_("tmpm")
        b_bm = [B_("bm0"), B_("bm1")]; b_wm = [B_("wm0"), B_("wm1")]
        b_mod = B_("modout")
        kb.dma(sp, [lambda e, j=j: e.dma_start(out=greps[j], in_=g_d[j].partition_broadcast(128)) for j in range(4)], writes=[b_g])
        kb.dma(sp, lambda e: e.dma_start(out=cTt, in_=cT_d), writes=[b_c])
        kb.op(act, lambda e: e.activation(out=sil, in_=cTt, func=AF.Silu), reads=[b_c], writes=[b_sil])
        for j in range(16):
            kb.op(dve, lambda e, j=j: e.tensor_copy(out=silrep[:, j, :], in_=sil[:, j:j + 1].to_broadcast([128, 128])),
                  reads=[b_sil], writes=[b_sil])
        gi = 0
        for j in range(6):
            for half in range(2):
                c0 = j * D + half * 512
                s = gi % 2
                gi += 1
                kb.dma(pool, lambda e, s=s, c0=c0: e.dma_start(out=wm[s], in_=wmod_v[:, :, c0:c0 + 512]), writes=[b_wm[s]])
                kb.dma(sp, lambda e, s=s, c0=c0: e.dma_start(out=bm[s], in_=bmod_d[0:1, c0:c0 + 512].partition_broadcast(128)), writes=[b_bm[s]])
                cs = slice(half * 512, half * 512 + 512)
                for which in range(2 if j < 2 else 1):
                    bk = 2 * s + which
                    kb.group(pe, [lambda e, k=k, bk=bk, s=s, which=which: e.matmul(banks[bk], lhsT=silrep[:, which * 8 + k, :], rhs=wm[s][:, k, :],
                                                                                  start=(k == 0), stop=(k == 7)) for k in range(8)],
                             reads=[b_sil, b_wm[s]], writes=[bbufs[bk]])
                    if j == 0 or j == 3:
                        dst = (sh1 if j == 0 else sh2[:]) if which == 0 else csh1
                        kb.op(dve, lambda e, bk=bk, s=s, dst=dst, cs=cs: e.tensor_tensor(out=dst[:, cs], in0=banks[bk], in1=bm[s], op=ALU.add),
                              reads=[bbufs[bk], b_bm[s]], writes=[b_mod])
                    else:
                        kb.op(dve, lambda e, bk=bk, s=s: e.tensor_tensor(out=tmpm, in0=banks[bk], in1=bm[s], op=ALU.add),
                              reads=[bbufs[bk], b_bm[s]], writes=[b_tmp])
                        if j == 1:
                            dst, gr = (G1, greps[0]) if which == 0 else (G1c, greps[0])
                        elif j == 2:
                            dst, gr = P1g[:], greps[1]
                        elif j == 4:
                            dst, gr = G2[:], greps[2]
                        else:
                            dst, gr = P2g[:], greps[3]
                        if j in (1, 4):
                            kb.op(dve, lambda e, dst=dst, gr=gr, cs=cs: e.scalar_tensor_tensor(out=dst[:, cs], in0=tmpm, scalar=1.0, in1=gr[:, cs], op0=ALU.add, op1=ALU.mult),
                                  reads=[b_tmp, b_g], writes=[b_mod])
                        else:
                            kb.op(dve, lambda e, dst=dst, gr=gr, cs=cs: e.tensor_tensor(out=dst[:, cs], in0=tmpm, in1=gr[:, cs], op=ALU.mult),
                                  reads=[b_tmp, b_g], writes=[b_mod])
        kb.barrier()
        if stop_after == 1:
            kb.emit()
            return nc

        AB.reset()
        hxT = AB.take(8 * (S + CT), [8, S + CT])
        yT = [AB.take(4 * S, [4, S]) for _ in range(2)]
        AB_base = AB.off
        AFr.reset(4 * D)
        xt = [AFr.take(D) for _ in range(2)]
        tmpf = AFr.take(D)
        hxb = [AB.take(D) for _ in range(2)]
        b_xt = [B_("xt0"), B_("xt1")]; b_tmpf = B_("tmpf"); b_hxb = [B_("hxb0"), B_("hxb1")]
        b_small = B_("small"); b_hxT = B_("hxT")

        def rms_rstd(src_ap, src_bufs, psum=False):
            c1, c2 = col(), col()
            if psum:
                kb.op(act, lambda e: e.activation(out=junkb[:], in_=src_ap, func=AF.Square, accum_out=c1), reads=list(src_bufs), writes=[b_small])
            else:
                kb.op(dve, lambda e: e.scalar_tensor_tensor(out=junkb[:], in0=src_ap, scalar=1.0, in1=src_ap, op0=ALU.mult, op1=ALU.mult, accum_out=c1),
                      reads=list(src_bufs), writes=[b_small])
            kb.op(dve, lambda e: e.tensor_scalar(out=c2, in0=c1, scalar1=1.0 / D, scalar2=EPS, op0=ALU.mult, op1=ALU.add), reads=[b_small], writes=[b_small])
            kb.op(act, lambda e: e.activation(out=c2, in_=c2, func=AF.Ln), reads=[b_small], writes=[b_small])
            kb.op(act, lambda e: e.activation(out=c2, in_=c2, func=AF.Exp, scale=-0.5), reads=[b_small], writes=[b_small])
            return c2

        tmpf2 = [tmpf, AFr.take(D)]
        b_tmpf2 = [b_tmpf, B_("tmpf1")]

        def p1_a(i):
            s = i % 2
            src = x_d[i * 128:(i + 1) * 128, :] if i < NT else ctx_d[(i - NT) * 128:(i - NT + 1) * 128, :]
            Gm, shm = (G1, sh1) if i < NT else (G1c, csh1)
            kb.dma(sp, lambda e: e.dma_start(out=xt[s], in_=src), writes=[b_xt[s]])
            r = rms_rstd(xt[s], [b_xt[s]])
            kb.op(dve, lambda e: e.scalar_tensor_tensor(out=tmpf2[s], in0=xt[s], scalar=r, in1=Gm, op0=ALU.mult, op1=ALU.mult),
                  reads=[b_xt[s], b_small, b_mod], writes=[b_tmpf2[s]])
            kb.op(pool, lambda e: e.tensor_tensor(out=hxb[s], in0=tmpf2[s], in1=shm, op=ALU.add), reads=[b_tmpf2[s], b_mod], writes=[b_hxb[s]])

        def p1_b(i):
            s = i % 2
            pb = psA if s == 0 else psB
            pbuf = bA if s == 0 else bB
            pbv = pb[:, 0:512].bitcast(BF16)
            kb.group(pe, [lambda e, k=k: e.transpose(pbv[:, k * 128:(k + 1) * 128], hxb[s][:, k * 128:(k + 1) * 128], identb[:]) for k in range(8)],
                     reads=[b_hxb[s], consts], writes=[pbuf])
            kb.op(act, lambda e: e.activation(out=hxT[:, :, i * 128:(i + 1) * 128], in_=pbv.rearrange("p (k t) -> p k t", k=8), func=AF.Copy),
                  reads=[pbuf], writes=[b_hxT])

        p1_a(0)
        for i in range(NTT):
            if i + 1 < NTT:
                p1_a(i + 1)
            p1_b(i)
        kb.barrier()
        dump("hxT", hxT, BF16)
        if stop_after == 2:
            kb.emit()
            return nc

        def attn_branch(kind, hg):
            AB.reset(AB_base); AFr.reset(0)
            if kind == "A":
                heads = list(range(8)); npair = 4; nkt = 2; nv = 2
                qc0, vc0 = 0, 640
            else:
                heads = list(range(4 * hg, 4 * hg + 4)); npair = 2; nkt = 2; nv = 4
                qc0, kc0, vc0 = 768 + hg * 256, 1280 + hg * 256, 1792 + hg * 256
            nh = len(heads)
            Wq = AB.take(8 * npair * 128, [8, npair * 128])
            Wk = AB.take(8 * nkt * 128, [8, nkt * 128])
            Wv = AB.take(8 * nv * 64, [8, nv * 64])
            QT = AB.take(npair * S, [npair, S])
            KT = AB.take(nkt * (S + CT), [nkt, S + CT])
            V = AB.take(NTT * nv * 80, [NTT, nv, 80])
            PT = [AB.take(7 * 128) for _ in range(2)]
            ytile = [AB.take(4 * 64, [4, 64]) for _ in range(2)]
            qf = AB.take(BS)
            b_W, b_QT, b_KT, b_V = B_("W"), B_("QT"), B_("KT"), B_("V")
            b_PT = [B_("PT0"), B_("PT1")]; b_yt = [B_("yt0"), B_("yt1")]; b_qf = B_("qf")
            b_yT = B_("yT")
            fl = [lambda e: e.dma_start(out=Wq, in_=win_v[:, :, qc0:qc0 + npair * 128]),
                  lambda e: e.dma_start(out=Wv, in_=win_v[:, :, vc0:vc0 + nv * 64])]
            if kind == "A":
                Wk4 = Wk.rearrange("p k (a b c) -> p k a b c", a=2, b=2)
                src = win_v[:, :, 512:640].rearrange("p k (a c) -> p k a c", a=2)
                for a_ in range(2):
                    for d_ in range(2):
                        fl.append(lambda e, a_=a_, d_=d_: e.dma_start(out=Wk4[:, :, a_, d_, :], in_=src[:, :, a_, :]))
            else:
                fl.append(lambda e: e.dma_start(out=Wk, in_=win_v[:, :, kc0:kc0 + nkt * 128]))
            kb.dma(pool, fl, writes=[b_W])
            kb.op(dve, lambda e: e.memset(V[:, :, :, 64:65], 1.0), writes=[b_V])
            if kind == "A":
                cos2 = AFr.take(S); sin2 = AFr.take(S)
                t1 = AFr.take(BS); t2 = AFr.take(BS)
                b_cs, b_t1, b_t2 = B_("cs"), B_("t1"), B_("t2")
                kb.dma(sp, [lambda e: e.dma_start(out=cos2, in_=cos_d), lambda e: e.dma_start(out=sin2, in_=sin_d)], writes=[b_cs])
            else:
                bres = AB.take(5 * 4 * 128, [5, 4, 128])
                bdyn = AB.take(5 * 4 * 128, [5, 4, 128])
                bst = AFr.take(4 * 128, [4, 128]); mst = AFr.take(128)
                b_bres, b_bdyn, b_bst = B_("bres"), B_("bdyn"), B_("bst")

                def load_pat(pi, dst, dbuf):
                    kb.dma(sp, [lambda e: e.dma_start(out=bst, in_=rpbx_d[pi, :, 4 * hg:4 * hg + 4, :]),
                                lambda e: e.dma_start(out=mst, in_=maskb_d[pi])], writes=[b_bst])
                    kb.op(dve, lambda e: e.tensor_tensor(out=dst, in0=bst, in1=mst.unsqueeze(1).to_broadcast([128, 4, 128]), op=ALU.add),
                          reads=[b_bst], writes=[dbuf])
                for pi, sl in res_slot.items():
                    load_pat(pi, bres[:, sl, :, :], b_bres)

            if stop_after == 2.1:
                kb.barrier()
                return
            def proj_fm(Wt, ct, col0, N, bk):
                kb.group(pe, [lambda e, k=k: e.matmul(banks[bk][:, 0:N], lhsT=Wt[:, k, ct * 128:(ct + 1) * 128], rhs=hxT[:, k, col0:col0 + N],
                                                      start=(k == 0), stop=(k == 7)) for k in range(8)],
                         reads=[b_W, b_hxT], writes=[bbufs[bk]])

            def rope(bk, bk2, dst, col0, dbuf):
                kb.op(act, lambda e: e.activation(out=qf, in_=banks[bk][:, 0:BS], func=AF.Copy), reads=[bbufs[bk]], writes=[b_qf])
                if stop_after == 2.31:
                    return
                kb.op(pe, lambda e: e.matmul(banks[bk2][:, 0:BS], lhsT=perm2[:], rhs=qf, start=True, stop=True), reads=[b_qf, consts], writes=[bbufs[bk2]])
                if stop_after == 2.32:
                    return
                kb.op(dve, lambda e: e.tensor_tensor(out=t1, in0=banks[bk][:, 0:BS], in1=cos2[:, col0:col0 + BS], op=ALU.mult), reads=[bbufs[bk], b_cs, b_qf], writes=[b_t1])
                if stop_after == 2.33:
                    return
                kb.op(dve, lambda e: e.tensor_tensor(out=t2, in0=banks[bk2][:, 0:BS], in1=sin2[:, col0:col0 + BS], op=ALU.mult), reads=[bbufs[bk2], b_cs], writes=[b_t2])
                if stop_after == 2.34:
                    return
                kb.op(pool, lambda e: e.tensor_tensor(out=dst, in0=t1, in1=t2, op=ALU.add), reads=[b_t1, b_t2], writes=[dbuf])

            n = 0
            for tb in range(NB):
                col0 = tb * BS
                for ct in range(npair):
                    bk = 4 + (n % 2); n += 1
                    proj_fm(Wq, ct, col0, BS, bk)
                    if kind == "A":
                        rope(bk, 6 + (n % 2), QT[:, ct, col0:col0 + BS], col0, b_QT)
                    else:
                        kb.op(act, lambda e, bk=bk, ct=ct, col0=col0: e.activation(out=QT[:, ct, col0:col0 + BS], in_=banks[bk][:, 0:BS], func=AF.Copy, scale=0.125),
                              reads=[bbufs[bk]], writes=[b_QT])
                for ct in range(nkt):
                    bk = 4 + (n % 2); n += 1
                    proj_fm(Wk, ct, col0, BS, bk)
                    if kind == "A":
                        rope(bk, 6 + (n % 2), KT[:, ct, col0:col0 + BS], col0, b_KT)
                    else:
                        kb.op(act, lambda e, bk=bk, ct=ct, col0=col0: e.activation(out=KT[:, ct, col0:col0 + BS], in_=banks[bk][:, 0:BS], func=AF.Copy),
                              reads=[bbufs[bk]], writes=[b_KT])
            for ct in range(nkt):
                bk = 4 + (n % 2); n += 1
                proj_fm(Wk, ct, S, CT, bk)
                kb.op(act, lambda e, bk=bk, ct=ct: e.activation(out=KT[:, ct, S:S + CT], in_=banks[bk][:, 0:CT], func=AF.Copy), reads=[bbufs[bk]], writes=[b_KT])
            for i in range(NTT):
                bk = 4 + (n % 2); n += 1
                kb.group(pe, [lambda e, k=k, bk=bk, i=i: e.matmul(banks[bk][:, 0:nv * 64], lhsT=hxT[:, k, i * 128:(i + 1) * 128], rhs=Wv[:, k, :],
                                                                  start=(k == 0), stop=(k == 7)) for k in range(8)],
                         reads=[b_W, b_hxT], writes=[bbufs[bk]])
                kb.op(dve, lambda e, bk=bk, i=i: e.tensor_copy(out=V[:, i, :, 0:64], in_=banks[bk][:, 0:nv * 64].rearrange("p (a c) -> p a c", a=nv)),
                      reads=[bbufs[bk]], writes=[b_V])

            dump("QT_%s%d" % (kind, hg), QT, BF16); dump("KT_%s%d" % (kind, hg), KT, BF16); dump("V_%s%d" % (kind, hg), V[:, :, :, 0:65], BF16)
            if stop_after == 2.5:
                kb.barrier()
                return
            if kind == "A":
                nrow = NSLOT + 512
                kb.dma(sp, [lambda e, r=r: e.dma_start(out=xs_d[r:r + 128, :], in_=zerob[:]) for r in range(0, nrow, 128)],
                       reads=[consts, b_V], writes=[xs_zero])
                kb.dma(sp, [lambda e, r=r: e.dma_start(out=ys_d[NSLOT + r:NSLOT + r + 128, :], in_=zerof[:]) for r in range(0, 512, 128)], reads=[consts], writes=[ys_zero])
            SP = [(psA, bA), (psB, bB)]
            units = []

            def mk_unit(u, qt, quad, hq, pre):
                qs = slice(qt * 128, (qt + 1) * 128)
                ob, obuf = banks[4 + (u // 4) % 2], bbufs[4 + (u // 4) % 2]
                hi = quad * 4 + hq
                h = heads[hi]
                half = hi % 2
                ps_ = slice(half * 64, half * 64 + 64)
                pair = hi // 2
                spt, sbuf_ = SP[u % 2]
                pt = PT[u % 2]
                st8 = {}

                def scores():
                    if kind == "B" and pre:
                        dyn_slot = {}
                        for c, pi in bchunks[qt]:
                            if pi not in res_slot and pi not in dyn_slot:
                                dyn_slot[pi] = len(dyn_slot)
                                load_pat(pi, bdyn[:, dyn_slot[pi], :, :], b_bdyn)
                    if kind == "A":
                        kti, vi = h // 4, h // 4
                        chunks = []
                        if qt > 0:
                            chunks.append((qt - 1, mA[:, 0, :], consts))
                        chunks.append((qt, None, None))
                        if qt < NT - 1:
                            chunks.append((qt + 1, mA[:, 1, :], consts))
                    else:
                        kti, vi = hi // 2, hi
                        dyn_slot = {}
                        for c, pi in bchunks[qt]:
                            if pi not in res_slot and pi not in dyn_slot:
                                dyn_slot[pi] = len(dyn_slot)
                        chunks = []
                        for c, pi in bchunks[qt]:
                            if pi in res_slot:
                                chunks.append((c, bres[:, res_slot[pi], hi, :], b_bres))
                            else:
                                chunks.append((c, bdyn[:, dyn_slot[pi], hi, :], b_bdyn))
                    for c in range(NCT):
                        chunks.append((NT + c, None, None))
                    ncu = len(chunks)
                    assert ncu <= 7
                    st8["chunks"], st8["vi"], st8["ncu"] = chunks, vi, ncu
                    fns = []
                    rd = [b_QT, b_KT, consts]
                    for ci, (kt_, bias, bb) in enumerate(chunks):
                        o_ = spt[:, ci * 128:(ci + 1) * 128]
                        fns.append(lambda e, o_=o_, kt_=kt_, bias=bias, kti=kti: e.matmul(
                            o_, lhsT=KT[ps_, kti, kt_ * 128:(kt_ + 1) * 128], rhs=QT[ps_, pair, qs], start=True, stop=(bias is None)))
                        if bias is not None:
                            fns.append(lambda e, o_=o_, bias=bias: e.matmul(o_, lhsT=identb[:], rhs=bias, start=False, stop=True))
                            if bb not in rd:
                                rd.append(bb)
                    kb.group(pe, fns, reads=rd, writes=[sbuf_])

                def rest():
                    chunks, vi, ncu = st8["chunks"], st8["vi"], st8["ncu"]
                    kb.op(act, lambda e: e.activation(out=pt[:, 0:ncu * 128], in_=spt[:, 0:ncu * 128], func=AF.Exp, scale=(0.125 if kind == "A" else 1.0)),
                          reads=[sbuf_], writes=[b_PT[u % 2]])
                    kb.group(pe, [lambda e, ci=ci, kt_=kt_: e.matmul(
                        ob[:, hq * 80:hq * 80 + 65], lhsT=pt[:, ci * 128:(ci + 1) * 128], rhs=V[:, kt_, vi, 0:65], start=(ci == 0), stop=(ci == ncu - 1))
                        for ci, (kt_, _, _) in enumerate(chunks)],
                             reads=[b_PT[u % 2], b_V], writes=[obuf] if hq == 0 else [], )
                    if hq > 0:
                        obuf.w = [pe.last()]
                    if hq < 3:
                        return
                    yi = ((u + 1) // 4) % 2
                    ob3 = ob[:, 0:320].rearrange("p (a c) -> p a c", a=4)
                    dn = dnt[:, yi, :]
                    if kind == "A":
                        kb.op(dve, lambda e: e.tensor_tensor(out=dn.unsqueeze(2), in0=ob3[:, :, 64:65], in1=esink[:, quad * 4:quad * 4 + 4].unsqueeze(2), op=ALU.add),
                              reads=[obuf, consts], writes=[b_small])
                        kb.op(dve, lambda e: e.reciprocal(out=dn, in_=dn), reads=[b_small], writes=[b_small])
                    else:
                        kb.op(dve, lambda e: e.reciprocal(out=dn.unsqueeze(2), in_=ob3[:, :, 64:65]), reads=[obuf], writes=[b_small])
                    kb.op(dve, lambda e: e.tensor_tensor(out=ytile[yi], in0=ob3[:, :, 0:64], in1=dn.unsqueeze(2).to_broadcast([128, 4, 64]), op=ALU.mult),
                          reads=[obuf, b_small], writes=[b_yt[yi]])
                    tb_, tbuf = banks[6 + yi], bbufs[6 + yi]
                    tbv = tb_.bitcast(BF16)
                    ytf = ytile[yi].rearrange("p a c -> p (a c)")
                    kb.group(pe, [lambda e, pr=pr: e.transpose(tbv[:, pr * 128:(pr + 1) * 128], ytf[:, pr * 128:(pr + 1) * 128], identb[:]) for pr in range(2)],
                             reads=[b_yt[yi], consts], writes=[tbuf])
                    yTd = yT[0] if kind == "A" else yT[1]
                    p0 = quad * 2 if kind == "A" else hg * 2
                    kb.op(act, lambda e: e.activation(out=yTd[:, p0:p0 + 2, qs], in_=tbv[:, 0:256].rearrange("p (a t) -> p a t", a=2), func=AF.Copy),
                          reads=[tbuf], writes=[b_yT])
                return scores, rest

            u = 0
            for qt in range(NT):
                for quad in range(nh // 4):
                    for hq in range(4):
                        units.append(mk_unit(u, qt, quad, hq, pre=(quad == 0 and hq == 0)))
                        u += 1
            units[0][0]()
            for i in range(len(units)):
                if i + 1 < len(units):
                    units[i + 1][0]()
                units[i][1]()
            kb.barrier()

        attn_branch("A", 0)
        if stop_after in (3, 2.5, 2.6, 2.7, 2.1, 2.2, 2.3, 2.31, 2.32, 2.33, 2.34):
            kb.emit()
            return nc
        attn_branch("B", 0)
        attn_branch("B", 1)
        dump("yaT", yT[0], BF16); dump("ybT", yT[1], BF16)
        if stop_after == 4:
            kb.emit()
            return nc

        AB.reset(AB_base); AFr.reset(0)
        mT = AB.take(8 * S, [8, S])
        AB_3b = AB.off
        wg = [[AB.take(8 * 128, [8, 128]) for _ in range(2)] for _ in range(2)]
        wbr = [[AB.take(4 * 128, [4, 128]) for _ in range(2)] for _ in range(2)]
        sg = [[AB.take(BS) for _ in range(2)] for _ in range(2)]
        m1 = [AFr.take(BS) for _ in range(2)]; m2 = [AFr.take(BS) for _ in range(2)]
        b_wg = [B_("wg0"), B_("wg1")]; b_sg = [[B_("sg00"), B_("sg01")], [B_("sg10"), B_("sg11")]]
        b_mT = B_("mT"); b_m1 = [B_("m10"), B_("m11")]; b_m2 = [B_("m20"), B_("m21")]
        wba_v = wba_d.rearrange("(k p) n -> p k n", p=128); wbb_v = wbb_d.rearrange("(k p) n -> p k n", p=128)

        def load_ct(ct):
            s = ct % 2
            kb.dma(pool, [lambda e: e.dma_start(out=wg[s][0], in_=win_v[:, :, 2304 + ct * 128:2304 + (ct + 1) * 128]),
                          lambda e: e.dma_start(out=wg[s][1], in_=win_v[:, :, 3328 + ct * 128:3328 + (ct + 1) * 128]),
                          lambda e: e.dma_start(out=wbr[s][0], in_=wba_v[:, :, ct * 128:(ct + 1) * 128]),
                          lambda e: e.dma_start(out=wbr[s][1], in_=wbb_v[:, :, ct * 128:(ct + 1) * 128])],
                   writes=[b_wg[s]])

        load_ct(0)
        n3 = 0
        for ct in range(8):
            s = ct % 2
            if ct + 1 < 8:
                load_ct(ct + 1)
            for tb in range(NB):
                cs_ = slice(tb * BS, (tb + 1) * BS)
                u3 = n3 % 2; n3 += 1
                for ab in range(2):
                    bk = 4 * u3 + ab
                    kb.group(pe, [lambda e, k=k, s=s, ab=ab, bk=bk, cs_=cs_: e.matmul(banks[bk][:, 0:BS], lhsT=wg[s][ab][:, k, :], rhs=hxT[:, k, cs_], start=(k == 0), stop=(k == 7))
                                  for k in range(8)], reads=[b_wg[s], b_hxT], writes=[bbufs[bk]])
                    kb.op(act, lambda e, u3=u3, ab=ab, bk=bk: e.activation(out=sg[u3][ab], in_=banks[bk][:, 0:BS], func=AF.Sigmoid), reads=[bbufs[bk]], writes=[b_sg[u3][ab]])
                    bk2 = 4 * u3 + 2 + ab
                    kb.group(pe, [lambda e, k=k, s=s, ab=ab, bk2=bk2, cs_=cs_: e.matmul(banks[bk2][:, 0:BS], lhsT=wbr[s][ab][:, k, :], rhs=yT[ab][:, k, cs_], start=(k == 0), stop=(k == 3))
                                  for k in range(4)], reads=[b_wg[s]], writes=[bbufs[bk2]])
                    mm, bmm = (m1[u3], b_m1[u3]) if ab == 0 else (m2[u3], b_m2[u3])
                    kb.op(dve, lambda e, u3=u3, ab=ab, bk2=bk2, mm=mm: e.tensor_tensor(out=mm, in0=banks[bk2][:, 0:BS], in1=sg[u3][ab], op=ALU.mult),
                          reads=[bbufs[bk2], b_sg[u3][ab]], writes=[bmm])
                kb.op(dve, lambda e, ct=ct, cs_=cs_, u3=u3: e.tensor_tensor(out=mT[:, ct, cs_], in0=m1[u3], in1=m2[u3], op=ALU.add), reads=[b_m1[u3], b_m2[u3]], writes=[b_mT])
        kb.barrier()

        AB4 = Arena(AB.t, AB.n, BF16)
        Wgu0 = AB4.take(8 * 2 * D, [8, 2 * D]); Wd0 = AB4.take(8 * D, [8, D])
        early_w0 = AB4.off <= AB_base
        Wgu1 = AB4.take(8 * 2 * D, [8, 2 * D]); Wd1 = AB4.take(8 * D, [8, D])
        AB4_off = AB4.off
        Wgu = [Wgu0, Wgu1]; Wd = [Wd0, Wd1]
        b_Wgu = [B_("Wgu0"), B_("Wgu1")]; b_Wd = [B_("Wd0"), B_("Wd1")]

        def load_wts(ex):
            s = ex % 2
            wgv = wgu_d[ex].rearrange("(k p) n -> p k n", p=128)
            wdv = wd_d[ex].rearrange("(k p) n -> p k n", p=128)
            kb.dma(pool, [lambda e, q4=q4: e.dma_start(out=Wgu[s][:, 2 * q4:2 * q4 + 2, :], in_=wgv[:, 2 * q4:2 * q4 + 2, :]) for q4 in range(4)],
                   writes=[b_Wgu[s]])
            kb.dma(pool, [lambda e, q2=q2: e.dma_start(out=Wd[s][:, 4 * q2:4 * q2 + 4, :], in_=wdv[:, 4 * q2:4 * q2 + 4, :]) for q2 in range(2)],
                   writes=[b_Wd[s]])

        if early_w0:
            load_wts(0)
        AB.reset(AB_3b); AFr.reset(0)
        Wout = AB.take(8 * D, [8, D])
        hx2b = [AB.take(D) for _ in range(2)]
        Mb = AB.take(E)
        xt3 = [AFr.take(D) for _ in range(2)]
        x1t = [AFr.take(D) for _ in range(2)]
        t3 = AFr.take(D); hx2f = [AFr.take(D) for _ in range(2)]
        hx2T = AFr.take(8 * 128, [8, 128])
        lg = AFr.take(E); mx8 = AFr.take(8); ew = AFr.take(4); dfull = AFr.take(E); spos = AFr.take(E); pen = AFr.take(E)
        junkE = AFr.take(E); destf = AFr.take(4); negm = AFr.take(1); sumw = AFr.take(1)
        b_Wout = B_("Wout")
        b_xt3 = [B_("xt30"), B_("xt31")]; b_x1t = [B_("x1t0"), B_("x1t1")]; b_t3 = B_("t3"); b_hx2f = [B_("hx2f0"), B_("hx2f1")]
        b_hx2b = [B_("hx2b0"), B_("hx2b1")]; b_hx2T = B_("hx2T"); b_r = B_("route"); b_Mb = B_("Mb"); b_cb = B_("cb")
        b_route_out = B_("routeout"); b_outd = Buf("outd", nowaw=True)
        kb.dma(pool, lambda e: e.dma_start(out=Wout, in_=wout_d.rearrange("(k p) n -> p k n", p=128)), writes=[b_Wout])

        def stage1(ti):
            s = ti % 2
            js = slice(ti * 128, (ti + 1) * 128)
            rows = js
            kb.dma(sp, lambda e: e.dma_start(out=xt3[s], in_=x_d[rows, :]), writes=[b_xt3[s]])
            yield
            for half in range(2):
                kb.group(pe, [lambda e, k=k, half=half: e.matmul(psC[:, half * 512:(half + 1) * 512], lhsT=mT[:, k, js], rhs=Wout[:, k, half * 512:(half + 1) * 512],
                                                                 start=(k == 0), stop=(k == 7)) for k in range(8)],
                         reads=[b_mT, b_Wout], writes=[bC] if half == 0 else [])
                yield
            bC.w = [pe.last()]
            r = rms_rstd(psC[:, :], [bC], psum=True)
            kb.op(dve, lambda e: e.scalar_tensor_tensor(out=t3, in0=psC[:, :], scalar=r, in1=P1g[:], op0=ALU.mult, op1=ALU.mult), reads=[bC, b_small], writes=[b_t3])
            yield
            kb.op(dve, lambda e: e.tensor_tensor(out=x1t[s], in0=t3, in1=xt3[s], op=ALU.add), reads=[b_t3, b_xt3[s]], writes=[b_x1t[s]])
            yield
            kb.dma(sp, lambda e: e.dma_start(out=out_d[rows, :], in_=x1t[s]), reads=[b_x1t[s]], writes=[b_outd], semof=b_x1t[s])
            yield
            r2 = rms_rstd(x1t[s], [b_x1t[s]])
            kb.op(dve, lambda e: e.scalar_tensor_tensor(out=t3, in0=x1t[s], scalar=r2, in1=G2[:], op0=ALU.mult, op1=ALU.mult), reads=[b_x1t[s], b_small], writes=[b_t3])
            yield
            kb.op(dve, lambda e: e.tensor_tensor(out=hx2f[s], in0=t3, in1=sh2[:], op=ALU.add), reads=[b_t3], writes=[b_hx2f[s]])
            yield
            kb.op(act, lambda e: e.activation(out=hx2b[s], in_=hx2f[s], func=AF.Copy), reads=[b_hx2f[s]], writes=[b_hx2b[s]])
            yield

        def stage2(ti):
            s = ti % 2
            kb.group(pe, [lambda e, k=k: e.transpose(psD[:, k * 128:(k + 1) * 128], hx2f[s][:, k * 128:(k + 1) * 128], identf[:]) for k in range(8)],
                     reads=[b_hx2f[s], consts], writes=[bD])
            yield
            kb.op(dve, lambda e: e.tensor_copy(out=hx2T, in_=psD[:, :].rearrange("p (k t) -> p k t", k=8)), reads=[bD], writes=[b_hx2T])
            yield
            kb.group(pe, [lambda e, k=k: e.matmul(banks[0][:, 0:E], lhsT=hx2T[:, k, :], rhs=wr_sb[:, k, :], start=(k == 0), stop=(k == 7)) for k in range(8)],
                     reads=[b_hx2T, consts], writes=[bbufs[0]])
            yield
            R = [b_r]
            kb.op(dve, lambda e: e.tensor_tensor(out=lg, in0=banks[0][:, 0:E], in1=brep[:], op=ALU.add), reads=[bbufs[0], consts], writes=R)
            yield
            kb.op(dve, lambda e: e.max(out=mx8, in_=lg), reads=R, writes=R)
            yield
            kb.op(dve, lambda e: e.tensor_scalar(out=negm, in0=mx8[:, 0:1], scalar1=-1.0, scalar2=None, op0=ALU.mult), reads=R, writes=R)
            yield
            kb.op(act, lambda e: e.activation(out=ew, in_=mx8[:, 0:4], func=AF.Exp, bias=negm, scale=1.0, accum_out=sumw), reads=R, writes=R)
            yield
            kb.op(dve, lambda e: e.reciprocal(out=sumw, in_=sumw), reads=R, writes=R)
            yield
            kb.op(dve, lambda e: e.tensor_scalar(out=wts[:, ti, :], in0=ew, scalar1=sumw, scalar2=None, op0=ALU.mult), reads=R, writes=[b_route_out])
            yield
            kb.op(dve, lambda e: e.tensor_scalar(out=Mb, in0=lg, scalar1=mx8[:, 3:4], scalar2=None, op0=ALU.is_ge), reads=R, writes=[b_Mb])
            yield
            kb.op(pe, lambda e: e.matmul(banks[1][:, 0:E], lhsT=tri[:], rhs=Mb, start=True, stop=True), reads=[b_Mb, consts], writes=[bbufs[1]])
            yield
            kb.op(pe, lambda e: e.matmul(banks[2][:, 0:E], lhsT=onesb[:], rhs=Mb, start=True, stop=True), reads=[b_Mb, consts], writes=[bbufs[2]])
            yield
            kb.op(dve, lambda e: e.tensor_tensor(out=spos, in0=banks[1][:, 0:E], in1=cb[:], op=ALU.add), reads=[bbufs[1], b_cb], writes=R)
            yield
            kb.op(dve, lambda e: e.tensor_tensor(out=cb[:], in0=banks[2][:, 0:E], in1=cb[:], op=ALU.add), reads=[bbufs[2], b_r], writes=[b_cb])
            yield
            kb.op(dve, lambda e: e.tensor_scalar(out=pen, in0=spos, scalar1=float(CAP), scalar2=1.0e6, op0=ALU.is_ge, op1=ALU.mult), reads=R, writes=R)
            yield
            kb.op(dve, lambda e: e.tensor_tensor(out=dfull, in0=spos, in1=baseE[:], op=ALU.add), reads=R + [consts], writes=R)
            yield
            kb.op(dve, lambda e: e.tensor_tensor(out=dfull, in0=dfull, in1=pen, op=ALU.add), reads=R, writes=R)
            yield
            for k in range(4):
                kb.op(dve, lambda e, k=k: e.scalar_tensor_tensor(out=junkE, in0=lg, scalar=mx8[:, k:k + 1], in1=dfull, op0=ALU.is_equal, op1=ALU.mult,
                                                                 accum_out=destf[:, k:k + 1]), reads=R, writes=R)
                yield
            kb.op(dve, lambda e: e.tensor_tensor(out=desti[:, ti, :], in0=destf, in1=trash[:], op=ALU.min), reads=R + [consts], writes=[b_route_out])
            yield
            kb.dma(pool, [lambda e, k=k: e.indirect_dma_start(out=xs_d[:, :], out_offset=bass.IndirectOffsetOnAxis(ap=desti[:, ti, k:k + 1], axis=0),
                                                             in_=hx2b[s], in_offset=None)
                          for k in range(4)], reads=[b_hx2b[s], b_route_out, xs_zero], writes=[xsb], semof=b_hx2b[s])
            yield

        def run_g(g):
            for _ in g:
                pass

        def zip_run(g1, g2):
            d1 = d2 = False
            while not (d1 and d2):
                if not d1:
                    try:
                        next(g1)
                    except StopIteration:
                        d1 = True
                if not d2:
                    try:
                        next(g2)
                    except StopIteration:
                        d2 = True

        run_g(stage1(0))
        for ti in range(NT):
            if ti + 1 < NT:
                zip_run(stage1(ti + 1), stage2(ti))
            else:
                run_g(stage2(ti))
        kb.barrier()
        dump("wts", wts[:], F32); dump("desti", desti[:], I32)
        if stop_after == 5:
            kb.emit()
            return nc

        AB.reset(AB4_off); AFr.reset(0)
        XT = [AB.take(8 * CAP, [8, CAP]) for _ in range(2)]
        xrows = AB.take(NJ * D, [NJ, D])
        hT = AB.take(8 * CAP, [8, CAP])
        gt = [AFr.take(CAP) for _ in range(2)]; sgm = [AFr.take(CAP) for _ in range(2)]
        ua = [AFr.take(CAP) for _ in range(2)]; glu = [AFr.take(CAP) for _ in range(2)]
        ysb = [AFr.take(512) for _ in range(4)]
        bdr = [AFr.take(D) for _ in range(2)]
        b_XT = [B_("XT0"), B_("XT1")]; b_xr, b_hT = B_("xrows"), B_("hT")
        b_gt = [B_("gt0"), B_("gt1")]; b_sgm = [B_("sgm0"), B_("sgm1")]; b_ua = [B_("ua0"), B_("ua1")]; b_glu = [B_("glu0"), B_("glu1")]
        b_ysb = [B_("ysb%d" % i) for i in range(4)]; b_bdr = [B_("bdr0"), B_("bdr1")]

        def load_w(ex):
            s = ex % 2
            if ex > 0 or not early_w0:
                load_wts(ex)
            kb.dma(sp, lambda e: e.dma_start(out=bdr[s], in_=bd_d[ex:ex + 1, :].partition_broadcast(128)), writes=[b_bdr[s]])

        def prep(ex):
            s = ex % 2
            kb.dma(sp, lambda e: e.dma_start(out=xrows, in_=xs_d[ex * CAP:(ex + 1) * CAP, :].rearrange("(j p) d -> p j d", p=128)), reads=[xsb], writes=[b_xr])
            for j in range(NJ):
                bk = j % 4
                pbv = banks[bk].bitcast(BF16)
                kb.group(pe, [lambda e, k=k, j=j, pbv=pbv: e.transpose(pbv[:, k * 128:(k + 1) * 128], xrows[:, j, k * 128:(k + 1) * 128], identb[:]) for k in range(8)],
                         reads=[b_xr, consts], writes=[bbufs[bk]])
                kb.op(act, lambda e, j=j, pbv=pbv: e.activation(out=XT[s][:, :, j * 128:(j + 1) * 128], in_=pbv.rearrange("p (k t) -> p k t", k=8), func=AF.Copy),
                      reads=[bbufs[bk]], writes=[b_XT[s]])

        load_w(0)
        prep(0)
        ny = 0
        for ex in range(E):
            s = ex % 2
            if ex + 1 < E:
                load_w(ex + 1)
            for f in range(8):
                gb_, ub_ = (0, 1) if f % 2 == 0 else (2, 3)
                fs = f % 2
                kb.group(pe, [lambda e, k=k, f=f, s=s, gb_=gb_: e.matmul(banks[gb_][:, 0:CAP], lhsT=Wgu[s][:, k, f * 128:(f + 1) * 128], rhs=XT[s][:, k, :], start=(k == 0), stop=(k == 7))
                              for k in range(8)], reads=[b_Wgu[s], b_XT[s]], writes=[bbufs[gb_]])
                kb.group(pe, [lambda e, k=k, f=f, s=s, ub_=ub_: e.matmul(banks[ub_][:, 0:CAP], lhsT=Wgu[s][:, k, D + f * 128:D + (f + 1) * 128], rhs=XT[s][:, k, :], start=(k == 0), stop=(k == 7))
                              for k in range(8)], reads=[b_Wgu[s], b_XT[s]], writes=[bbufs[ub_]])
                kb.op(dve, lambda e, f=f, ex=ex, gb_=gb_, fs=fs: e.tensor_scalar(out=gt[fs], in0=banks[gb_][:, 0:CAP], scalar1=bguT[:, f, ex:ex + 1], scalar2=7.0, op0=ALU.add, op1=ALU.min),
                      reads=[bbufs[gb_], consts], writes=[b_gt[fs]])
                kb.op(act, lambda e, fs=fs: e.activation(out=sgm[fs], in_=gt[fs], func=AF.Sigmoid, scale=1.702), reads=[b_gt[fs]], writes=[b_sgm[fs]])
                kb.op(dve, lambda e, f=f, ex=ex, ub_=ub_, fs=fs: e.tensor_scalar(out=ua[fs], in0=banks[ub_][:, 0:CAP], scalar1=bguT[:, 8 + f, ex:ex + 1], scalar2=8.0, op0=ALU.add, op1=ALU.min),
                      reads=[bbufs[ub_], consts], writes=[b_ua[fs]])
                kb.op(pool, lambda e, fs=fs: e.tensor_tensor(out=glu[fs], in0=gt[fs], in1=sgm[fs], op=ALU.mult), reads=[b_gt[fs], b_sgm[fs]], writes=[b_glu[fs]])
                kb.op(dve, lambda e, f=f, fs=fs: e.scalar_tensor_tensor(out=hT[:, f, :], in0=ua[fs], scalar=-6.0, in1=glu[fs], op0=ALU.max, op1=ALU.mult), reads=[b_ua[fs], b_glu[fs]], writes=[b_hT])
            if ex + 1 < E:
                prep(ex + 1)
            for j in range(NJ):
                js = slice(j * 128, (j + 1) * 128)
                r0 = ex * CAP + j * 128
                for half in range(2):
                    yb_ = ny % 4; ny += 1
                    bk = 4 + yb_
                    kb.group(pe, [lambda e, k=k, half=half, s=s, js=js, bk=bk: e.matmul(banks[bk], lhsT=hT[:, k, js], rhs=Wd[s][:, k, half * 512:(half + 1) * 512],
                                                                                      start=(k == 0), stop=(k == 7)) for k in range(8)],
                             reads=[b_hT, b_Wd[s]], writes=[bbufs[bk]])
                    kb.op(dve, lambda e, yb_=yb_, s=s, bk=bk, half=half: e.tensor_tensor(out=ysb[yb_], in0=banks[bk], in1=bdr[s][:, half * 512:(half + 1) * 512], op=ALU.add),
                          reads=[bbufs[bk], b_bdr[s]], writes=[b_ysb[yb_]])
                    kb.dma(sp, lambda e, yb_=yb_, r0=r0, half=half: e.dma_start(out=ys_d[r0:r0 + 128, half * 512:(half + 1) * 512], in_=ysb[yb_]), reads=[b_ysb[yb_]], writes=[ysb_b], semof=b_ysb[yb_])
        kb.barrier()
        if stop_after == 6:
            kb.emit()
            return nc

        AFr.reset(0); AB.reset(0)
        Yk = [[AB.take(2 * D).bitcast(F32) for _ in range(4)] for _ in range(2)]
        x1r = [AFr.take(D) for _ in range(2)]; acc = AFr.take(D); ot = [AFr.take(D) for _ in range(2)]
        b_Yk = [[B_("Yk%d%d" % (st_, k)) for k in range(4)] for st_ in range(2)]
        b_x1r = [B_("x1r0"), B_("x1r1")]; b_acc = B_("acc"); b_ot = [B_("ot0"), B_("ot1")]
        b_fin = Buf("fin", nowaw=True)

        def gath(ti):
            st_ = ti % 2
            rows = slice(ti * 128, (ti + 1) * 128)
            for k in range(4):
                kb.dma(pool, lambda e, k=k: e.indirect_dma_start(out=Yk[st_][k], out_offset=None, in_=ys_d[:, :],
                                                                 in_offset=bass.IndirectOffsetOnAxis(ap=desti[:, ti, k:k + 1], axis=0)),
                       reads=[ysb_b, ys_zero, b_route_out], writes=[b_Yk[st_][k]])
            kb.dma(sp, lambda e: e.dma_start(out=x1r[st_], in_=out_d[rows, :]), reads=[b_outd], writes=[b_x1r[st_]])

        gath(0)
        for ti in range(NT):
            st_ = ti % 2
            rows = slice(ti * 128, (ti + 1) * 128)
            if ti + 1 < NT:
                gath(ti + 1)
            kb.op(dve, lambda e, ti=ti, st_=st_: e.tensor_scalar(out=acc, in0=Yk[st_][0], scalar1=wts[:, ti, 0:1], scalar2=None, op0=ALU.mult), reads=[b_Yk[st_][0], b_route_out], writes=[b_acc])
            for k in range(1, 4):
                kb.op(dve, lambda e, ti=ti, k=k, st_=st_: e.scalar_tensor_tensor(out=acc, in0=Yk[st_][k], scalar=wts[:, ti, k:k + 1], in1=acc, op0=ALU.mult, op1=ALU.add),
                      reads=[b_Yk[st_][k], b_route_out, b_acc], writes=[b_acc])
            r = rms_rstd(acc, [b_acc])
            kb.op(dve, lambda e, r=r: e.scalar_tensor_tensor(out=acc, in0=acc, scalar=r, in1=P2g[:], op0=ALU.mult, op1=ALU.mult), reads=[b_acc, b_small], writes=[b_acc])
            kb.op(dve, lambda e, st_=st_: e.tensor_tensor(out=ot[st_], in0=acc, in1=x1r[st_], op=ALU.add), reads=[b_acc, b_x1r[st_]], writes=[b_ot[st_]])
            kb.dma(sp, lambda e, rows=rows, st_=st_: e.dma_start(out=out_d[rows, :], in_=ot[st_]), reads=[b_ot[st_], b_x1r[st_]], writes=[b_fin], semof=b_ot[st_])
        kb.barrier()
        kb.emit()
    return nc


def host_inputs(cfg, inp, b):
    S, CT, E, CAP = cfg["S"], cfg["CT"], cfg["E"], cfg["CAP"]
    f = np.float32
    c = np.asarray(inp["c"][b], f); cc = np.asarray(inp["c_ctx"], f)
    cT = np.concatenate([c.reshape(8, 128).T, cc.reshape(8, 128).T], axis=1)
    pats, _ = nbr_patterns(S)
    rpb = np.asarray(inp["b_rpb"][0], f)
    rpbx = np.stack([rpb[:, dr, dc].transpose(1, 0, 2) for ok, dr, dc in pats]).astype(f)
    maskb = np.stack([np.where(ok, 0.0, NEG) for ok, dr, dc in pats]).astype(f)
    cos2, sin2, perm2 = rope_tables(S)
    kj = np.arange(128)[:, None]; qi = np.arange(128)[None, :]
    maskA = np.stack([np.where(qi <= kj, 0.0, NEG), np.where(kj <= qi, 0.0, NEG)]).astype(ml_dtypes.bfloat16)
    tri = (kj < qi).astype(ml_dtypes.bfloat16)
    return {
        "x": np.ascontiguousarray(inp["x"][b], f), "ctx": np.ascontiguousarray(inp["ctx"][b], f), "cT": np.ascontiguousarray(cT, f),
        "w_mod": np.asarray(inp["w_mod"][0], f), "b_mod": np.asarray(inp["b_mod"], f).reshape(1, -1),
        "g_pre_mix": np.asarray(inp["g_pre_mix"], f).reshape(1, -1), "g_post_mix": np.asarray(inp["g_post_mix"], f).reshape(1, -1),
        "g_pre_ffn": np.asarray(inp["g_pre_ffn"], f).reshape(1, -1), "g_post_ffn": np.asarray(inp["g_post_ffn"], f).reshape(1, -1),
        "w_in": np.asarray(inp["w_in"][0], f), "a_sink": np.asarray(inp["a_sink"], f).reshape(1, 8),
        "rpbx": rpbx, "maskb": maskb,
        "w_branch_a": np.asarray(inp["w_branch_a"][0], f), "w_branch_b": np.asarray(inp["w_branch_b"][0], f), "w_out": np.asarray(inp["w_out"][0], f),
        "w_router": np.asarray(inp["w_router"][0], f), "b_router": np.asarray(inp["b_router"], f).reshape(1, -1),
        "w_gate_up": np.asarray(inp["w_gate_up"][0], f), "b_gate_up": np.asarray(inp["b_gate_up"][0], f),
        "w_down": np.asarray(inp["w_down"][0], f), "b_down": np.asarray(inp["b_down"][0], f),
        "identf": np.eye(128, dtype=f), "tri": tri, "identb": np.eye(128).astype(ml_dtypes.bfloat16),
        "maskA": maskA, "perm2": perm2.astype(ml_dtypes.bfloat16), "cos2": cos2, "sin2": sin2,
        "baseE": np.tile((np.arange(E, dtype=f) * CAP)[None, :], (128, 1)).astype(f),
        "trash": (E * CAP + np.arange(4, dtype=f)[None, :] * 128 + np.arange(128, dtype=f)[:, None]).astype(f),
    }


def kernel(**inputs):
    cfg = FULL
    nb = inputs["x"].shape[0]
    nc = build(cfg)
    in_maps = [host_inputs(cfg, inputs, b) for b in range(nb)]
    res = run_bass_kernel_spmd(nc, in_maps, core_ids=list(range(nb)))
    return np.stack([np.asarray(r["out"], np.float32) for r in res.results], axis=0)
```
